# Optimizing a Trainium2 kernel written in Bass

```python
import math
import jax, jax.numpy as jnp
from jax import lax
import numpy as np

D_MODEL = 1024
BATCH = 8
SEQ = 4096
DEPTH = 1

MIX_WIDTH = D_MODEL
DIFF_WIDTH = MIX_WIDTH // 2
RET_WIDTH = MIX_WIDTH - DIFF_WIDTH
N_DIFF_HEADS = 4
DIFF_V_DIM = DIFF_WIDTH // N_DIFF_HEADS
DIFF_QK_DIM = DIFF_V_DIM // 2
N_RET_HEADS = 4
RET_V_DIM = RET_WIDTH // N_RET_HEADS
RET_QK_DIM = RET_V_DIM // 2
DIFF_QK_COLS = N_DIFF_HEADS * 2 * DIFF_QK_DIM
RET_QK_COLS = N_RET_HEADS * RET_QK_DIM
SPLIT_SIZES = (DIFF_QK_COLS, DIFF_QK_COLS, DIFF_WIDTH, RET_QK_COLS, RET_QK_COLS, RET_WIDTH, RET_WIDTH)
SPLIT_POINTS = (512, 1024, 1536, 1792, 2048, 2560)
IN_COLS = 3072

ROPE_THETA = 500000.0
ROPE_FRACTION = 4
RET_THETA = 10000.0
Q_BLOCK = 128
RET_CHUNK = 128

N_KEYS = 128
N_EXPERTS = N_KEYS * N_KEYS
PEER_HEADS = 8
PEER_TOPK = 16
PEER_KEY_DIM = 256
PEER_TOKEN_BLOCK = 128

EPS = 1e-6

kernel_name = 'hybrid_diffattn_retnet_peer_encoder'


def rmsnorm(x, g):
    x32 = x.astype(jnp.float32)
    y = x32 * lax.rsqrt(jnp.mean(x32 * x32, axis=-1, keepdims=True) + EPS)
    return (y * g.astype(jnp.float32)).astype(x.dtype)


def group_norm(o, g):
    mu = jnp.mean(o, axis=-1, keepdims=True)
    c = o - mu
    var = jnp.mean(c * c, axis=-1, keepdims=True)
    return c * lax.rsqrt(var + EPS) * g.astype(jnp.float32)


def rotate_prefix(x, pos, inv_freq):
    half = inv_freq.shape[0]
    rot = 2 * half
    ang = pos[:, None] * inv_freq[None, :]
    shape = (1, pos.shape[0]) + (1,) * (x.ndim - 3) + (half,)
    cos = jnp.cos(ang).reshape(shape)
    sin = jnp.sin(ang).reshape(shape)
    xf = x.astype(jnp.float32)
    x1 = xf[..., :half]
    x2 = xf[..., half:rot]
    out = jnp.concatenate([x1 * cos - x2 * sin, x2 * cos + x1 * sin, xf[..., rot:]], axis=-1)
    return out.astype(x.dtype)


def diff_attention(q, k, v, lam):
    B, S, H, _, d = q.shape
    nb = S // Q_BLOCK
    scale = d ** -0.5
    q_blocks = q.reshape(B, nb, Q_BLOCK, H, 2, d).transpose(1, 0, 2, 3, 4, 5)

    def one_block(qb):
        s = jnp.einsum('bqhmd,bkhmd->bhmqk', qb, k).astype(jnp.float32) * scale
        p = jax.nn.softmax(s, axis=-1)
        a = p[:, :, 0] - lam * p[:, :, 1]
        return jnp.einsum('bhqk,bkhe->bqhe', a.astype(v.dtype), v)

    o = lax.map(one_block, q_blocks)
    return o.transpose(1, 0, 2, 3, 4).reshape(B, S, H, v.shape[-1])


def retention_direction(q, k, v, log_gamma, strict):
    B, S, H, dk = q.shape
    dv = v.shape[-1]
    C = RET_CHUNK
    nc = S // C
    qc = q.reshape(B, nc, C, H, dk).transpose(1, 0, 3, 2, 4)
    kc = k.reshape(B, nc, C, H, dk).transpose(1, 0, 3, 2, 4)
    vc = v.reshape(B, nc, C, H, dv).transpose(1, 0, 3, 2, 4)
    idx = jnp.arange(C, dtype=jnp.float32)
    dist = idx[:, None] - idx[None, :]
    mask = (dist > 0) if strict else (dist >= 0)
    lg = log_gamma[:, None, None]
    decay_in = jnp.where(mask[None], jnp.exp(lg * jnp.maximum(dist, 0.0)[None]), 0.0)
    q_decay = jnp.exp(log_gamma[:, None] * (idx + 1.0)[None])[None, :, :, None]
    k_decay = jnp.exp(log_gamma[:, None] * (C - 1.0 - idx)[None])[None, :, :, None]
    chunk_decay = jnp.exp(log_gamma * C)[None, :, None, None]

    def step(state, inp):
        qi, ki, vi = inp
        inner = jnp.einsum('bhid,bhjd->bhij', qi, ki) * decay_in[None]
        o = jnp.einsum('bhij,bhjv->bhiv', inner, vi) + jnp.einsum('bhid,bhdv->bhiv', qi * q_decay, state)
        state = state * chunk_decay + jnp.einsum('bhjd,bhjv->bhdv', ki * k_decay, vi)
        return state, o

    state0 = jnp.zeros((B, H, dk, dv), jnp.float32)
    _, o = lax.scan(step, state0, (qc, kc, vc))
    return o.transpose(1, 0, 3, 2, 4).reshape(B, S, H, dv)


def peer_ffn(xn, w_query, sub_keys, expert_down, expert_up):
    B, S, D = xn.shape
    T = B * S
    nb = T // PEER_TOKEN_BLOCK
    x_blocks = xn.reshape(nb, PEER_TOKEN_BLOCK, D)

    def one_block(xb):
        tb = xb.shape[0]
        q = (xb @ w_query).reshape(tb, PEER_HEADS, 2, PEER_KEY_DIM // 2)
        s = jnp.einsum('thpd,hpnd->thpn', q, sub_keys).astype(jnp.float32)
        top_s, top_i = lax.top_k(s, PEER_TOPK)
        cand_s = (top_s[:, :, 0, :, None] + top_s[:, :, 1, None, :]).reshape(tb, PEER_HEADS, PEER_TOPK * PEER_TOPK)
        cand_i = (top_i[:, :, 0, :, None] * N_KEYS + top_i[:, :, 1, None, :]).reshape(tb, PEER_HEADS, PEER_TOPK * PEER_TOPK)
        best_s, pos = lax.top_k(cand_s, PEER_TOPK)
        eid = jnp.take_along_axis(cand_i, pos, axis=-1)
        gate = jax.nn.softmax(best_s, axis=-1)
        u = jnp.take(expert_down, eid, axis=0)
        act = jax.nn.gelu(jnp.einsum('thkd,td->thk', u, xb).astype(jnp.float32), approximate=False)
        v = jnp.take(expert_up, eid, axis=0)
        return jnp.einsum('thk,thkd->td', (gate * act).astype(v.dtype), v)

    out = lax.map(one_block, x_blocks)
    return out.reshape(B, S, D).astype(xn.dtype)


def setup_inputs(seed: int = 0) -> dict:
    key = jax.random.key(seed)
    ks = jax.random.split(key, 16)
    f32 = jnp.float32
    nrm = jax.random.normal
    x = nrm(ks[0], (BATCH, SEQ, D_MODEL), f32)
    attn_norm_g = 1.0 + 0.02 * nrm(ks[1], (DEPTH, D_MODEL), f32)
    w_in = nrm(ks[2], (DEPTH, D_MODEL, IN_COLS), f32) * D_MODEL ** -0.5
    diff_lambda = 0.1 * nrm(ks[3], (DEPTH, 4, DIFF_QK_DIM), f32)
    diff_norm_g = 1.0 + 0.02 * nrm(ks[4], (DEPTH, DIFF_V_DIM), f32)
    gammas = 1.0 - jnp.exp2(-5.0 - jnp.arange(N_RET_HEADS, dtype=f32))
    base = jnp.log(-jnp.log(gammas))
    ret_log_decay = base[None, None, :] + 0.05 * nrm(ks[5], (DEPTH, 2, N_RET_HEADS), f32)
    ret_norm_g = 1.0 + 0.02 * nrm(ks[6], (DEPTH, N_RET_HEADS, RET_V_DIM), f32)
    w_out = nrm(ks[7], (DEPTH, MIX_WIDTH, D_MODEL), f32) * MIX_WIDTH ** -0.5
    ffn_norm_g = 1.0 + 0.02 * nrm(ks[8], (DEPTH, D_MODEL), f32)
    peer_w_query = nrm(ks[9], (DEPTH, D_MODEL, PEER_HEADS * PEER_KEY_DIM), f32) * D_MODEL ** -0.5
    peer_sub_keys = nrm(ks[10], (DEPTH, PEER_HEADS, 2, N_KEYS, PEER_KEY_DIM // 2), f32) * (PEER_KEY_DIM // 2) ** -0.5
    peer_u = nrm(ks[11], (DEPTH, N_EXPERTS, D_MODEL), f32) * D_MODEL ** -0.5
    peer_v = nrm(ks[12], (DEPTH, N_EXPERTS, D_MODEL), f32) * PEER_HEADS ** -0.5
    final_norm_g = 1.0 + 0.02 * nrm(ks[13], (D_MODEL,), f32)
    return {'x': x, 'attn_norm_g': attn_norm_g, 'w_in': w_in, 'diff_lambda': diff_lambda,
            'diff_norm_g': diff_norm_g, 'ret_log_decay': ret_log_decay, 'ret_norm_g': ret_norm_g,
            'w_out': w_out, 'ffn_norm_g': ffn_norm_g, 'peer_w_query': peer_w_query,
            'peer_sub_keys': peer_sub_keys, 'peer_u': peer_u, 'peer_v': peer_v,
            'final_norm_g': final_norm_g}


def reference(x, attn_norm_g, w_in, diff_lambda, diff_norm_g, ret_log_decay, ret_norm_g,
              w_out, ffn_norm_g, peer_w_query, peer_sub_keys, peer_u, peer_v, final_norm_g):
    B, S, _ = x.shape
    pos = jnp.arange(S, dtype=jnp.float32)
    rot_dim = DIFF_QK_DIM // ROPE_FRACTION
    rope_inv = jnp.power(jnp.float32(ROPE_THETA), -jnp.arange(rot_dim // 2, dtype=jnp.float32) * 2.0 / rot_dim)
    ret_inv = 1.0 / jnp.power(jnp.float32(RET_THETA), jnp.linspace(0.0, 1.0, RET_QK_DIM // 2, dtype=jnp.float32))

    for l in range(DEPTH):
        lambda_init = 0.8 - 0.6 * math.exp(-0.3 * l)
        h = rmsnorm(x, attn_norm_g[l])
        proj = h @ w_in[l]
        dq, dk, dv, rq, rk, rv, rg = jnp.split(proj, SPLIT_POINTS, axis=-1)

        dq = rotate_prefix(dq.reshape(B, S, N_DIFF_HEADS, 2, DIFF_QK_DIM), pos, rope_inv)
        dk = rotate_prefix(dk.reshape(B, S, N_DIFF_HEADS, 2, DIFF_QK_DIM), pos, rope_inv)
        dv = dv.reshape(B, S, N_DIFF_HEADS, DIFF_V_DIM)
        lam_p = diff_lambda[l].astype(jnp.float32)
        lam = jnp.exp(jnp.sum(lam_p[0] * lam_p[1])) - jnp.exp(jnp.sum(lam_p[2] * lam_p[3])) + lambda_init
        a = diff_attention(dq, dk, dv, lam)
        a = rmsnorm(a, diff_norm_g[l]).astype(jnp.float32) * (1.0 - lambda_init)
        a = a.reshape(B, S, DIFF_WIDTH)

        rq = rotate_prefix(rq.reshape(B, S, N_RET_HEADS, RET_QK_DIM), pos, ret_inv).astype(jnp.float32)
        rk = rotate_prefix(rk.reshape(B, S, N_RET_HEADS, RET_QK_DIM), pos, ret_inv).astype(jnp.float32) * RET_QK_DIM ** -0.5
        rv = rv.reshape(B, S, N_RET_HEADS, RET_V_DIM).astype(jnp.float32)
        log_gamma = -jnp.exp(ret_log_decay[l].astype(jnp.float32))
        r_fwd = retention_direction(rq, rk, rv, log_gamma[0], False)
        r_bwd = retention_direction(rq[:, ::-1], rk[:, ::-1], rv[:, ::-1], log_gamma[1], True)[:, ::-1]
        r = group_norm(r_fwd + r_bwd, ret_norm_g[l]).reshape(B, S, RET_WIDTH)
        r = jax.nn.silu(rg.astype(jnp.float32)) * r

        mix = jnp.concatenate([a, r], axis=-1).astype(x.dtype)
        x = x + mix @ w_out[l]

        x = x + peer_ffn(rmsnorm(x, ffn_norm_g[l]), peer_w_query[l], peer_sub_keys[l], peer_u[l], peer_v[l])

    return rmsnorm(x, final_norm_g)
```

```python
from contextlib import ExitStack
import math
import numpy as np
import concourse.bass as bass
import concourse.mybir as mybir
from concourse.bass_utils import run_bass_kernel_spmd

F32 = mybir.dt.float32
BF16 = mybir.dt.bfloat16
U32 = mybir.dt.uint32
AF = mybir.ActivationFunctionType
ALU = mybir.AluOpType
AX = mybir.AxisListType

ENGS = ("pe", "act", "dve", "pool", "sp")
S_TOK = 4096
D = 1024
NTB = S_TOK // 128
EPS = 1e-6
LAMBDA_INIT = 0.8 - 0.6 * math.exp(-0.3 * 0)
LN8 = math.log(0.125)
DBG = {"heads": list(range(8)), "nqb": 16, "post": True, "steps": True}


class Sched:
    def __init__(self, nc):
        self.nc = nc
        self.ops = []
        self.last_w = {}
        self.readers = {}
        self.chan_last = {}

    def add(self, eng, fn, reads=(), writes=(), chan=None):
        i = len(self.ops)
        deps = {}
        for k in reads:
            w = self.last_w.get(k)
            if w is not None:
                deps[w] = True
        for k in writes:
            w = self.last_w.get(k)
            if w is not None:
                deps.setdefault(w, False)
            for r in self.readers.get(k, ()):
                deps.setdefault(r, False)
        if chan is not None:
            p = self.chan_last.get(chan)
            if p is not None:
                deps[p] = True
            self.chan_last[chan] = i
        self.ops.append((eng, fn, deps, chan))
        for k in writes:
            self.last_w[k] = i
            self.readers[k] = []
        for k in reads:
            self.readers.setdefault(k, []).append(i)
        return i

    def barrier(self):
        self.ops.append(("BARRIER", None, {}, None))
        self.last_w.clear()
        self.readers.clear()
        self.chan_last.clear()

    def _skip(self, eng, chan, de, raw):
        return de == eng and chan is None and (eng == "pe" or not raw)

    def emit(self):
        nc = self.nc
        ops = self.ops
        n = len(ops)
        need = [False] * n
        for i, (eng, fn, deps, chan) in enumerate(ops):
            for d, raw in deps.items():
                de, _, _, dchan = ops[d]
                if dchan is not None:
                    continue
                if self._skip(eng, chan, de, raw):
                    continue
                need[d] = True
        cnt = {e: 0 for e in ENGS}
        chan_cnt = {}
        sig = [0] * n
        bar_snap = {}
        for i, (eng, fn, deps, chan) in enumerate(ops):
            if eng == "BARRIER":
                bar_snap[i] = dict(chan_cnt)
                continue
            if chan is not None:
                chan_cnt[chan] = chan_cnt.get(chan, 0) + 16
                sig[i] = chan_cnt[chan]
            elif need[i]:
                cnt[eng] += 1
                sig[i] = cnt[eng]
        with ExitStack() as es:
            engsem = {e: es.enter_context(nc.semaphore("sem_" + e)) for e in ENGS}
            bsem = es.enter_context(nc.semaphore("sem_bar"))
            chansem = {c: es.enter_context(nc.semaphore("dsem_%d" % j))
                       for j, c in enumerate(chan_cnt)}
            block = es.enter_context(nc.Block())

            def make(eng):
                def body(e):
                    waited = {}
                    nbar = 0
                    for i, (oeng, fn, deps, chan) in enumerate(ops):
                        if oeng == "BARRIER":
                            nbar += 1
                            if eng == "sp":
                                for c, v in bar_snap[i].items():
                                    if waited.get(("c", c), 0) < v:
                                        e.wait_ge(chansem[c], v)
                                        waited[("c", c)] = v
                            e.drain().then_inc(bsem, 1)
                            e.wait_ge(bsem, len(ENGS) * nbar)
                            continue
                        if oeng != eng:
                            continue
                        want = {}
                        for d, raw in deps.items():
                            de, _, _, dchan = ops[d]
                            if dchan is not None:
                                key = ("c", dchan)
                                s = chansem[dchan]
                            else:
                                if self._skip(eng, chan, de, raw):
                                    continue
                                key = ("e", de)
                                s = engsem[de]
                            v = sig[d]
                            if waited.get(key, 0) >= v:
                                continue
                            if key not in want or want[key][1] < v:
                                want[key] = (s, v)
                        for key, (s, v) in want.items():
                            e.wait_ge(s, v)
                            waited[key] = v
                        ins = fn(e)
                        if chan is not None:
                            ins.then_inc(chansem[chan], 16)
                        elif need[i]:
                            ins.then_inc(engsem[eng], 1)
                    if eng == "sp":
                        for c, v in chan_cnt.items():
                            if waited.get(("c", c), 0) < v:
                                e.wait_ge(chansem[c], v)
                return body

            block.tensor(make("pe"))
            block.scalar(make("act"))
            block.vector(make("dve"))
            block.gpsimd(make("pool"))
            block.sync(make("sp"))


def build(stage="full"):
    nc = bass.Bass("TRN2", target_bir_lowering=False)
    S = Sched(nc)

    def din(name, shape, dt=F32):
        return nc.dram_tensor(name, list(shape), dt, kind="ExternalInput").ap()

    x_d = din("x", [S_TOK, D])
    w_in_d = din("w_in", [D, 3072])
    gattn_d = din("gattn", [128, 8])
    ropd_d = din("ropd", [128, 2, NTB, 8])
    ropr_d = din("ropr", [128, 2, NTB, 32])
    dlam_d = din("dlam", [128, 256])
    rld_d = din("rld", [128, 8])
    rtab_d = din("rtab", [128, 10, 256])
    e128_d = din("e128", [128, 32])
    ident_d = din("ident", [128, 128])
    w_out_d = din("w_out", [D, D])
    gmix_d = din("gmix", [128, 8])
    wq_d = din("wq", [D, 2048])
    gffn_d = din("gffn", [128, 8])
    skT_d = din("skT", [128, 16, 128])
    uT_d = din("uT", [D, 16384])
    pv_d = din("pv", [16384, D])
    gfin_d = din("gfin", [128, D])
    iota_d = din("iota", [128, 128])
    io4_d = din("io4", [128, 2048])
    out_d = nc.dram_tensor("out", [S_TOK, D], F32, kind="ExternalOutput").ap()

    QKT = nc.dram_tensor("scr_qkt", [12, 128, S_TOK], BF16).ap()
    Vs = nc.dram_tensor("scr_v", [S_TOK, 512], BF16).ap()
    RVs = nc.dram_tensor("scr_rv", [S_TOK, 512], BF16).ap()
    RGs = nc.dram_tensor("scr_rg", [S_TOK, 512], BF16).ap()
    X2s = nc.dram_tensor("scr_x2", [S_TOK, D], F32).ap()
    UT2 = nc.dram_tensor("scr_ut2", [128, 128, 8, 128], BF16).ap()
    WQ2 = nc.dram_tensor("scr_wq2", [16, 128, 8, 128], BF16).ap()
    Vb = nc.dram_tensor("scr_vb", [16384, D], BF16).ap()

    def dma(out, in_, r, w, chan, eng="sp"):
        S.add(eng, lambda e: e.dma_start(out=out, in_=in_), r, w, chan=chan)

    def act(out, in_, func, r, w, scale=None, bias=None, accum=None):
        kw = {}
        if scale is not None:
            kw["scale"] = scale
        if bias is not None:
            kw["bias"] = bias
        if accum is not None:
            kw["accum_out"] = accum
        S.add("act", lambda e: e.activation(out=out, in_=in_, func=func, **kw), r, w)

    def vcopy(out, in_, r, w, eng="dve"):
        S.add(eng, lambda e: e.tensor_copy(out=out, in_=in_), r, w)

    def tt(out, in0, in1, op, r, w, eng="dve"):
        S.add(eng, lambda e: e.tensor_tensor(out=out, in0=in0, in1=in1, op=op), r, w)

    def ts(out, in0, s1, op0, r, w, s2=None, op1=None, eng="dve"):
        if op1 is None:
            S.add(eng, lambda e: e.tensor_scalar(out=out, in0=in0, scalar1=s1, scalar2=None, op0=op0), r, w)
        else:
            S.add(eng, lambda e: e.tensor_scalar(out=out, in0=in0, scalar1=s1, scalar2=s2, op0=op0, op1=op1), r, w)

    def stt(out, in0, scalar, in1, op0, op1, r, w):
        S.add("dve", lambda e: e.scalar_tensor_tensor(out=out, in0=in0, scalar=scalar, in1=in1, op0=op0, op1=op1), r, w)

    def ttr(out, in0, in1, accum, r, w):
        S.add("dve", lambda e: e.scalar_tensor_tensor(out=out, in0=in0, scalar=1.0, in1=in1, op0=ALU.mult,
                                                      op1=ALU.mult, accum_out=accum), r, w)

    def mm(out, lhsT, rhs, start, stop, r, w):
        S.add("pe", lambda e: e.matmul(out, lhsT=lhsT, rhs=rhs, start=start, stop=stop), r, w)

    def tr(out, in_, ident, r, w):
        S.add("pe", lambda e: e.transpose(out=out, in_=in_, identity=ident), r, w)

    def run_gen(gen, n):
        if gen is None:
            return
        for _ in range(n):
            try:
                next(gen)
            except StopIteration:
                return

    def recip(out, in_, r, w):
        S.add("dve", lambda e: e.reciprocal(out=out, in_=in_), r, w)

    def memset(ap, val, w, eng="dve"):
        S.add(eng, lambda e: e.memset(ap, val), (), w)

    with ExitStack() as top:
        def sbt(es, name, shape, dt):
            return es.enter_context(nc.sbuf_tensor("sb_" + name, list(shape), dt))

        def pst(es, name, shape, dt):
            return es.enter_context(nc.psum_tensor("ps_" + name, list(shape), dt))

        identf = sbt(top, "identf", [128, 128], F32)
        identb = sbt(top, "identb", [128, 128], BF16)
        eps_t = sbt(top, "eps_t", [128, 1], F32)
        ln8_t = sbt(top, "ln8_t", [128, 1], F32)
        junk = sbt(top, "junk", [128, D], BF16)
        small = sbt(top, "small", [128, 64], F32)
        dma(identf[:], ident_d, (), ["identf"], "c_id")
        vcopy(identb[:], identf[:], ["identf"], ["identb"])
        memset(eps_t[:], EPS, ["eps_t"])
        memset(ln8_t[:], LN8, ["ln8_t"])
        cm = ExitStack()
        mix = sbt(cm, "mix", [128, NTB, D], BF16)


        def PEER_PHASE():
          with ExitStack() as p3:
            gffn = sbt(p3, "gffn", [128, 8], F32)
            gfin = sbt(p3, "gfin", [128, D], BF16)
            skT = sbt(p3, "skT", [128, 16, 128], BF16)
            iota = sbt(p3, "iota", [128, 128], F32)
            io4 = sbt(p3, "io4", [128, 1024], BF16)
            dma(gffn[:], gffn_d, (), ["gffn"], "c_gf")
            dma(iota[:], iota_d, (), ["iota"], "c_io")
            with ExitStack() as pc:
                cf = [sbt(pc, "cf%d" % i, [128, 2048], F32) for i in range(2)]
                cb = [sbt(pc, "cb%d" % i, [128, 2048], BF16) for i in range(2)]
                j = 0

                def conv(s_, scale_ap, dst, even):
                    if scale_ap is None:
                        if even:
                            act(dst, cf[s_][:], AF.Copy, [("cf", s_)], [("cb", s_)])
                        else:
                            vcopy(dst, cf[s_][:], [("cf", s_)], [("cb", s_)])
                    elif even:
                        act(dst, cf[s_][:], AF.Copy, [("cf", s_), "gffn"], [("cb", s_)], scale=scale_ap)
                    else:
                        ts(dst, cf[s_][:], scale_ap, ALU.mult, [("cf", s_), "gffn"], [("cb", s_)])

                dma(cf[0][:, 0:D], gfin_d, (), [("cf", 0)], "c_cf0")
                vcopy(gfin[:], cf[0][:, 0:D], [("cf", 0)], ["gfin"])
                dma(cf[0][:], io4_d, (), [("cf", 0)], "c_cf0")
                vcopy(io4[:], cf[0][:, 0:1024], [("cf", 0)], ["io4"])
                dma(cf[1][:], skT_d.rearrange("p g n -> p (g n)"), (), [("cf", 1)], "c_cf1")
                vcopy(skT[:].rearrange("p g n -> p (g n)"), cf[1][:], [("cf", 1)], ["skT"])
                for k in range(8):
                    s_ = j % 2
                    dma(cf[s_][:], wq_d[k * 128:(k + 1) * 128, :], (), [("cf", s_)], "c_cf%d" % s_)
                    conv(s_, gffn[:, k:k + 1], cb[s_][:], j % 2 == 0)
                    dma(WQ2[:, :, k, :].rearrange("g p e -> p g e"), cb[s_][:].rearrange("p (g e) -> p g e", e=128),
                        [("cb", s_)], ["WQ2"], "s_cb%d" % s_)
                    j += 1
            S.barrier()
            with ExitStack() as pb:
                G = [sbt(pb, "G%d" % i, [128, 256, 128], BF16) for i in range(2)]
                NSL = 5
                ut8 = [sbt(pb, "ut8_%d" % i, [128, 8, 128], BF16) for i in range(NSL)]
                v8 = [sbt(pb, "v8_%d" % i, [128, D], BF16) for i in range(NSL)]
                wqp = [sbt(pb, "wqp%d" % i, [128, 8, 128], BF16) for i in range(2)]
                xnT = [sbt(pb, "xnT%d" % i, [128, 8, 256], BF16) for i in range(2)]
                x2s = sbt(pb, "x2s", [128, D], F32)
                xnb = sbt(pb, "xnb", [128, D], BF16)
                qTs = sbt(pb, "qTs", [128, 16, 128], BF16)
                buf1 = sbt(pb, "buf1", [128, 2048], F32)
                s2g = [sbt(pb, "s2g%d" % i, [128, 256], F32) for i in range(2)]
                eqt = sbt(pb, "eqt", [128, 1024], BF16)
                topv = sbt(pb, "topv", [128, 16, 16], F32)
                idxu = sbt(pb, "idxu", [128, 16, 16], U32)
                idxf = sbt(pb, "idxf", [128, 16, 16], F32)
                best = sbt(pb, "best", [128, 8, 16], F32)
                posu = sbt(pb, "posu", [128, 8, 16], U32)
                abu = sbt(pb, "abu", [128, 2, 128], U32)
                abf = sbt(pb, "abf", [128, 2, 128], F32)
                ijg = sbt(pb, "ijg", [128, 3, 128], F32)
                gsm = sbt(pb, "gsm", [128, 16], F32)
                ijgT = sbt(pb, "ijgT", [128, 3, 128], F32)
                At = [sbt(pb, "At%d" % i, [128, 128], BF16) for i in range(4)]
                Bt = [sbt(pb, "Bt%d" % i, [128, 128], BF16) for i in range(4)]
                ga = [sbt(pb, "ga%d" % i, [128, 256], BF16) for i in range(2)]
                gw = [sbt(pb, "gw%d" % i, [128, 256], BF16) for i in range(2)]
                st3 = sbt(pb, "st3", [128, 8], F32)
                big = [pst(pb, "big%d" % i, [128, 512], F32) for i in range(4)]
                pa2 = [pst(pb, "pa%d" % i, [128, 512], F32) for i in range(2)]
                pp = [pst(pb, "ppx%d" % i, [128, 512], F32) for i in range(2)]
                io4v = io4[:].rearrange("p (h k a) -> p h k a", h=4, k=16)
                eq4 = eqt[:].rearrange("p (h k a) -> p h k a", h=4, k=16)
                cand4 = buf1[:].rearrange("p (h a b) -> p h a b", h=8, a=16)
                SK = [("s", b4) for b4 in range(4)]
                NBLK = DBG.get("nblk", 16)
                ppc = [0]

                def nextpp():
                    ppc[0] += 1
                    return ppc[0] % 2

                def prologue(blk):
                    gb = blk % 2
                    for sub in range(2):
                        tb = blk * 2 + sub
                        dma(x2s[:], X2s[tb * 128:(tb + 1) * 128, :], (), ["x2s"], "l_x2s", eng=DBG.get("pdma", "sp"))
                        ttr(junk[:], x2s[:], x2s[:], st3[:, 0:1], ["x2s"], ["ss3", "junk"])
                        act(st3[:, 1:2], st3[:, 0:1], AF.Sqrt, ["ss3", "eps_t"], ["rs3"], scale=1.0 / D, bias=eps_t[:])
                        recip(st3[:, 2:3], st3[:, 1:2], ["rs3"], ["rstd3"])
                        act(xnb[:], x2s[:], AF.Copy, ["x2s", "rstd3"], ["xnb"], scale=st3[:, 2:3])
                        q_ = nextpp()
                        pT3 = pp[q_][:].bitcast(BF16).rearrange("p (k t) -> p k t", k=8)
                        for k in range(8):
                            tr(pT3[:, k, :], xnb[:, k * 128:(k + 1) * 128], identb[:], ["xnb", "identb"], [("pp", q_)])
                        vcopy(xnT[gb][:, :, sub * 128:(sub + 1) * 128], pT3, [("pp", q_)], [("xnT", gb, sub)])
                        yield
                    XN = [("xnT", gb, 0), ("xnT", gb, 1)]
                    def sub_gen(sub):
                        tsl = slice(sub * 128, (sub + 1) * 128)
                        for g in range(16):
                            ws = g % 2
                            dma(wqp[ws][:], WQ2[g], (), [("wqp", ws)], "l_wq%d" % ws, eng=DBG.get("pdma", "sp"))
                            q_ = nextpp()
                            for k in range(8):
                                mm(pp[q_][:, 0:128], wqp[ws][:, k, :], xnT[gb][:, k, tsl], k == 0, k == 7,
                                   XN + [("wqp", ws)], [("pp", q_)])
                            act(qTs[:, g, :], pp[q_][:, 0:128], AF.Copy, [("pp", q_)], [("qTs", g)])
                            if g % 4 == 3:
                                yield ("proj_done" if g == 15 else "proj")
                        for b4 in range(4):
                            q_ = nextpp()
                            for g in range(b4 * 4, b4 * 4 + 4):
                                mm(pp[q_][:, (g % 4) * 128:(g % 4 + 1) * 128], qTs[:, g, :], skT[:, g, :], True, True,
                                   [("qTs", g), "skT"], [("pp", q_)])
                            act(buf1[:, b4 * 512:(b4 + 1) * 512], pp[q_][:], AF.Copy, [("pp", q_)], [("s", b4), "cand"])
                            yield ("scores_done" if b4 == 3 else "scores")
                        for g in range(16):
                            sg = buf1[:, g * 128:(g + 1) * 128]
                            kk = ("s", g // 4)
                            z = s2g[g % 2][:, 0:128]
                            zk = ("s2g", g % 2)
                            S.add("dve", lambda e, g=g, sg=sg: e.max(out=topv[:, g, 0:8], in_=sg), [kk], [("topv", g, 0)])
                            S.add("dve", lambda e, g=g, sg=sg: e.max_index(out=idxu[:, g, 0:8], in_max=topv[:, g, 0:8], in_values=sg),
                                  [kk, ("topv", g, 0)], [("idxu", g, 0)])
                            S.add("dve", lambda e, g=g, sg=sg, z=z: e.match_replace(out=z, in_to_replace=topv[:, g, 0:8], in_values=sg, imm_value=-1e30),
                                  [kk, ("topv", g, 0)], [zk])
                            S.add("dve", lambda e, g=g, z=z: e.max(out=topv[:, g, 8:16], in_=z), [zk], [("topv", g, 1)])
                            S.add("dve", lambda e, g=g, z=z: e.max_index(out=idxu[:, g, 8:16], in_max=topv[:, g, 8:16], in_values=z),
                                  [zk, ("topv", g, 1)], [("idxu", g, 1)])
                            if g % 2 == 1:
                                yield ("stage1_done" if g == 15 else "stage1")
                        TOPV = [("topv", g, q) for g in range(16) for q in range(2)]
                        IDXU = [("idxu", g, q) for g in range(16) for q in range(2)]
                        vcopy(idxf[:], idxu[:], IDXU, ["idxf"])
                        tv = topv[:].rearrange("p (h q) a -> p h q a", q=2)
                        idf = idxf[:].rearrange("p (h q) a -> p h q a", q=2)
                        tt(cand4, tv[:, :, 0, :].unsqueeze(3).to_broadcast([128, 8, 16, 16]),
                           tv[:, :, 1, :].unsqueeze(2).to_broadcast([128, 8, 16, 16]), ALU.add, TOPV, ["cand"] + SK)
                        yield "x"
                        for h in range(8):
                            ch = buf1[:, h * 256:(h + 1) * 256]
                            z = s2g[h % 2][:, 0:256]
                            zk = ("s2g", h % 2)
                            S.add("dve", lambda e, h=h, ch=ch: e.max(out=best[:, h, 0:8], in_=ch), ["cand"], [("best", h, 0)])
                            S.add("dve", lambda e, h=h, ch=ch: e.max_index(out=posu[:, h, 0:8], in_max=best[:, h, 0:8], in_values=ch),
                                  ["cand", ("best", h, 0)], [("posu", h, 0)])
                            S.add("dve", lambda e, h=h, ch=ch, z=z: e.match_replace(out=z, in_to_replace=best[:, h, 0:8], in_values=ch, imm_value=-1e30),
                                  ["cand", ("best", h, 0)], [zk])
                            S.add("dve", lambda e, h=h, z=z: e.max(out=best[:, h, 8:16], in_=z), [zk], [("best", h, 1)])
                            S.add("dve", lambda e, h=h, z=z: e.max_index(out=posu[:, h, 8:16], in_max=best[:, h, 8:16], in_values=z),
                                  [zk, ("best", h, 1)], [("posu", h, 1)])
                            if h % 2 == 1:
                                yield ("stage2_done" if h == 7 else "stage2")
                        BEST = [("best", h, q) for h in range(8) for q in range(2)]
                        POSU = [("posu", h, q) for h in range(8) for q in range(2)]
                        posf = posu[:].rearrange("p h k -> p (h k)")
                        ts(abu[:, 0, :], posf, 4, ALU.arith_shift_right, POSU, ["abu0"])
                        ts(abu[:, 1, :], posf, 15, ALU.bitwise_and, POSU, ["abu1"])
                        vcopy(abf[:], abu[:], ["abu0", "abu1"], ["abf"])
                        for q in range(2):
                            for hf in range(2):
                                hs = slice(hf * 4, hf * 4 + 4)
                                a_b = abf[:, q, :].rearrange("p (h k) -> p h k", h=8)[:, hs, :].unsqueeze(3).to_broadcast([128, 4, 16, 16])
                                tt(eq4, a_b, io4v, ALU.is_equal, ["abf", "io4"], ["eqt"])
                                tt(eq4, eq4, idf[:, hs, q, :].unsqueeze(2).to_broadcast([128, 4, 16, 16]), ALU.mult, ["eqt", "idxf"], ["eqt"])
                                S.add("dve", lambda e, q=q, hs=hs: e.tensor_reduce(
                                    out=ijg[:, q, :].rearrange("p (h k) -> p h k", h=8)[:, hs, :], in_=eq4,
                                    axis=AX.X, op=ALU.add), ["eqt"], [("ijg", q)])
                            yield "x"
                        g3 = ijg[:, 2, :].rearrange("p (h k) -> p h k", h=8)
                        tt(g3, best[:], best[:, :, 0:1].to_broadcast([128, 8, 16]), ALU.subtract, BEST, [("ijg", 2)])
                        act(g3, g3, AF.Exp, [("ijg", 2)], [("ijg", 2)])
                        S.add("dve", lambda e, g3=g3: e.tensor_reduce(out=gsm[:, 0:8], in_=g3, axis=AX.X, op=ALU.add), [("ijg", 2)], ["gsm"])
                        recip(gsm[:, 8:16], gsm[:, 0:8], ["gsm"], ["grc"])
                        tt(g3, g3, gsm[:, 8:16].unsqueeze(2).to_broadcast([128, 8, 16]), ALU.mult, [("ijg", 2), "grc"], [("ijg", 2)])
                        for _e in range(DBG.get("eyield", 4)):
                            yield "decode"
                        yield "decode_done"
                        q_ = nextpp()
                        for q in range(3):
                            tr(pp[q_][:, q * 128:(q + 1) * 128], ijg[:, q, :], identf[:], [("ijg", q), "identf"], [("pp", q_)])
                        vcopy(ijgT[:], pp[q_][:, 0:384].rearrange("p (q t) -> p q t", q=3), [("pp", q_)], ["ijgT"])
                        yield "x"
                        for t in range(128):
                            u4 = t % 4
                            u8 = t % 4
                            if u4 == 0:
                                q_ = nextpp()
                            S.add("dve", lambda e, t=t, u8=u8, sub=sub: e.tensor_scalar(
                                out=At[u8][:], in0=iota[:], scalar1=ijgT[:, 0, t:t + 1], scalar2=ijgT[:, 2, t:t + 1],
                                op0=ALU.is_equal, op1=ALU.mult), ["ijgT", "iota"], [("At", u8)])
                            S.add("dve", lambda e, t=t, u8=u8, sub=sub: e.tensor_scalar(
                                out=Bt[u8][:], in0=iota[:], scalar1=ijgT[:, 1, t:t + 1], scalar2=None,
                                op0=ALU.is_equal), ["ijgT", "iota"], [("Bt", u8)])
                            mm(pp[q_][:, u4 * 128:(u4 + 1) * 128], Bt[u8][:], At[u8][:], True, True,
                               [("At", u8), ("Bt", u8)], [("pp", q_)])
                            if u4 == 3:
                                t0 = sub * 128 + t - 3
                                act(G[gb][:, t0:t0 + 4, :], pp[q_][:].rearrange("p (t i) -> p t i", i=128), AF.Copy,
                                    [("pp", q_)], [("G", gb)])
                                yield "x"

                    def drive(g, until):
                        for m in g:
                            yield
                            if m == until:
                                return

                    def inter(a, b):
                        da = db = False
                        while not (da and db):
                            if not da:
                                try:
                                    next(a)
                                except StopIteration:
                                    da = True
                            if not db:
                                try:
                                    next(b)
                                except StopIteration:
                                    db = True
                            yield

                    g0 = sub_gen(0)
                    g1 = sub_gen(1)
                    yield from drive(g0, "scores_done")
                    yield from inter(drive(g0, "stage1_done"), drive(g1, "proj_done"))
                    yield from drive(g0, "stage2_done")
                    yield from inter(drive(g0, "decode_done"), drive(g1, "scores_done"))
                    yield from drive(g0, None)
                    yield from drive(g1, None)

                def run(gen, n):
                    if gen is None:
                        return
                    for _ in range(n):
                        try:
                            next(gen)
                        except StopIteration:
                            return

                run(prologue(0), 10 ** 6)
                for blk in range(NBLK):
                    gb = blk % 2
                    XN = [("xnT", gb, 0), ("xnT", gb, 1)]
                    gen = prologue(blk + 1) if blk + 1 < NBLK else None
                    def emit_u(i):
                        sl = i % NSL
                        pg = i % 2
                        pah = pa2[pg][:, 0:256]
                        for k in range(8):
                            mm(pah, ut8[sl][:, k, :], xnT[gb][:, k, :], k == 0, k == 7,
                               XN + [("ut8", sl)], [("pa", pg)])

                    def emit_mid(i):
                        pg = i % 2
                        pah = pa2[pg][:, 0:256]
                        act(ga[pg][:], pah, AF.Gelu, [("pa", pg)], [("ga", pg)])
                        tt(gw[pg][:], ga[pg][:], G[gb][:, :, i], ALU.mult, [("ga", pg), ("G", gb)], [("gw", pg)], eng=DBG.get("gweng", "pool"))

                    def emit_v(i):
                        sl = i % NSL
                        pg = i % 2
                        for sub in range(2):
                            for dh in range(2):
                                mm(big[sub * 2 + dh][:], gw[pg][:, sub * 128:(sub + 1) * 128], v8[sl][:, dh * 512:(dh + 1) * 512],
                                   i == 0, i == 127, [("gw", pg), ("v8", sl)], [("big", sub * 2 + dh)])

                    def emit_load(i):
                        sl = i % NSL
                        dma(ut8[sl][:], UT2[i], (), [("ut8", sl)], "l_ut%d" % sl)
                        dma(v8[sl][:], Vb[i * 128:(i + 1) * 128, :], (), [("v8", sl)], "l_v8%d" % sl)

                    for i in range(NSL - 2):
                        emit_load(i)
                    emit_u(0)
                    for i in range(128):
                        if i + NSL - 2 < 128:
                            emit_load(i + NSL - 2)
                        if i + 1 < 128:
                            emit_u(i + 1)
                        if i == DBG.get("reload_at", 122):
                            xe_ = buf1[:].rearrange("p (a d) -> p a d", d=D)
                            for sub_ in range(2):
                                tb_ = blk * 2 + sub_
                                dma(xe_[:, sub_, :], X2s[tb_ * 128:(tb_ + 1) * 128, :], (), [("xe", sub_)] + SK + ["cand"], "l_xe%d" % sub_)
                        emit_mid(i)
                        if i >= 1:
                            emit_v(i - 1)
                        run(gen, 1)
                    emit_v(127)
                    xe = buf1[:].rearrange("p (a d) -> p a d", d=D)
                    for sub in range(2):
                        tb = blk * 2 + sub
                        for dh in range(2):
                            tt(xe[:, sub, dh * 512:(dh + 1) * 512], big[sub * 2 + dh][:], xe[:, sub, dh * 512:(dh + 1) * 512], ALU.add,
                               [("big", sub * 2 + dh), ("xe", sub)], [("xe", sub)])
                        ttr(junk[:], xe[:, sub, :], xe[:, sub, :], st3[:, 4:5], [("xe", sub)], ["ss4", "junk"])
                        act(st3[:, 5:6], st3[:, 4:5], AF.Sqrt, ["ss4", "eps_t"], ["rs4"], scale=1.0 / D, bias=eps_t[:])
                        recip(st3[:, 6:7], st3[:, 5:6], ["rs4"], ["rstd4"])
                        stt(xe[:, sub, :], xe[:, sub, :], st3[:, 6:7], gfin[:], ALU.mult, ALU.mult, [("xe", sub), "rstd4", "gfin"], [("xe", sub)])
                        dma(out_d[tb * 128:(tb + 1) * 128, :], xe[:, sub, :], [("xe", sub)], ["out"] + SK + ["cand"], "s_out%d" % sub)
                    run(gen, 10 ** 6)

        with ExitStack() as p0:
            w_bf = sbt(p0, "w_bf", [128, 8, 3072], BF16)
            wst = [sbt(p0, "wst%d" % i, [128, 1024], F32) for i in range(2)]
            gattn = sbt(p0, "gattn", [128, 8], F32)
            ropd = sbt(p0, "ropd", [128, 2, NTB, 8], F32)
            ropr = sbt(p0, "ropr", [128, 2, NTB, 32], F32)
            xt = [sbt(p0, "xt%d" % i, [128, D], F32) for i in range(2)]
            hb = [sbt(p0, "hb%d" % i, [128, D], BF16) for i in range(2)]
            hT = [sbt(p0, "hT%d" % i, [128, 8, 128], BF16) for i in range(2)]
            qk32 = sbt(p0, "qk32", [128, 1536], F32)
            qkb = sbt(p0, "qkb", [128, 1536], BF16)
            rt = [sbt(p0, "rt%d" % i, [128, 256], F32) for i in range(4)]
            qkTst = sbt(p0, "qkTst", [128, 12, 512], BF16)
            vst = [sbt(p0, "vst%d" % i, [128, 512], BF16) for i in range(2)]
            rvst = [sbt(p0, "rvst%d" % i, [128, 512], BF16) for i in range(2)]
            rgst = [sbt(p0, "rgst%d" % i, [128, 512], BF16) for i in range(2)]
            st0 = sbt(p0, "st0", [128, 8], F32)
            pT = [pst(p0, "pT%d" % i, [128, 8, 128], BF16) for i in range(2)]
            pp = [pst(p0, "pp%d" % i, [128, 512], F32) for i in range(2)]
            pQ1 = pst(p0, "pQ1", [128, 8, 128], BF16)
            pQ2 = pst(p0, "pQ2", [128, 4, 128], BF16)

            dma(gattn[:], gattn_d, (), ["gattn"], "c_g")
            dma(ropd[:], ropd_d, (), ["ropd"], "c_rd")
            dma(ropr[:], ropr_d, (), ["ropr"], "c_rr")
            j = 0
            for k in range(8):
                for c in range(3):
                    s = j % 2
                    dma(wst[s][:], w_in_d[k * 128:(k + 1) * 128, c * 1024:(c + 1) * 1024], (), [("wst", s)], "c_w%d" % s)
                    if j % 2 == 0:
                        act(w_bf[:, k, c * 1024:(c + 1) * 1024], wst[s][:], AF.Copy, [("wst", s), "gattn"], [("w_bf", k, c)],
                            scale=gattn[:, k:k + 1])
                    else:
                        ts(w_bf[:, k, c * 1024:(c + 1) * 1024], wst[s][:], gattn[:, k:k + 1], ALU.mult,
                           [("wst", s), "gattn"], [("w_bf", k, c)])
                    j += 1
            WALL = [("w_bf", k, c) for k in range(8) for c in range(3)]

            for tb in range(NTB):
                s = tb % 2
                dma(xt[s][:], x_d[tb * 128:(tb + 1) * 128, :], (), [("xt", s)], "c_x%d" % s)
                ss = st0[:, s:s + 1]
                rs = st0[:, 2 + s:3 + s]
                rstd = st0[:, 4 + s:5 + s]
                ttr(junk[:], xt[s][:], xt[s][:], ss, [("xt", s)], [("ss", s), "junk"])
                act(rs, ss, AF.Sqrt, [("ss", s), "eps_t"], [("rs", s)], scale=1.0 / D, bias=eps_t[:])
                recip(rstd, rs, [("rs", s)], [("rstd", s)])
                act(hb[s][:], xt[s][:], AF.Copy, [("xt", s), ("rstd", s)], [("hb", s)], scale=rstd)
                for k in range(8):
                    tr(pT[s][:, k, :], hb[s][:, k * 128:(k + 1) * 128], identb[:], [("hb", s), "identb"], [("pT", s)])
                vcopy(hT[s][:], pT[s][:], [("pT", s)], [("hT", s)])
                for cg in range(6):
                    ps = pp[cg % 2]
                    pk = ("pp", cg % 2)
                    for k in range(8):
                        mm(ps[:], hT[s][:, k, :], w_bf[:, k, cg * 512:(cg + 1) * 512], k == 0, k == 7,
                           [("hT", s)] + WALL, [pk])
                    if cg == 0:
                        act(qk32[:, 0:512], ps[:], AF.Copy, [pk], [("qk32", 0)])
                    elif cg == 1:
                        vcopy(qk32[:, 512:1024], ps[:], [pk], [("qk32", 1)])
                    elif cg == 2:
                        act(vst[s][:], ps[:], AF.Copy, [pk], [("vst", s)])
                    elif cg == 3:
                        vcopy(qk32[:, 1024:1536], ps[:], [pk], [("qk32", 2)])
                    elif cg == 4:
                        vcopy(rvst[s][:], ps[:], [pk], [("rvst", s)])
                    else:
                        act(rgst[s][:], ps[:], AF.Silu, [pk], [("rgst", s)])
                QK32 = [("qk32", i) for i in range(3)]
                v_d = qk32[:, 0:1024].rearrange("p (g d) -> p g d", d=64)
                o_d = qkb[:, 0:1024].rearrange("p (g d) -> p g d", d=64)
                cd = ropd[:, 0, tb:tb + 1, :].to_broadcast([128, 16, 8])
                sd = ropd[:, 1, tb:tb + 1, :].to_broadcast([128, 16, 8])
                t = [rt[i][:, 0:128].rearrange("p (g d) -> p g d", d=8) for i in range(4)]
                tt(t[0], v_d[:, :, 0:8], cd, ALU.mult, QK32 + ["ropd"], [("rt", 0)])
                tt(t[1], v_d[:, :, 8:16], sd, ALU.mult, QK32 + ["ropd"], [("rt", 1)])
                tt(o_d[:, :, 0:8], t[0], t[1], ALU.subtract, [("rt", 0), ("rt", 1)], [("qkb", 0)])
                tt(t[2], v_d[:, :, 8:16], cd, ALU.mult, QK32 + ["ropd"], [("rt", 2)])
                tt(t[3], v_d[:, :, 0:8], sd, ALU.mult, QK32 + ["ropd"], [("rt", 3)])
                tt(o_d[:, :, 8:16], t[2], t[3], ALU.add, [("rt", 2), ("rt", 3)], [("qkb", 1)])
                act(o_d[:, :, 16:64], v_d[:, :, 16:64], AF.Copy, QK32, [("qkb", 2)])
                v_r = qk32[:, 1024:1536].rearrange("p (g d) -> p g d", d=64)
                o_r = qkb[:, 1024:1536].rearrange("p (g d) -> p g d", d=64)
                cr = ropr[:, 0, tb:tb + 1, :].to_broadcast([128, 8, 32])
                sr = ropr[:, 1, tb:tb + 1, :].to_broadcast([128, 8, 32])
                u = [rt[i][:, 0:256].rearrange("p (g d) -> p g d", d=32) for i in range(4)]
                tt(u[0], v_r[:, :, 0:32], cr, ALU.mult, QK32 + ["ropr"], [("rt", 0)])
                tt(u[1], v_r[:, :, 32:64], sr, ALU.mult, QK32 + ["ropr"], [("rt", 1)])
                tt(o_r[:, :, 0:32], u[0], u[1], ALU.subtract, [("rt", 0), ("rt", 1)], [("qkb", 3)])
                tt(u[2], v_r[:, :, 32:64], cr, ALU.mult, QK32 + ["ropr"], [("rt", 2)])
                tt(u[3], v_r[:, :, 0:32], sr, ALU.mult, QK32 + ["ropr"], [("rt", 3)])
                tt(o_r[:, :, 32:64], u[2], u[3], ALU.add, [("rt", 2), ("rt", 3)], [("qkb", 4)])
                QKB = [("qkb", i) for i in range(5)]
                for c in range(8):
                    tr(pQ1[:, c, :], qkb[:, c * 128:(c + 1) * 128], identb[:], QKB + ["identb"], ["pQ1"])
                for c in range(4):
                    tr(pQ2[:, c, :], qkb[:, (8 + c) * 128:(9 + c) * 128], identb[:], QKB + ["identb"], ["pQ2"])
                q4 = tb % 4
                act(qkTst[:, 0:8, q4 * 128:(q4 + 1) * 128], pQ1[:], AF.Copy, ["pQ1"], [("qkTst", q4, 0)])
                vcopy(qkTst[:, 8:12, q4 * 128:(q4 + 1) * 128], pQ2[:], ["pQ2"], [("qkTst", q4, 1)])
                dma(Vs[tb * 128:(tb + 1) * 128, :], vst[s][:], [("vst", s)], ["Vs"], "s_v%d" % s)
                dma(RVs[tb * 128:(tb + 1) * 128, :], rvst[s][:], [("rvst", s)], ["RVs"], "s_rv%d" % s)
                dma(RGs[tb * 128:(tb + 1) * 128, :], rgst[s][:], [("rgst", s)], ["RGs"], "s_rg%d" % s)
                if q4 == 3:
                    t4 = tb // 4
                    for c in range(12):
                        dma(QKT[c, :, t4 * 512:(t4 + 1) * 512], qkTst[:, c, :],
                            [("qkTst", a, b) for a in range(4) for b in range(2)], ["QKT"], "s_qkt")
        S.barrier()

        with ExitStack() as p1:
          if stage != "P0":
              qA = [sbt(p1, "qA%d" % i, [128, S_TOK], BF16) for i in range(2)]
              qB = [sbt(p1, "qB%d" % i, [128, S_TOK], BF16) for i in range(2)]
              kT = [sbt(p1, "kT%d" % i, [128, S_TOK], BF16) for i in range(2)]
              vv = [sbt(p1, "vv%d" % i, [128, NTB, 129], BF16) for i in range(2)]
              rgh = [sbt(p1, "rgh%d" % i, [128, NTB, 128], BF16) for i in range(2)]
              et = [sbt(p1, "et%d" % i, [128, 512], BF16) for i in range(3)]
              dlam = sbt(p1, "dlam", [128, 256], F32)
              rld = sbt(p1, "rld", [128, 8], F32)
              lg = sbt(p1, "lg", [128, 8], F32)
              rtab = sbt(p1, "rtab", [128, 10, 256], F32)
              e128 = sbt(p1, "e128", [128, 32], F32)
              Wm = sbt(p1, "Wm", [128, 4, 256], F32)
              wtmp = sbt(p1, "wtmp", [128, 2, 256], F32)
              pw = sbt(p1, "pw", [128, 2, 32], F32)
              oc = sbt(p1, "oc", [128, 2, 129], F32)
              rtmp = [sbt(p1, "rtmp%d" % i, [128, 256], F32) for i in range(2)]
              av = sbt(p1, "av", [128, 2, 128], F32)
              st1 = sbt(p1, "st1", [128, 32], F32)
              sT = [pst(p1, "sT%d" % i, [128, 512], F32) for i in range(4)]
              oacc = [pst(p1, "oacc%d" % i, [128, 512], F32) for i in range(4)]

              conv_eng = ["dve"]
              cgen = None
              if stage == "full":
                  cf1 = [sbt(p1, "cf1_%d" % i, [128, 2048], F32) for i in range(2)]
                  cb1 = [sbt(p1, "cb1_%d" % i, [128, 2048], BF16) for i in range(2)]
                  gffn1 = sbt(p1, "gffn1", [128, 8], F32)
                  dma(gffn1[:], gffn_d, (), ["gffn1"], "c_gf1")

                  def conv_gen():
                      j = 0
                      for k in range(8):
                          for cg in range(8):
                              s_ = j % 2
                              dma(cf1[s_][:], uT_d[k * 128:(k + 1) * 128, cg * 2048:(cg + 1) * 2048], (), [("cf1", s_)], "c_cf1%d" % s_)
                              if conv_eng[0] == "act":
                                  act(cb1[s_][:], cf1[s_][:], AF.Copy, [("cf1", s_), "gffn1"], [("cb1", s_)], scale=gffn1[:, k:k + 1])
                              else:
                                  ts(cb1[s_][:], cf1[s_][:], gffn1[:, k:k + 1], ALU.mult, [("cf1", s_), "gffn1"], [("cb1", s_)])
                              for hf in range(2):
                                  dma(UT2[cg * 16 + hf * 8:cg * 16 + (hf + 1) * 8, :, k, :].rearrange("c p e -> p c e"),
                                      cb1[s_][:, hf * 1024:(hf + 1) * 1024].rearrange("p (c e) -> p c e", e=128),
                                      [("cb1", s_)], ["UT2"], "s_cb1%d" % s_)
                              j += 1
                              yield
                      for r in range(64):
                          s_ = j % 2
                          dma(cf1[s_][:].rearrange("p (a d) -> p a d", d=D),
                              pv_d[r * 256:(r + 1) * 256, :].rearrange("(a p) d -> p a d", p=128), (), [("cf1", s_)], "c_cf1%d" % s_)
                          if conv_eng[0] == "act":
                              act(cb1[s_][:], cf1[s_][:], AF.Copy, [("cf1", s_)], [("cb1", s_)])
                          else:
                              vcopy(cb1[s_][:], cf1[s_][:], [("cf1", s_)], [("cb1", s_)])
                          dma(Vb[r * 256:(r + 1) * 256, :].rearrange("(a p) d -> p a d", p=128),
                              cb1[s_][:].rearrange("p (a d) -> p a d", d=D), [("cb1", s_)], ["Vb"], "s_cb1%d" % s_)
                          j += 1
                          yield

                  cgen = conv_gen()
              dma(dlam[:], dlam_d, (), ["dlam"], "c_dl")
              dma(rld[:], rld_d, (), ["rld"], "c_rl")
              dma(rtab[:], rtab_d, (), ["rtab"], "c_rt")
              dma(e128[:], e128_d, (), ["e128"], "c_e1")
              for i in range(2):
                  memset(vv[i][:, :, 128:129], 1.0, [("vv", i)])
                  memset(qA[i][64:128, :], 0.0, [("qT", i)], eng="pool")
                  memset(qB[i][0:64, :], 0.0, [("qT", i)], eng="pool")
              ttr(junk[:, 0:64], dlam[:, 0:64], dlam[:, 64:128], st1[:, 0:1], ["dlam"], ["l1", "junk"])
              ttr(junk[:, 0:64], dlam[:, 128:192], dlam[:, 192:256], st1[:, 1:2], ["dlam"], ["l2", "junk"])
              act(st1[:, 2:4], st1[:, 0:2], AF.Exp, ["l1", "l2"], ["l12e"])
              tt(st1[:, 4:5], st1[:, 3:4], st1[:, 2:3], ALU.subtract, ["l12e"], ["nl0"])
              ts(st1[:, 5:6], st1[:, 4:5], -LAMBDA_INIT, ALU.add, ["nl0"], ["neglam"])
              neglam = st1[:, 5:6]
              act(lg[:], rld[:], AF.Exp, ["rld"], ["lg0"])
              ts(lg[:], lg[:], -1.0, ALU.mult, ["lg0"], ["lg"])

              step = 0
              def emit_head_loads(hh):
                  is_diff = hh < 4
                  h = hh % 4
                  s = hh % 2
                  if is_diff:
                      dma(qA[s][0:64, :], QKT[h, 0:64, :], (), [("qT", s)], "l_q%d" % s)
                      dma(qB[s][64:128, :], QKT[h, 64:128, :], (), [("qT", s)], "l_q%d" % s)
                      dma(kT[s][:], QKT[4 + h], (), [("kT", s)], "l_k%d" % s)
                      for t8 in range(8):
                          dma(vv[s][:, t8 * 4:(t8 + 1) * 4, 0:128],
                              Vs.rearrange("(t p) c -> p t c", p=128)[:, t8 * 4:(t8 + 1) * 4, h * 128:(h + 1) * 128],
                              (), [("vv", s)], "l_v%d" % s)
                  else:
                      if h % 2 == 0:
                          dma(qA[s][0:64, :], QKT[8 + h // 2, 0:64, :], (), [("qT", s)], "l_q%d" % s)
                      else:
                          dma(qB[s][64:128, :], QKT[8 + h // 2, 64:128, :], (), [("qT", s)], "l_q%d" % s)
                      dma(kT[s][:], QKT[10 + h // 2], (), [("kT", s)], "l_k%d" % s)
                      for t8 in range(8):
                          dma(vv[s][:, t8 * 4:(t8 + 1) * 4, 0:128],
                              RVs.rearrange("(t p) c -> p t c", p=128)[:, t8 * 4:(t8 + 1) * 4, h * 128:(h + 1) * 128],
                              (), [("vv", s)], "l_v%d" % s)
                          dma(rgh[s][:, t8 * 4:(t8 + 1) * 4, :],
                              RGs.rearrange("(t p) c -> p t c", p=128)[:, t8 * 4:(t8 + 1) * 4, h * 128:(h + 1) * 128],
                              (), [("rgh", s)], "l_g%d" % s)

              HEADS = DBG["heads"]
              if HEADS:
                  emit_head_loads(HEADS[0])
              for hidx, hh in enumerate(HEADS):
                  is_diff = hh < 4
                  h = hh % 4
                  s = hh % 2
                  OPS = [("qT", s), ("kT", s), ("vv", s)]
                  if hidx + 1 < len(HEADS):
                      emit_head_loads(HEADS[hidx + 1])
                  if not is_diff:
                      pb = (h % 2) * 64
                      lgf = lg[:, h:h + 1]
                      lgb = lg[:, 4 + h:5 + h]
                      act(Wm[:, 0, :], rtab[:, 0, :], AF.Exp, ["rtab", "lg", "ln8_t"], [("Wm", 0)], scale=lgf, bias=ln8_t[:])
                      act(Wm[:, 1, :], rtab[:, 1, :], AF.Exp, ["rtab", "lg", "ln8_t"], [("Wm", 1)], scale=lgb, bias=ln8_t[:])
                      for r_ in range(2):
                          act(wtmp[:, 0, :], rtab[:, 2 + r_, :], AF.Exp, ["rtab", "lg", "ln8_t"], [("wtmp", 0)], scale=lgf, bias=ln8_t[:])
                          act(wtmp[:, 1, :], rtab[:, 4 + r_, :], AF.Exp, ["rtab", "lg", "ln8_t"], [("wtmp", 1)], scale=lgb, bias=ln8_t[:])
                          tt(wtmp[:, 0, :], wtmp[:, 0, :], rtab[:, 6 + r_, :], ALU.mult, [("wtmp", 0), "rtab"], [("wtmp", 0)])
                          tt(wtmp[:, 1, :], wtmp[:, 1, :], rtab[:, 8 + r_, :], ALU.mult, [("wtmp", 1), "rtab"], [("wtmp", 1)])
                          tt(Wm[:, 2 + r_, :], wtmp[:, 0, :], wtmp[:, 1, :], ALU.add, [("wtmp", 0), ("wtmp", 1)], [("Wm", 2 + r_)])
                      act(pw[:, 0, :], e128[:], AF.Exp, ["e128", "lg"], [("pw", 0)], scale=lgf)
                      act(pw[:, 1, :], e128[:], AF.Exp, ["e128", "lg"], [("pw", 1)], scale=lgb)
                  def emit_post(qb):
                      for qs in (range(2) if DBG["post"] else []):
                          tb = qb * 2 + qs
                          if is_diff:
                              vcopy(oc[:, 0, :], oacc[qs][:, 0:129], [("oacc", qs)], [("oc", 0)])
                              act(oc[:, 1, :], oacc[2 + qs][:, 0:129], AF.Copy, [("oacc", 2 + qs)], [("oc", 1)])
                              recip(st1[:, 8:10], oc[:, :, 128], [("oc", 0), ("oc", 1)], ["rs2"])
                              tt(st1[:, 10:11], st1[:, 9:10], neglam, ALU.mult, ["rs2", "neglam"], ["nl"])
                              ts(av[:, 0, :], oc[:, 0, 0:128], st1[:, 8:9], ALU.mult, [("oc", 0), "rs2"], [("av", 0)])
                              stt(av[:, 1, :], oc[:, 1, 0:128], st1[:, 10:11], av[:, 0, :], ALU.mult, ALU.add,
                                  [("oc", 1), "nl", ("av", 0)], [("av", 1)])
                              ttr(junk[:, 0:128], av[:, 1, :], av[:, 1, :], st1[:, 11:12], [("av", 1)], ["ssq", "junk"])
                              act(st1[:, 12:13], st1[:, 11:12], AF.Ln, ["ssq", "eps_t"], ["lnv"], scale=1.0 / 128, bias=eps_t[:])
                              act(st1[:, 13:14], st1[:, 12:13], AF.Exp, ["lnv"], ["rstd1"], scale=-0.5)
                              ts(mix[:, tb, h * 128:(h + 1) * 128], av[:, 1, :], st1[:, 13:14], ALU.mult,
                                 [("av", 1), "rstd1"], [("mix", tb, hh)])
                          else:
                              vcopy(oc[:, 0, 0:128], oacc[qs][:, 0:128], [("oacc", qs)], [("oc", 0)])
                              S.add("dve", lambda e: e.tensor_reduce(out=st1[:, 16:17], in_=oc[:, 0, 0:128], axis=AX.X, op=ALU.add),
                                    [("oc", 0)], ["rsum"])
                              ts(st1[:, 17:18], st1[:, 16:17], -1.0 / 128, ALU.mult, ["rsum"], ["nmean"])
                              ts(av[:, 0, :], oc[:, 0, 0:128], st1[:, 17:18], ALU.add, [("oc", 0), "nmean"], [("av", 0)])
                              ttr(junk[:, 0:128], av[:, 0, :], av[:, 0, :], st1[:, 18:19], [("av", 0)], ["ssq", "junk"])
                              act(st1[:, 19:20], st1[:, 18:19], AF.Ln, ["ssq", "eps_t"], ["lnv"], scale=1.0 / 128, bias=eps_t[:])
                              act(st1[:, 20:21], st1[:, 19:20], AF.Exp, ["lnv"], ["rstd1"], scale=-0.5)
                              stt(mix[:, tb, 512 + h * 128:512 + (h + 1) * 128], av[:, 0, :], st1[:, 20:21], rgh[s][:, tb, :],
                                  ALU.mult, ALU.mult, [("av", 0), "rstd1", ("rgh", s)], [("mix", tb, hh)])
                  def emit_qk(qb, kc, si):
                      qsl = slice(qb * 256, (qb + 1) * 256)
                      ksl = slice(kc * 128, (kc + 1) * 128)
                      if is_diff:
                          mm(sT[si][:, 0:256], kT[s][:, ksl], qA[s][:, qsl], True, True, OPS, [("sT", si)])
                          mm(sT[si][:, 256:512], kT[s][:, ksl], qB[s][:, qsl], True, True, OPS, [("sT", si)])
                      else:
                          mm(sT[si][:, 0:256], kT[s][:, ksl], (qA if h % 2 == 0 else qB)[s][:, qsl], True, True, OPS, [("sT", si)])
                  def emit_mid_av(qb, kc, si, ei):
                      if is_diff:
                          act(et[ei][:], sT[si][:], AF.Exp, [("sT", si)], [("et", ei)], scale=0.125)
                          for m in range(2):
                              for qs in range(2):
                                  a = m * 2 + qs
                                  mm(oacc[a][:, 0:129], et[ei][:, m * 256 + qs * 128:m * 256 + (qs + 1) * 128], vv[s][:, kc, :],
                                     kc == 0, kc == NTB - 1, [("et", ei)] + OPS, [("oacc", a)])
                      else:
                          nn = 2 * qb - kc
                          use_pool = (kc % 3 == 2) and DBG.get("retpool", True)
                          if (nn >= 1 or nn <= -2) and use_pool:
                              d_ = 0 if nn >= 1 else 1
                              pcol = pw[:, 0, nn:nn + 1] if nn >= 1 else pw[:, 1, -nn - 1:-nn]
                              tp_ = rtmp[kc % 2]
                              act(tp_[:], sT[si][:, 0:256], AF.Copy, [("sT", si), ("pw", d_)], [("rtmp", kc % 2)], scale=pcol)
                              tt(et[ei][:, 0:256], tp_[:], Wm[:, d_, :], ALU.mult, [("rtmp", kc % 2), ("Wm", d_)], [("et", ei)], eng="pool")
                          elif nn >= 1:
                              stt(et[ei][:, 0:256], sT[si][:, 0:256], pw[:, 0, nn:nn + 1], Wm[:, 0, :], ALU.mult, ALU.mult,
                                  [("sT", si), ("pw", 0), ("Wm", 0)], [("et", ei)])
                          elif nn <= -2:
                              stt(et[ei][:, 0:256], sT[si][:, 0:256], pw[:, 1, -nn - 1:-nn], Wm[:, 1, :], ALU.mult, ALU.mult,
                                  [("sT", si), ("pw", 1), ("Wm", 1)], [("et", ei)])
                          else:
                              r_ = kc - 2 * qb
                              tt(et[ei][:, 0:256], sT[si][:, 0:256], Wm[:, 2 + r_, :], ALU.mult,
                                 [("sT", si), ("Wm", 2 + r_)], [("et", ei)])
                          for qs in range(2):
                              mm(oacc[qs][:, 0:128], et[ei][:, qs * 128:(qs + 1) * 128], vv[s][:, kc, 0:128],
                                 kc == 0, kc == NTB - 1, [("et", ei)] + OPS, [("oacc", qs)])
                  steps = [(qb_, kc_) for qb_ in range(DBG["nqb"]) for kc_ in range(NTB)]
                  LA = 2
                  for j_ in range(min(LA, len(steps))):
                      emit_qk(steps[j_][0], steps[j_][1], (step + j_) % 4)
                  for n_, (qb_, kc_) in enumerate(steps):
                      if n_ + LA < len(steps):
                          emit_qk(steps[n_ + LA][0], steps[n_ + LA][1], (step + n_ + LA) % 4)
                      emit_mid_av(qb_, kc_, (step + n_) % 4, (step + n_) % 3)
                      if kc_ == NTB - 1:
                          emit_post(qb_)
                          conv_eng[0] = "dve" if is_diff else "act"
                          run_gen(cgen, 1)
                  if hh == DBG["heads"][-1]:
                      run_gen(cgen, 10 ** 6)
                  step += len(steps)
        S.barrier()

        with ExitStack() as p2:
            wo_bf = sbt(p2, "wo_bf", [128, 8, D], BF16)
            wst2 = [sbt(p2, "wst2_%d" % i, [128, D], F32) for i in range(2)]
            gmix = sbt(p2, "gmix", [128, 8], F32)
            mixT = sbt(p2, "mixT", [128, 8, 128], BF16)
            xt2 = [sbt(p2, "xt2_%d" % i, [128, D], F32) for i in range(2)]
            x2 = [sbt(p2, "x2_%d" % i, [128, D], F32) for i in range(2)]
            pM = pst(p2, "pM", [128, 8, 128], BF16)
            pX = [pst(p2, "pX%d" % i, [128, 512], F32) for i in range(2)]
            dma(gmix[:], gmix_d, (), ["gmix"], "c_gm")
            ts(gmix[:, 0:4], gmix[:, 0:4], 1.0 - LAMBDA_INIT, ALU.mult, ["gmix"], ["gmix"])
            for k in range(8):
                s = k % 2
                dma(wst2[s][:], w_out_d[k * 128:(k + 1) * 128, :], (), [("wst2", s)], "c_wo%d" % s)
                act(wo_bf[:, k, :], wst2[s][:], AF.Copy, [("wst2", s), "gmix"], [("wo_bf", k)], scale=gmix[:, k:k + 1])
            WO = [("wo_bf", k) for k in range(8)]
            for tb in range(NTB if stage in ("A", "full") else 0):
                s = tb % 2
                dma(xt2[s][:], x_d[tb * 128:(tb + 1) * 128, :], (), [("xt2", s)], "c_x2%d" % s)
                MIXK = [("mix", tb, hh) for hh in range(8)]
                for k in range(8):
                    tr(pM[:, k, :], mix[:, tb, k * 128:(k + 1) * 128], identb[:], MIXK + ["identb"], ["pM"])
                vcopy(mixT[:], pM[:], ["pM"], ["mixT"])
                for dh in range(2):
                    for k in range(8):
                        mm(pX[dh][:], mixT[:, k, :], wo_bf[:, k, dh * 512:(dh + 1) * 512], k == 0, k == 7,
                           ["mixT"] + WO, [("pX", dh)])
                    tt(x2[s][:, dh * 512:(dh + 1) * 512], pX[dh][:], xt2[s][:, dh * 512:(dh + 1) * 512], ALU.add,
                       [("pX", dh), ("xt2", s)], [("x2", s, dh)])
                if stage == "A":
                    dma(out_d[tb * 128:(tb + 1) * 128, :], x2[s][:], [("x2", s, 0), ("x2", s, 1)], ["out"], "s_o%d" % s)
                else:
                    dma(X2s[tb * 128:(tb + 1) * 128, :], x2[s][:], [("x2", s, 0), ("x2", s, 1)], ["X2s"], "s_o%d" % s)
        cm.close()
        if stage == "full":
            S.barrier()
            PEER_PHASE()
        if stage not in ("A", "full"):
            dma(out_d[0:128, 0:128], identf[:], ["identf"], ["out"], "s_o0")
        S.emit()
    return nc


def host_inputs(inputs, b, shared=None):
    if shared is None:
        shared = host_shared(inputs)
    f32 = np.float32
    x = np.ascontiguousarray(inputs["x"][b], dtype=f32)
    pos = np.arange(S_TOK, dtype=f32)
    rot_dim = 16
    rope_inv = np.power(f32(500000.0), -np.arange(rot_dim // 2, dtype=f32) * f32(2.0) / f32(rot_dim)).astype(f32)
    ret_inv = (f32(1.0) / np.power(f32(10000.0), np.linspace(0.0, 1.0, 32, dtype=f32))).astype(f32)

    def tab(inv):
        ang = (pos[:, None] * inv[None, :]).astype(f32)
        c = np.cos(ang).astype(f32).reshape(NTB, 128, -1).transpose(1, 0, 2)
        s_ = np.sin(ang).astype(f32).reshape(NTB, 128, -1).transpose(1, 0, 2)
        return np.ascontiguousarray(np.stack([c, s_], axis=1))

    jl = np.arange(128, dtype=f32)[:, None]
    xx = np.arange(256, dtype=f32)[None, :]
    rt = np.zeros((128, 10, 256), f32)
    rt[:, 0] = xx - jl
    rt[:, 1] = jl - xx + 128.0
    for r in range(2):
        dd = xx - 128.0 * r - jl
        rt[:, 2 + r] = np.maximum(dd, 0)
        rt[:, 4 + r] = np.maximum(-dd, 0)
        rt[:, 6 + r] = (dd >= 0).astype(f32)
        rt[:, 8 + r] = (dd < 0).astype(f32)
    gm = np.concatenate([np.tile(inputs["diff_norm_g"][0][:, None], (1, 4)), inputs["ret_norm_g"][0].T], axis=1)
    return {
        "x": x,
        "w_in": np.ascontiguousarray(inputs["w_in"][0], dtype=f32),
        "gattn": np.ascontiguousarray(inputs["attn_norm_g"][0].reshape(8, 128).T, dtype=f32),
        "ropd": tab(rope_inv),
        "ropr": tab(ret_inv),
        "dlam": np.ascontiguousarray(np.tile(inputs["diff_lambda"][0].reshape(1, 256), (128, 1)), dtype=f32),
        "rld": np.ascontiguousarray(np.tile(inputs["ret_log_decay"][0].reshape(1, 8), (128, 1)), dtype=f32),
        "rtab": rt,
        "e128": np.ascontiguousarray(np.tile((128.0 * np.arange(32, dtype=f32))[None, :], (128, 1))),
        "ident": np.eye(128, dtype=f32),
        "w_out": np.ascontiguousarray(inputs["w_out"][0], dtype=f32),
        "gmix": np.ascontiguousarray(gm, dtype=f32),
        "wq": np.ascontiguousarray(inputs["peer_w_query"][0], dtype=f32),
        "gffn": np.ascontiguousarray(inputs["ffn_norm_g"][0].reshape(8, 128).T, dtype=f32),
        "skT": np.ascontiguousarray(inputs["peer_sub_keys"][0].reshape(16, 128, 128).transpose(2, 0, 1), dtype=f32),
        "uT": shared["uT"],
        "pv": shared["pv"],
        "gfin": np.ascontiguousarray(np.tile(inputs["final_norm_g"].reshape(1, D), (128, 1)), dtype=f32),
        "iota": np.ascontiguousarray(np.tile(np.arange(128, dtype=f32)[None, :], (128, 1))),
        "io4": np.ascontiguousarray(np.tile(np.arange(16, dtype=f32)[None, :], (128, 128))),
    }


def host_shared(inputs):
    return {"uT": np.ascontiguousarray(np.asarray(inputs["peer_u"][0], dtype=np.float32).T),
            "pv": np.ascontiguousarray(inputs["peer_v"][0], dtype=np.float32)}


def kernel(**inputs):
    inputs = {k: np.asarray(v) for k, v in inputs.items()}
    nc = build("full")
    shared = host_shared(inputs)
    in_maps = [host_inputs(inputs, b, shared) for b in range(8)]
    res = run_bass_kernel_spmd(nc, in_maps, core_ids=list(range(8)))
    return np.stack([np.asarray(r["out"], dtype=np.float32) for r in res.results], axis=0)
```

```python
from contextlib import ExitStack
import math
import numpy as np
import concourse.bass as bass
import concourse.mybir as mybir
from concourse.bass_utils import run_bass_kernel_spmd

F32 = mybir.dt.float32
BF16 = mybir.dt.bfloat16
U32 = mybir.dt.uint32
AF = mybir.ActivationFunctionType
ALU = mybir.AluOpType
AX = mybir.AxisListType

ENGS = ("pe", "act", "dve", "pool", "sp")
S_TOK = 4096
D = 1024
NTB = S_TOK // 128
EPS = 1e-6
LAMBDA_INIT = 0.8 - 0.6 * math.exp(-0.3 * 0)
LN8 = math.log(0.125)
DBG = {"heads": list(range(8)), "nqb": 16, "post": True, "steps": True}


class Sched:
    def __init__(self, nc):
        self.nc = nc
        self.ops = []
        self.last_w = {}
        self.readers = {}
        self.chan_last = {}

    def add(self, eng, fn, reads=(), writes=(), chan=None):
        i = len(self.ops)
        deps = {}
        for k in reads:
            w = self.last_w.get(k)
            if w is not None:
                deps[w] = True
        for k in writes:
            w = self.last_w.get(k)
            if w is not None:
                deps.setdefault(w, False)
            for r in self.readers.get(k, ()):
                deps.setdefault(r, False)
        if chan is not None:
            p = self.chan_last.get(chan)
            if p is not None:
                deps[p] = True
            self.chan_last[chan] = i
        self.ops.append((eng, fn, deps, chan))
        for k in writes:
            self.last_w[k] = i
            self.readers[k] = []
        for k in reads:
            self.readers.setdefault(k, []).append(i)
        return i

    def barrier(self):
        self.ops.append(("BARRIER", None, {}, None))
        self.last_w.clear()
        self.readers.clear()
        self.chan_last.clear()

    def _skip(self, eng, chan, de, raw):
        return de == eng and chan is None and (eng == "pe" or not raw)

    def emit(self):
        nc = self.nc
        ops = self.ops
        n = len(ops)
        need = [False] * n
        for i, (eng, fn, deps, chan) in enumerate(ops):
            for d, raw in deps.items():
                de, _, _, dchan = ops[d]
                if dchan is not None:
                    continue
                if self._skip(eng, chan, de, raw):
                    continue
                need[d] = True
        cnt = {e: 0 for e in ENGS}
        chan_cnt = {}
        sig = [0] * n
        bar_snap = {}
        for i, (eng, fn, deps, chan) in enumerate(ops):
            if eng == "BARRIER":
                bar_snap[i] = dict(chan_cnt)
                continue
            if chan is not None:
                chan_cnt[chan] = chan_cnt.get(chan, 0) + 16
                sig[i] = chan_cnt[chan]
            elif need[i]:
                cnt[eng] += 1
                sig[i] = cnt[eng]
        with ExitStack() as es:
            engsem = {e: es.enter_context(nc.semaphore("sem_" + e)) for e in ENGS}
            bsem = es.enter_context(nc.semaphore("sem_bar"))
            chansem = {c: es.enter_context(nc.semaphore("dsem_%d" % j))
                       for j, c in enumerate(chan_cnt)}
            block = es.enter_context(nc.Block())

            def make(eng):
                def body(e):
                    waited = {}
                    nbar = 0
                    for i, (oeng, fn, deps, chan) in enumerate(ops):
                        if oeng == "BARRIER":
                            nbar += 1
                            if eng == "sp":
                                for c, v in bar_snap[i].items():
                                    if waited.get(("c", c), 0) < v:
                                        e.wait_ge(chansem[c], v)
                                        waited[("c", c)] = v
                            e.drain().then_inc(bsem, 1)
                            e.wait_ge(bsem, len(ENGS) * nbar)
                            continue
                        if oeng != eng:
                            continue
                        want = {}
                        for d, raw in deps.items():
                            de, _, _, dchan = ops[d]
                            if dchan is not None:
                                key = ("c", dchan)
                                s = chansem[dchan]
                            else:
                                if self._skip(eng, chan, de, raw):
                                    continue
                                key = ("e", de)
                                s = engsem[de]
                            v = sig[d]
                            if waited.get(key, 0) >= v:
                                continue
                            if key not in want or want[key][1] < v:
                                want[key] = (s, v)
                        for key, (s, v) in want.items():
                            e.wait_ge(s, v)
                            waited[key] = v
                        ins = fn(e)
                        if chan is not None:
                            ins.then_inc(chansem[chan], 16)
                        elif need[i]:
                            ins.then_inc(engsem[eng], 1)
                    if eng == "sp":
                        for c, v in chan_cnt.items():
                            if waited.get(("c", c), 0) < v:
                                e.wait_ge(chansem[c], v)
                return body

            block.tensor(make("pe"))
            block.scalar(make("act"))
            block.vector(make("dve"))
            block.gpsimd(make("pool"))
            block.sync(make("sp"))


def build(stage="full"):
    nc = bass.Bass("TRN2", target_bir_lowering=False)
    S = Sched(nc)

    def din(name, shape, dt=F32):
        return nc.dram_tensor(name, list(shape), dt, kind="ExternalInput").ap()

    x_d = din("x", [S_TOK, D])
    w_in_d = din("w_in", [D, 3072])
    gattn_d = din("gattn", [128, 8])
    ropd_d = din("ropd", [128, 2, NTB, 8])
    ropr_d = din("ropr", [128, 2, NTB, 32])
    dlam_d = din("dlam", [128, 256])
    rld_d = din("rld", [128, 8])
    rtab_d = din("rtab", [128, 10, 256])
    e128_d = din("e128", [128, 32])
    ident_d = din("ident", [128, 128])
    w_out_d = din("w_out", [D, D])
    gmix_d = din("gmix", [128, 8])
    wq_d = din("wq", [D, 2048])
    gffn_d = din("gffn", [128, 8])
    skT_d = din("skT", [128, 16, 128])
    uT_d = din("uT", [D, 16384])
    pv_d = din("pv", [16384, D])
    gfin_d = din("gfin", [128, D])
    iota_d = din("iota", [128, 128])
    io4_d = din("io4", [128, 2048])
    out_d = nc.dram_tensor("out", [S_TOK, D], F32, kind="ExternalOutput").ap()

    QKT = nc.dram_tensor("scr_qkt", [12, 128, S_TOK], BF16).ap()
    Vs = nc.dram_tensor("scr_v", [S_TOK, 512], BF16).ap()
    RVs = nc.dram_tensor("scr_rv", [S_TOK, 512], BF16).ap()
    RGs = nc.dram_tensor("scr_rg", [S_TOK, 512], BF16).ap()
    X2s = nc.dram_tensor("scr_x2", [S_TOK, D], F32).ap()
    UT2 = nc.dram_tensor("scr_ut2", [128, 128, 8, 128], BF16).ap()
    WQ2 = nc.dram_tensor("scr_wq2", [16, 128, 8, 128], BF16).ap()
    Vb = nc.dram_tensor("scr_vb", [16384, D], BF16).ap()

    def dma(out, in_, r, w, chan, eng="sp"):
        S.add(eng, lambda e: e.dma_start(out=out, in_=in_), r, w, chan=chan)

    def act(out, in_, func, r, w, scale=None, bias=None, accum=None):
        kw = {}
        if scale is not None:
            kw["scale"] = scale
        if bias is not None:
            kw["bias"] = bias
        if accum is not None:
            kw["accum_out"] = accum
        S.add("act", lambda e: e.activation(out=out, in_=in_, func=func, **kw), r, w)

    def vcopy(out, in_, r, w, eng="dve"):
        S.add(eng, lambda e: e.tensor_copy(out=out, in_=in_), r, w)

    def tt(out, in0, in1, op, r, w, eng="dve"):
        S.add(eng, lambda e: e.tensor_tensor(out=out, in0=in0, in1=in1, op=op), r, w)

    def ts(out, in0, s1, op0, r, w, s2=None, op1=None, eng="dve"):
        if op1 is None:
            S.add(eng, lambda e: e.tensor_scalar(out=out, in0=in0, scalar1=s1, scalar2=None, op0=op0), r, w)
        else:
            S.add(eng, lambda e: e.tensor_scalar(out=out, in0=in0, scalar1=s1, scalar2=s2, op0=op0, op1=op1), r, w)

    def stt(out, in0, scalar, in1, op0, op1, r, w):
        S.add("dve", lambda e: e.scalar_tensor_tensor(out=out, in0=in0, scalar=scalar, in1=in1, op0=op0, op1=op1), r, w)

    def ttr(out, in0, in1, accum, r, w):
        S.add("dve", lambda e: e.scalar_tensor_tensor(out=out, in0=in0, scalar=1.0, in1=in1, op0=ALU.mult,
                                                      op1=ALU.mult, accum_out=accum), r, w)

    def mm(out, lhsT, rhs, start, stop, r, w):
        S.add("pe", lambda e: e.matmul(out, lhsT=lhsT, rhs=rhs, start=start, stop=stop), r, w)

    def tr(out, in_, ident, r, w):
        S.add("pe", lambda e: e.transpose(out=out, in_=in_, identity=ident), r, w)

    def run_gen(gen, n):
        if gen is None:
            return
        for _ in range(n):
            try:
                next(gen)
            except StopIteration:
                return

    def recip(out, in_, r, w):
        S.add("dve", lambda e: e.reciprocal(out=out, in_=in_), r, w)

    def memset(ap, val, w, eng="dve"):
        S.add(eng, lambda e: e.memset(ap, val), (), w)

    with ExitStack() as top:
        def sbt(es, name, shape, dt):
            return es.enter_context(nc.sbuf_tensor("sb_" + name, list(shape), dt))

        def pst(es, name, shape, dt):
            return es.enter_context(nc.psum_tensor("ps_" + name, list(shape), dt))

        identf = sbt(top, "identf", [128, 128], F32)
        identb = sbt(top, "identb", [128, 128], BF16)
        eps_t = sbt(top, "eps_t", [128, 1], F32)
        ln8_t = sbt(top, "ln8_t", [128, 1], F32)
        junk = sbt(top, "junk", [128, D], BF16)
        small = sbt(top, "small", [128, 64], F32)
        dma(identf[:], ident_d, (), ["identf"], "c_id")
        vcopy(identb[:], identf[:], ["identf"], ["identb"])
        memset(eps_t[:], EPS, ["eps_t"])
        memset(ln8_t[:], LN8, ["ln8_t"])
        cm = ExitStack()
        mix = sbt(cm, "mix", [128, NTB, D], BF16)


        def PEER_PHASE():
          with ExitStack() as p3:
            gffn = sbt(p3, "gffn", [128, 8], F32)
            gfin = sbt(p3, "gfin", [128, D], BF16)
            skT = sbt(p3, "skT", [128, 16, 128], BF16)
            iota = sbt(p3, "iota", [128, 128], F32)
            io4 = sbt(p3, "io4", [128, 1024], BF16)
            dma(gffn[:], gffn_d, (), ["gffn"], "c_gf")
            dma(iota[:], iota_d, (), ["iota"], "c_io")
            with ExitStack() as pc:
                cf = [sbt(pc, "cf%d" % i, [128, 2048], F32) for i in range(2)]
                cb = [sbt(pc, "cb%d" % i, [128, 2048], BF16) for i in range(2)]
                j = 0

                def conv(s_, scale_ap, dst, even):
                    if scale_ap is None:
                        if even:
                            act(dst, cf[s_][:], AF.Copy, [("cf", s_)], [("cb", s_)])
                        else:
                            vcopy(dst, cf[s_][:], [("cf", s_)], [("cb", s_)])
                    elif even:
                        act(dst, cf[s_][:], AF.Copy, [("cf", s_), "gffn"], [("cb", s_)], scale=scale_ap)
                    else:
                        ts(dst, cf[s_][:], scale_ap, ALU.mult, [("cf", s_), "gffn"], [("cb", s_)])

                dma(cf[0][:, 0:D], gfin_d, (), [("cf", 0)], "c_cf0")
                vcopy(gfin[:], cf[0][:, 0:D], [("cf", 0)], ["gfin"])
                dma(cf[0][:], io4_d, (), [("cf", 0)], "c_cf0")
                vcopy(io4[:], cf[0][:, 0:1024], [("cf", 0)], ["io4"])
                dma(cf[1][:], skT_d.rearrange("p g n -> p (g n)"), (), [("cf", 1)], "c_cf1")
                vcopy(skT[:].rearrange("p g n -> p (g n)"), cf[1][:], [("cf", 1)], ["skT"])
                for k in range(8):
                    s_ = j % 2
                    dma(cf[s_][:], wq_d[k * 128:(k + 1) * 128, :], (), [("cf", s_)], "c_cf%d" % s_)
                    conv(s_, gffn[:, k:k + 1], cb[s_][:], j % 2 == 0)
                    dma(WQ2[:, :, k, :].rearrange("g p e -> p g e"), cb[s_][:].rearrange("p (g e) -> p g e", e=128),
                        [("cb", s_)], ["WQ2"], "s_cb%d" % s_)
                    j += 1
            S.barrier()
            with ExitStack() as pb:
                G = [sbt(pb, "G%d" % i, [128, 256, 128], BF16) for i in range(2)]
                NSL = 5
                ut8 = [sbt(pb, "ut8_%d" % i, [128, 8, 128], BF16) for i in range(NSL)]
                v8 = [sbt(pb, "v8_%d" % i, [128, D], BF16) for i in range(NSL)]
                wqp = [sbt(pb, "wqp%d" % i, [128, 8, 128], BF16) for i in range(2)]
                xnT = [sbt(pb, "xnT%d" % i, [128, 8, 256], BF16) for i in range(2)]
                x2s = sbt(pb, "x2s", [128, D], F32)
                xnb = sbt(pb, "xnb", [128, D], BF16)
                qTs = sbt(pb, "qTs", [128, 16, 128], BF16)
                buf1 = sbt(pb, "buf1", [128, 2048], F32)
                s2g = [sbt(pb, "s2g%d" % i, [128, 256], F32) for i in range(2)]
                eqt = sbt(pb, "eqt", [128, 1024], BF16)
                topv = sbt(pb, "topv", [128, 16, 16], F32)
                idxu = sbt(pb, "idxu", [128, 16, 16], U32)
                idxf = sbt(pb, "idxf", [128, 16, 16], F32)
                best = sbt(pb, "best", [128, 8, 16], F32)
                posu = sbt(pb, "posu", [128, 8, 16], U32)
                abu = sbt(pb, "abu", [128, 2, 128], U32)
                abf = sbt(pb, "abf", [128, 2, 128], F32)
                ijg = sbt(pb, "ijg", [128, 3, 128], F32)
                gsm = sbt(pb, "gsm", [128, 16], F32)
                ijgT = sbt(pb, "ijgT", [128, 3, 128], F32)
                At = [sbt(pb, "At%d" % i, [128, 128], BF16) for i in range(4)]
                Bt = [sbt(pb, "Bt%d" % i, [128, 128], BF16) for i in range(4)]
                ga = [sbt(pb, "ga%d" % i, [128, 256], BF16) for i in range(2)]
                gw = [sbt(pb, "gw%d" % i, [128, 256], BF16) for i in range(2)]
                st3 = sbt(pb, "st3", [128, 8], F32)
                big = [pst(pb, "big%d" % i, [128, 512], F32) for i in range(4)]
                pa2 = [pst(pb, "pa%d" % i, [128, 512], F32) for i in range(2)]
                pp = [pst(pb, "ppx%d" % i, [128, 512], F32) for i in range(2)]
                io4v = io4[:].rearrange("p (h k a) -> p h k a", h=4, k=16)
                eq4 = eqt[:].rearrange("p (h k a) -> p h k a", h=4, k=16)
                cand4 = buf1[:].rearrange("p (h a b) -> p h a b", h=8, a=16)
                SK = [("s", b4) for b4 in range(4)]
                NBLK = DBG.get("nblk", 16)
                ppc = [0]

                def nextpp():
                    ppc[0] += 1
                    return ppc[0] % 2

                def prologue(blk):
                    gb = blk % 2
                    for sub in range(2):
                        tb = blk * 2 + sub
                        dma(x2s[:], X2s[tb * 128:(tb + 1) * 128, :], (), ["x2s"], "l_x2s", eng=DBG.get("pdma", "sp"))
                        ttr(junk[:], x2s[:], x2s[:], st3[:, 0:1], ["x2s"], ["ss3", "junk"])
                        act(st3[:, 1:2], st3[:, 0:1], AF.Sqrt, ["ss3", "eps_t"], ["rs3"], scale=1.0 / D, bias=eps_t[:])
                        recip(st3[:, 2:3], st3[:, 1:2], ["rs3"], ["rstd3"])
                        act(xnb[:], x2s[:], AF.Copy, ["x2s", "rstd3"], ["xnb"], scale=st3[:, 2:3])
                        q_ = nextpp()
                        pT3 = pp[q_][:].bitcast(BF16).rearrange("p (k t) -> p k t", k=8)
                        for k in range(8):
                            tr(pT3[:, k, :], xnb[:, k * 128:(k + 1) * 128], identb[:], ["xnb", "identb"], [("pp", q_)])
                        vcopy(xnT[gb][:, :, sub * 128:(sub + 1) * 128], pT3, [("pp", q_)], [("xnT", gb, sub)])
                        yield
                    XN = [("xnT", gb, 0), ("xnT", gb, 1)]
                    def sub_gen(sub):
                        tsl = slice(sub * 128, (sub + 1) * 128)
                        for g in range(16):
                            ws = g % 2
                            dma(wqp[ws][:], WQ2[g], (), [("wqp", ws)], "l_wq%d" % ws, eng=DBG.get("pdma", "sp"))
                            q_ = nextpp()
                            for k in range(8):
                                mm(pp[q_][:, 0:128], wqp[ws][:, k, :], xnT[gb][:, k, tsl], k == 0, k == 7,
                                   XN + [("wqp", ws)], [("pp", q_)])
                            act(qTs[:, g, :], pp[q_][:, 0:128], AF.Copy, [("pp", q_)], [("qTs", g)])
                            if g % 4 == 3:
                                yield ("proj_done" if g == 15 else "proj")
                        for b4 in range(4):
                            q_ = nextpp()
                            for g in range(b4 * 4, b4 * 4 + 4):
                                mm(pp[q_][:, (g % 4) * 128:(g % 4 + 1) * 128], qTs[:, g, :], skT[:, g, :], True, True,
                                   [("qTs", g), "skT"], [("pp", q_)])
                            act(buf1[:, b4 * 512:(b4 + 1) * 512], pp[q_][:], AF.Copy, [("pp", q_)], [("s", b4), "cand"])
                            yield ("scores_done" if b4 == 3 else "scores")
                        for g2 in range(8):
                            gs = (2 * g2, 2 * g2 + 1)
                            sgs = [buf1[:, g * 128:(g + 1) * 128] for g in gs]
                            kks = [("s", g // 4) for g in gs]
                            zs = [s2g[j_][:, 0:128] for j_ in range(2)]
                            zks = [("s2g", j_) for j_ in range(2)]
                            for j_, g in enumerate(gs):
                                S.add("dve", lambda e, g=g, sg=sgs[j_]: e.max(out=topv[:, g, 0:8], in_=sg), [kks[j_]], [("topv", g, 0)])
                            for j_, g in enumerate(gs):
                                S.add("dve", lambda e, g=g, sg=sgs[j_]: e.max_index(out=idxu[:, g, 0:8], in_max=topv[:, g, 0:8], in_values=sg),
                                      [kks[j_], ("topv", g, 0)], [("idxu", g, 0)])
                            for j_, g in enumerate(gs):
                                S.add("dve", lambda e, g=g, sg=sgs[j_], z=zs[j_]: e.match_replace(out=z, in_to_replace=topv[:, g, 0:8], in_values=sg, imm_value=-1e30),
                                      [kks[j_], ("topv", g, 0)], [zks[j_]])
                            for j_, g in enumerate(gs):
                                S.add("dve", lambda e, g=g, z=zs[j_]: e.max(out=topv[:, g, 8:16], in_=z), [zks[j_]], [("topv", g, 1)])
                            for j_, g in enumerate(gs):
                                S.add("dve", lambda e, g=g, z=zs[j_]: e.max_index(out=idxu[:, g, 8:16], in_max=topv[:, g, 8:16], in_values=z),
                                      [zks[j_], ("topv", g, 1)], [("idxu", g, 1)])
                            yield ("stage1_done" if g2 == 7 else "stage1")
                        TOPV = [("topv", g, q) for g in range(16) for q in range(2)]
                        IDXU = [("idxu", g, q) for g in range(16) for q in range(2)]
                        vcopy(idxf[:], idxu[:], IDXU, ["idxf"])
                        tv = topv[:].rearrange("p (h q) a -> p h q a", q=2)
                        idf = idxf[:].rearrange("p (h q) a -> p h q a", q=2)
                        tt(cand4, tv[:, :, 0, :].unsqueeze(3).to_broadcast([128, 8, 16, 16]),
                           tv[:, :, 1, :].unsqueeze(2).to_broadcast([128, 8, 16, 16]), ALU.add, TOPV, ["cand"] + SK)
                        yield "x"
                        for h2 in range(4):
                            hs_ = (2 * h2, 2 * h2 + 1)
                            chs = [buf1[:, h * 256:(h + 1) * 256] for h in hs_]
                            zs = [s2g[j_][:, 0:256] for j_ in range(2)]
                            zks = [("s2g", j_) for j_ in range(2)]
                            for j_, h in enumerate(hs_):
                                S.add("dve", lambda e, h=h, ch=chs[j_]: e.max(out=best[:, h, 0:8], in_=ch), ["cand"], [("best", h, 0)])
                            for j_, h in enumerate(hs_):
                                S.add("dve", lambda e, h=h, ch=chs[j_]: e.max_index(out=posu[:, h, 0:8], in_max=best[:, h, 0:8], in_values=ch),
                                      ["cand", ("best", h, 0)], [("posu", h, 0)])
                            for j_, h in enumerate(hs_):
                                S.add("dve", lambda e, h=h, ch=chs[j_], z=zs[j_]: e.match_replace(out=z, in_to_replace=best[:, h, 0:8], in_values=ch, imm_value=-1e30),
                                      ["cand", ("best", h, 0)], [zks[j_]])
                            for j_, h in enumerate(hs_):
                                S.add("dve", lambda e, h=h, z=zs[j_]: e.max(out=best[:, h, 8:16], in_=z), [zks[j_]], [("best", h, 1)])
                            for j_, h in enumerate(hs_):
                                S.add("dve", lambda e, h=h, z=zs[j_]: e.max_index(out=posu[:, h, 8:16], in_max=best[:, h, 8:16], in_values=z),
                                      [zks[j_], ("best", h, 1)], [("posu", h, 1)])
                            yield ("stage2_done" if h2 == 3 else "stage2")
                        BEST = [("best", h, q) for h in range(8) for q in range(2)]
                        POSU = [("posu", h, q) for h in range(8) for q in range(2)]
                        posf = posu[:].rearrange("p h k -> p (h k)")
                        ts(abu[:, 0, :], posf, 4, ALU.arith_shift_right, POSU, ["abu0"])
                        ts(abu[:, 1, :], posf, 15, ALU.bitwise_and, POSU, ["abu1"])
                        vcopy(abf[:], abu[:], ["abu0", "abu1"], ["abf"])
                        for q in range(2):
                            for hf in range(2):
                                hs = slice(hf * 4, hf * 4 + 4)
                                a_b = abf[:, q, :].rearrange("p (h k) -> p h k", h=8)[:, hs, :].unsqueeze(3).to_broadcast([128, 4, 16, 16])
                                tt(eq4, a_b, io4v, ALU.is_equal, ["abf", "io4"], ["eqt"])
                                tt(eq4, eq4, idf[:, hs, q, :].unsqueeze(2).to_broadcast([128, 4, 16, 16]), ALU.mult, ["eqt", "idxf"], ["eqt"])
                                S.add("dve", lambda e, q=q, hs=hs: e.tensor_reduce(
                                    out=ijg[:, q, :].rearrange("p (h k) -> p h k", h=8)[:, hs, :], in_=eq4,
                                    axis=AX.X, op=ALU.add), ["eqt"], [("ijg", q)])
                            yield "x"
                        g3 = ijg[:, 2, :].rearrange("p (h k) -> p h k", h=8)
                        tt(g3, best[:], best[:, :, 0:1].to_broadcast([128, 8, 16]), ALU.subtract, BEST, [("ijg", 2)])
                        act(g3, g3, AF.Exp, [("ijg", 2)], [("ijg", 2)])
                        S.add("dve", lambda e, g3=g3: e.tensor_reduce(out=gsm[:, 0:8], in_=g3, axis=AX.X, op=ALU.add), [("ijg", 2)], ["gsm"])
                        recip(gsm[:, 8:16], gsm[:, 0:8], ["gsm"], ["grc"])
                        tt(g3, g3, gsm[:, 8:16].unsqueeze(2).to_broadcast([128, 8, 16]), ALU.mult, [("ijg", 2), "grc"], [("ijg", 2)])
                        for _e in range(DBG.get("eyield", 4)):
                            yield "decode"
                        yield "decode_done"
                        q_ = nextpp()
                        for q in range(3):
                            tr(pp[q_][:, q * 128:(q + 1) * 128], ijg[:, q, :], identf[:], [("ijg", q), "identf"], [("pp", q_)])
                        vcopy(ijgT[:], pp[q_][:, 0:384].rearrange("p (q t) -> p q t", q=3), [("pp", q_)], ["ijgT"])
                        yield "x"
                        for t in range(128):
                            u4 = t % 4
                            u8 = t % 4
                            if u4 == 0:
                                q_ = nextpp()
                            S.add("dve", lambda e, t=t, u8=u8, sub=sub: e.tensor_scalar(
                                out=At[u8][:], in0=iota[:], scalar1=ijgT[:, 0, t:t + 1], scalar2=ijgT[:, 2, t:t + 1],
                                op0=ALU.is_equal, op1=ALU.mult), ["ijgT", "iota"], [("At", u8)])
                            S.add("dve", lambda e, t=t, u8=u8, sub=sub: e.tensor_scalar(
                                out=Bt[u8][:], in0=iota[:], scalar1=ijgT[:, 1, t:t + 1], scalar2=None,
                                op0=ALU.is_equal), ["ijgT", "iota"], [("Bt", u8)])
                            mm(pp[q_][:, u4 * 128:(u4 + 1) * 128], Bt[u8][:], At[u8][:], True, True,
                               [("At", u8), ("Bt", u8)], [("pp", q_)])
                            if u4 == 3:
                                t0 = sub * 128 + t - 3
                                act(G[gb][:, t0:t0 + 4, :], pp[q_][:].rearrange("p (t i) -> p t i", i=128), AF.Copy,
                                    [("pp", q_)], [("G", gb)])
                                yield "x"

                    def drive(g, until):
                        for m in g:
                            yield
                            if m == until:
                                return

                    def inter(a, b):
                        da = db = False
                        while not (da and db):
                            if not da:
                                try:
                                    next(a)
                                except StopIteration:
                                    da = True
                            if not db:
                                try:
                                    next(b)
                                except StopIteration:
                                    db = True
                            yield

                    g0 = sub_gen(0)
                    g1 = sub_gen(1)
                    yield from drive(g0, "scores_done")
                    yield from inter(drive(g0, "stage1_done"), drive(g1, "proj_done"))
                    yield from drive(g0, "stage2_done")
                    yield from inter(drive(g0, "decode_done"), drive(g1, "scores_done"))
                    yield from drive(g0, None)
                    yield from drive(g1, None)

                def run(gen, n):
                    if gen is None:
                        return
                    for _ in range(n):
                        try:
                            next(gen)
                        except StopIteration:
                            return

                run(prologue(0), 10 ** 6)
                for blk in range(NBLK):
                    gb = blk % 2
                    XN = [("xnT", gb, 0), ("xnT", gb, 1)]
                    gen = prologue(blk + 1) if blk + 1 < NBLK else None
                    def emit_u(i):
                        sl = i % NSL
                        pg = i % 2
                        pah = pa2[pg][:, 0:256]
                        for k in range(8):
                            mm(pah, ut8[sl][:, k, :], xnT[gb][:, k, :], k == 0, k == 7,
                               XN + [("ut8", sl)], [("pa", pg)])

                    def emit_mid(i):
                        pg = i % 2
                        pah = pa2[pg][:, 0:256]
                        act(ga[pg][:], pah, AF.Gelu, [("pa", pg)], [("ga", pg)])
                        tt(gw[pg][:], ga[pg][:], G[gb][:, :, i], ALU.mult, [("ga", pg), ("G", gb)], [("gw", pg)], eng=DBG.get("gweng", "pool"))

                    def emit_v(i):
                        sl = i % NSL
                        pg = i % 2
                        for sub in range(2):
                            for dh in range(2):
                                mm(big[sub * 2 + dh][:], gw[pg][:, sub * 128:(sub + 1) * 128], v8[sl][:, dh * 512:(dh + 1) * 512],
                                   i == 0, i == 127, [("gw", pg), ("v8", sl)], [("big", sub * 2 + dh)])

                    def emit_load(i):
                        sl = i % NSL
                        dma(ut8[sl][:], UT2[i], (), [("ut8", sl)], "l_ut%d" % sl)
                        dma(v8[sl][:], Vb[i * 128:(i + 1) * 128, :], (), [("v8", sl)], "l_v8%d" % sl)

                    for i in range(NSL - 2):
                        emit_load(i)
                    emit_u(0)
                    for i in range(128):
                        if i + NSL - 2 < 128:
                            emit_load(i + NSL - 2)
                        if i + 1 < 128:
                            emit_u(i + 1)
                        if i == DBG.get("reload_at", 122):
                            xe_ = buf1[:].rearrange("p (a d) -> p a d", d=D)
                            for sub_ in range(2):
                                tb_ = blk * 2 + sub_
                                dma(xe_[:, sub_, :], X2s[tb_ * 128:(tb_ + 1) * 128, :], (), [("xe", sub_)] + SK + ["cand"], "l_xe%d" % sub_)
                        emit_mid(i)
                        if i >= 1:
                            emit_v(i - 1)
                        run(gen, 1)
                    emit_v(127)
                    xe = buf1[:].rearrange("p (a d) -> p a d", d=D)
                    for sub in range(2):
                        tb = blk * 2 + sub
                        for dh in range(2):
                            tt(xe[:, sub, dh * 512:(dh + 1) * 512], big[sub * 2 + dh][:], xe[:, sub, dh * 512:(dh + 1) * 512], ALU.add,
                               [("big", sub * 2 + dh), ("xe", sub)], [("xe", sub)])
                        ttr(junk[:], xe[:, sub, :], xe[:, sub, :], st3[:, 4:5], [("xe", sub)], ["ss4", "junk"])
                        act(st3[:, 5:6], st3[:, 4:5], AF.Sqrt, ["ss4", "eps_t"], ["rs4"], scale=1.0 / D, bias=eps_t[:])
                        recip(st3[:, 6:7], st3[:, 5:6], ["rs4"], ["rstd4"])
                        stt(xe[:, sub, :], xe[:, sub, :], st3[:, 6:7], gfin[:], ALU.mult, ALU.mult, [("xe", sub), "rstd4", "gfin"], [("xe", sub)])
                        dma(out_d[tb * 128:(tb + 1) * 128, :], xe[:, sub, :], [("xe", sub)], ["out"] + SK + ["cand"], "s_out%d" % sub)
                    run(gen, 10 ** 6)

        with ExitStack() as p0:
            w_bf = sbt(p0, "w_bf", [128, 8, 3072], BF16)
            wst = [sbt(p0, "wst%d" % i, [128, 1024], F32) for i in range(2)]
            gattn = sbt(p0, "gattn", [128, 8], F32)
            ropd = sbt(p0, "ropd", [128, 2, NTB, 8], F32)
            ropr = sbt(p0, "ropr", [128, 2, NTB, 32], F32)
            xt = [sbt(p0, "xt%d" % i, [128, D], F32) for i in range(2)]
            hb = [sbt(p0, "hb%d" % i, [128, D], BF16) for i in range(2)]
            hT = [sbt(p0, "hT%d" % i, [128, 8, 128], BF16) for i in range(2)]
            qk32 = sbt(p0, "qk32", [128, 1536], F32)
            qkb = sbt(p0, "qkb", [128, 1536], BF16)
            rt = [sbt(p0, "rt%d" % i, [128, 256], F32) for i in range(4)]
            qkTst = sbt(p0, "qkTst", [128, 12, 512], BF16)
            vst = [sbt(p0, "vst%d" % i, [128, 512], BF16) for i in range(2)]
            rvst = [sbt(p0, "rvst%d" % i, [128, 512], BF16) for i in range(2)]
            rgst = [sbt(p0, "rgst%d" % i, [128, 512], BF16) for i in range(2)]
            st0 = sbt(p0, "st0", [128, 8], F32)
            pT = [pst(p0, "pT%d" % i, [128, 8, 128], BF16) for i in range(2)]
            pp = [pst(p0, "pp%d" % i, [128, 512], F32) for i in range(2)]
            pQ1 = pst(p0, "pQ1", [128, 8, 128], BF16)
            pQ2 = pst(p0, "pQ2", [128, 4, 128], BF16)

            dma(gattn[:], gattn_d, (), ["gattn"], "c_g")
            dma(ropd[:], ropd_d, (), ["ropd"], "c_rd")
            dma(ropr[:], ropr_d, (), ["ropr"], "c_rr")
            j = 0
            for k in range(8):
                for c in range(3):
                    s = j % 2
                    dma(wst[s][:], w_in_d[k * 128:(k + 1) * 128, c * 1024:(c + 1) * 1024], (), [("wst", s)], "c_w%d" % s)
                    if j % 2 == 0:
                        act(w_bf[:, k, c * 1024:(c + 1) * 1024], wst[s][:], AF.Copy, [("wst", s), "gattn"], [("w_bf", k, c)],
                            scale=gattn[:, k:k + 1])
                    else:
                        ts(w_bf[:, k, c * 1024:(c + 1) * 1024], wst[s][:], gattn[:, k:k + 1], ALU.mult,
                           [("wst", s), "gattn"], [("w_bf", k, c)])
                    j += 1
            WALL = [("w_bf", k, c) for k in range(8) for c in range(3)]

            for tb in range(NTB):
                s = tb % 2
                dma(xt[s][:], x_d[tb * 128:(tb + 1) * 128, :], (), [("xt", s)], "c_x%d" % s)
                ss = st0[:, s:s + 1]
                rs = st0[:, 2 + s:3 + s]
                rstd = st0[:, 4 + s:5 + s]
                ttr(junk[:], xt[s][:], xt[s][:], ss, [("xt", s)], [("ss", s), "junk"])
                act(rs, ss, AF.Sqrt, [("ss", s), "eps_t"], [("rs", s)], scale=1.0 / D, bias=eps_t[:])
                recip(rstd, rs, [("rs", s)], [("rstd", s)])
                act(hb[s][:], xt[s][:], AF.Copy, [("xt", s), ("rstd", s)], [("hb", s)], scale=rstd)
                for k in range(8):
                    tr(pT[s][:, k, :], hb[s][:, k * 128:(k + 1) * 128], identb[:], [("hb", s), "identb"], [("pT", s)])
                vcopy(hT[s][:], pT[s][:], [("pT", s)], [("hT", s)])
                for cg in range(6):
                    ps = pp[cg % 2]
                    pk = ("pp", cg % 2)
                    for k in range(8):
                        mm(ps[:], hT[s][:, k, :], w_bf[:, k, cg * 512:(cg + 1) * 512], k == 0, k == 7,
                           [("hT", s)] + WALL, [pk])
                    if cg == 0:
                        act(qk32[:, 0:512], ps[:], AF.Copy, [pk], [("qk32", 0)])
                    elif cg == 1:
                        vcopy(qk32[:, 512:1024], ps[:], [pk], [("qk32", 1)])
                    elif cg == 2:
                        act(vst[s][:], ps[:], AF.Copy, [pk], [("vst", s)])
                    elif cg == 3:
                        vcopy(qk32[:, 1024:1536], ps[:], [pk], [("qk32", 2)])
                    elif cg == 4:
                        vcopy(rvst[s][:], ps[:], [pk], [("rvst", s)])
                    else:
                        act(rgst[s][:], ps[:], AF.Silu, [pk], [("rgst", s)])
                QK32 = [("qk32", i) for i in range(3)]
                v_d = qk32[:, 0:1024].rearrange("p (g d) -> p g d", d=64)
                o_d = qkb[:, 0:1024].rearrange("p (g d) -> p g d", d=64)
                cd = ropd[:, 0, tb:tb + 1, :].to_broadcast([128, 16, 8])
                sd = ropd[:, 1, tb:tb + 1, :].to_broadcast([128, 16, 8])
                t = [rt[i][:, 0:128].rearrange("p (g d) -> p g d", d=8) for i in range(4)]
                tt(t[0], v_d[:, :, 0:8], cd, ALU.mult, QK32 + ["ropd"], [("rt", 0)])
                tt(t[1], v_d[:, :, 8:16], sd, ALU.mult, QK32 + ["ropd"], [("rt", 1)])
                tt(o_d[:, :, 0:8], t[0], t[1], ALU.subtract, [("rt", 0), ("rt", 1)], [("qkb", 0)])
                tt(t[2], v_d[:, :, 8:16], cd, ALU.mult, QK32 + ["ropd"], [("rt", 2)])
                tt(t[3], v_d[:, :, 0:8], sd, ALU.mult, QK32 + ["ropd"], [("rt", 3)])
                tt(o_d[:, :, 8:16], t[2], t[3], ALU.add, [("rt", 2), ("rt", 3)], [("qkb", 1)])
                act(o_d[:, :, 16:64], v_d[:, :, 16:64], AF.Copy, QK32, [("qkb", 2)])
                v_r = qk32[:, 1024:1536].rearrange("p (g d) -> p g d", d=64)
                o_r = qkb[:, 1024:1536].rearrange("p (g d) -> p g d", d=64)
                cr = ropr[:, 0, tb:tb + 1, :].to_broadcast([128, 8, 32])
                sr = ropr[:, 1, tb:tb + 1, :].to_broadcast([128, 8, 32])
                u = [rt[i][:, 0:256].rearrange("p (g d) -> p g d", d=32) for i in range(4)]
                tt(u[0], v_r[:, :, 0:32], cr, ALU.mult, QK32 + ["ropr"], [("rt", 0)])
                tt(u[1], v_r[:, :, 32:64], sr, ALU.mult, QK32 + ["ropr"], [("rt", 1)])
                tt(o_r[:, :, 0:32], u[0], u[1], ALU.subtract, [("rt", 0), ("rt", 1)], [("qkb", 3)])
                tt(u[2], v_r[:, :, 32:64], cr, ALU.mult, QK32 + ["ropr"], [("rt", 2)])
                tt(u[3], v_r[:, :, 0:32], sr, ALU.mult, QK32 + ["ropr"], [("rt", 3)])
                tt(o_r[:, :, 32:64], u[2], u[3], ALU.add, [("rt", 2), ("rt", 3)], [("qkb", 4)])
                QKB = [("qkb", i) for i in range(5)]
                for c in range(8):
                    tr(pQ1[:, c, :], qkb[:, c * 128:(c + 1) * 128], identb[:], QKB + ["identb"], ["pQ1"])
                for c in range(4):
                    tr(pQ2[:, c, :], qkb[:, (8 + c) * 128:(9 + c) * 128], identb[:], QKB + ["identb"], ["pQ2"])
                q4 = tb % 4
                act(qkTst[:, 0:8, q4 * 128:(q4 + 1) * 128], pQ1[:], AF.Copy, ["pQ1"], [("qkTst", q4, 0)])
                vcopy(qkTst[:, 8:12, q4 * 128:(q4 + 1) * 128], pQ2[:], ["pQ2"], [("qkTst", q4, 1)])
                dma(Vs[tb * 128:(tb + 1) * 128, :], vst[s][:], [("vst", s)], ["Vs"], "s_v%d" % s)
                dma(RVs[tb * 128:(tb + 1) * 128, :], rvst[s][:], [("rvst", s)], ["RVs"], "s_rv%d" % s)
                dma(RGs[tb * 128:(tb + 1) * 128, :], rgst[s][:], [("rgst", s)], ["RGs"], "s_rg%d" % s)
                if q4 == 3:
                    t4 = tb // 4
                    for c in range(12):
                        dma(QKT[c, :, t4 * 512:(t4 + 1) * 512], qkTst[:, c, :],
                            [("qkTst", a, b) for a in range(4) for b in range(2)], ["QKT"], "s_qkt")
        S.barrier()

        with ExitStack() as p1:
          if stage != "P0":
              qA = [sbt(p1, "qA%d" % i, [128, S_TOK], BF16) for i in range(2)]
              qB = [sbt(p1, "qB%d" % i, [128, S_TOK], BF16) for i in range(2)]
              kT = [sbt(p1, "kT%d" % i, [128, S_TOK], BF16) for i in range(2)]
              vv = [sbt(p1, "vv%d" % i, [128, NTB, 129], BF16) for i in range(2)]
              rgh = [sbt(p1, "rgh%d" % i, [128, NTB, 128], BF16) for i in range(2)]
              et = [sbt(p1, "et%d" % i, [128, 512], BF16) for i in range(3)]
              dlam = sbt(p1, "dlam", [128, 256], F32)
              rld = sbt(p1, "rld", [128, 8], F32)
              lg = sbt(p1, "lg", [128, 8], F32)
              rtab = sbt(p1, "rtab", [128, 10, 256], F32)
              e128 = sbt(p1, "e128", [128, 32], F32)
              Wm = sbt(p1, "Wm", [128, 4, 256], F32)
              wtmp = sbt(p1, "wtmp", [128, 2, 256], F32)
              pw = sbt(p1, "pw", [128, 2, 32], F32)
              oc = sbt(p1, "oc", [128, 2, 129], F32)
              rtmp = [sbt(p1, "rtmp%d" % i, [128, 256], F32) for i in range(2)]
              av = sbt(p1, "av", [128, 2, 128], F32)
              st1 = sbt(p1, "st1", [128, 32], F32)
              sT = [pst(p1, "sT%d" % i, [128, 512], F32) for i in range(4)]
              oacc = [pst(p1, "oacc%d" % i, [128, 512], F32) for i in range(4)]

              conv_eng = ["dve"]
              cgen = None
              if stage == "full":
                  cf1 = [sbt(p1, "cf1_%d" % i, [128, 2048], F32) for i in range(2)]
                  cb1 = [sbt(p1, "cb1_%d" % i, [128, 2048], BF16) for i in range(2)]
                  gffn1 = sbt(p1, "gffn1", [128, 8], F32)
                  dma(gffn1[:], gffn_d, (), ["gffn1"], "c_gf1")

                  def conv_gen():
                      j = 0
                      for k in range(8):
                          for cg in range(8):
                              s_ = j % 2
                              dma(cf1[s_][:], uT_d[k * 128:(k + 1) * 128, cg * 2048:(cg + 1) * 2048], (), [("cf1", s_)], "c_cf1%d" % s_)
                              if conv_eng[0] == "act":
                                  act(cb1[s_][:], cf1[s_][:], AF.Copy, [("cf1", s_), "gffn1"], [("cb1", s_)], scale=gffn1[:, k:k + 1])
                              else:
                                  ts(cb1[s_][:], cf1[s_][:], gffn1[:, k:k + 1], ALU.mult, [("cf1", s_), "gffn1"], [("cb1", s_)])
                              for hf in range(2):
                                  dma(UT2[cg * 16 + hf * 8:cg * 16 + (hf + 1) * 8, :, k, :].rearrange("c p e -> p c e"),
                                      cb1[s_][:, hf * 1024:(hf + 1) * 1024].rearrange("p (c e) -> p c e", e=128),
                                      [("cb1", s_)], ["UT2"], "s_cb1%d" % s_)
                              j += 1
                              yield
                      for r in range(64):
                          s_ = j % 2
                          dma(cf1[s_][:].rearrange("p (a d) -> p a d", d=D),
                              pv_d[r * 256:(r + 1) * 256, :].rearrange("(a p) d -> p a d", p=128), (), [("cf1", s_)], "c_cf1%d" % s_)
                          if conv_eng[0] == "act":
                              act(cb1[s_][:], cf1[s_][:], AF.Copy, [("cf1", s_)], [("cb1", s_)])
                          else:
                              vcopy(cb1[s_][:], cf1[s_][:], [("cf1", s_)], [("cb1", s_)])
                          dma(Vb[r * 256:(r + 1) * 256, :].rearrange("(a p) d -> p a d", p=128),
                              cb1[s_][:].rearrange("p (a d) -> p a d", d=D), [("cb1", s_)], ["Vb"], "s_cb1%d" % s_)
                          j += 1
                          yield

                  cgen = conv_gen()
              dma(dlam[:], dlam_d, (), ["dlam"], "c_dl")
              dma(rld[:], rld_d, (), ["rld"], "c_rl")
              dma(rtab[:], rtab_d, (), ["rtab"], "c_rt")
              dma(e128[:], e128_d, (), ["e128"], "c_e1")
              for i in range(2):
                  memset(vv[i][:, :, 128:129], 1.0, [("vv", i)])
                  memset(qA[i][64:128, :], 0.0, [("qT", i)], eng="pool")
                  memset(qB[i][0:64, :], 0.0, [("qT", i)], eng="pool")
              ttr(junk[:, 0:64], dlam[:, 0:64], dlam[:, 64:128], st1[:, 0:1], ["dlam"], ["l1", "junk"])
              ttr(junk[:, 0:64], dlam[:, 128:192], dlam[:, 192:256], st1[:, 1:2], ["dlam"], ["l2", "junk"])
              act(st1[:, 2:4], st1[:, 0:2], AF.Exp, ["l1", "l2"], ["l12e"])
              tt(st1[:, 4:5], st1[:, 3:4], st1[:, 2:3], ALU.subtract, ["l12e"], ["nl0"])
              ts(st1[:, 5:6], st1[:, 4:5], -LAMBDA_INIT, ALU.add, ["nl0"], ["neglam"])
              neglam = st1[:, 5:6]
              act(lg[:], rld[:], AF.Exp, ["rld"], ["lg0"])
              ts(lg[:], lg[:], -1.0, ALU.mult, ["lg0"], ["lg"])

              step = 0
              def emit_head_loads(hh):
                  is_diff = hh < 4
                  h = hh % 4
                  s = hh % 2
                  if is_diff:
                      dma(qA[s][0:64, :], QKT[h, 0:64, :], (), [("qT", s)], "l_q%d" % s)
                      dma(qB[s][64:128, :], QKT[h, 64:128, :], (), [("qT", s)], "l_q%d" % s)
                      dma(kT[s][:], QKT[4 + h], (), [("kT", s)], "l_k%d" % s)
                      for t8 in range(8):
                          dma(vv[s][:, t8 * 4:(t8 + 1) * 4, 0:128],
                              Vs.rearrange("(t p) c -> p t c", p=128)[:, t8 * 4:(t8 + 1) * 4, h * 128:(h + 1) * 128],
                              (), [("vv", s)], "l_v%d" % s)
                  else:
                      if h % 2 == 0:
                          dma(qA[s][0:64, :], QKT[8 + h // 2, 0:64, :], (), [("qT", s)], "l_q%d" % s)
                      else:
                          dma(qB[s][64:128, :], QKT[8 + h // 2, 64:128, :], (), [("qT", s)], "l_q%d" % s)
                      dma(kT[s][:], QKT[10 + h // 2], (), [("kT", s)], "l_k%d" % s)
                      for t8 in range(8):
                          dma(vv[s][:, t8 * 4:(t8 + 1) * 4, 0:128],
                              RVs.rearrange("(t p) c -> p t c", p=128)[:, t8 * 4:(t8 + 1) * 4, h * 128:(h + 1) * 128],
                              (), [("vv", s)], "l_v%d" % s)
                          dma(rgh[s][:, t8 * 4:(t8 + 1) * 4, :],
                              RGs.rearrange("(t p) c -> p t c", p=128)[:, t8 * 4:(t8 + 1) * 4, h * 128:(h + 1) * 128],
                              (), [("rgh", s)], "l_g%d" % s)

              HEADS = DBG["heads"]
              if HEADS:
                  emit_head_loads(HEADS[0])
              for hidx, hh in enumerate(HEADS):
                  is_diff = hh < 4
                  h = hh % 4
                  s = hh % 2
                  OPS = [("qT", s), ("kT", s), ("vv", s)]
                  if hidx + 1 < len(HEADS):
                      emit_head_loads(HEADS[hidx + 1])
                  if not is_diff:
                      pb = (h % 2) * 64
                      lgf = lg[:, h:h + 1]
                      lgb = lg[:, 4 + h:5 + h]
                      act(Wm[:, 0, :], rtab[:, 0, :], AF.Exp, ["rtab", "lg", "ln8_t"], [("Wm", 0)], scale=lgf, bias=ln8_t[:])
                      act(Wm[:, 1, :], rtab[:, 1, :], AF.Exp, ["rtab", "lg", "ln8_t"], [("Wm", 1)], scale=lgb, bias=ln8_t[:])
                      for r_ in range(2):
                          act(wtmp[:, 0, :], rtab[:, 2 + r_, :], AF.Exp, ["rtab", "lg", "ln8_t"], [("wtmp", 0)], scale=lgf, bias=ln8_t[:])
                          act(wtmp[:, 1, :], rtab[:, 4 + r_, :], AF.Exp, ["rtab", "lg", "ln8_t"], [("wtmp", 1)], scale=lgb, bias=ln8_t[:])
                          tt(wtmp[:, 0, :], wtmp[:, 0, :], rtab[:, 6 + r_, :], ALU.mult, [("wtmp", 0), "rtab"], [("wtmp", 0)])
                          tt(wtmp[:, 1, :], wtmp[:, 1, :], rtab[:, 8 + r_, :], ALU.mult, [("wtmp", 1), "rtab"], [("wtmp", 1)])
                          tt(Wm[:, 2 + r_, :], wtmp[:, 0, :], wtmp[:, 1, :], ALU.add, [("wtmp", 0), ("wtmp", 1)], [("Wm", 2 + r_)])
                      act(pw[:, 0, :], e128[:], AF.Exp, ["e128", "lg"], [("pw", 0)], scale=lgf)
                      act(pw[:, 1, :], e128[:], AF.Exp, ["e128", "lg"], [("pw", 1)], scale=lgb)
                  def emit_post(qb):
                      for qs in (range(2) if DBG["post"] else []):
                          tb = qb * 2 + qs
                          if is_diff:
                              vcopy(oc[:, 0, :], oacc[qs][:, 0:129], [("oacc", qs)], [("oc", 0)])
                              act(oc[:, 1, :], oacc[2 + qs][:, 0:129], AF.Copy, [("oacc", 2 + qs)], [("oc", 1)])
                              recip(st1[:, 8:10], oc[:, :, 128], [("oc", 0), ("oc", 1)], ["rs2"])
                              tt(st1[:, 10:11], st1[:, 9:10], neglam, ALU.mult, ["rs2", "neglam"], ["nl"])
                              ts(av[:, 0, :], oc[:, 0, 0:128], st1[:, 8:9], ALU.mult, [("oc", 0), "rs2"], [("av", 0)])
                              stt(av[:, 1, :], oc[:, 1, 0:128], st1[:, 10:11], av[:, 0, :], ALU.mult, ALU.add,
                                  [("oc", 1), "nl", ("av", 0)], [("av", 1)])
                              ttr(junk[:, 0:128], av[:, 1, :], av[:, 1, :], st1[:, 11:12], [("av", 1)], ["ssq", "junk"])
                              act(st1[:, 12:13], st1[:, 11:12], AF.Ln, ["ssq", "eps_t"], ["lnv"], scale=1.0 / 128, bias=eps_t[:])
                              act(st1[:, 13:14], st1[:, 12:13], AF.Exp, ["lnv"], ["rstd1"], scale=-0.5)
                              ts(mix[:, tb, h * 128:(h + 1) * 128], av[:, 1, :], st1[:, 13:14], ALU.mult,
                                 [("av", 1), "rstd1"], [("mix", tb, hh)])
                          else:
                              vcopy(oc[:, 0, 0:128], oacc[qs][:, 0:128], [("oacc", qs)], [("oc", 0)])
                              S.add("dve", lambda e: e.tensor_reduce(out=st1[:, 16:17], in_=oc[:, 0, 0:128], axis=AX.X, op=ALU.add),
                                    [("oc", 0)], ["rsum"])
                              ts(st1[:, 17:18], st1[:, 16:17], -1.0 / 128, ALU.mult, ["rsum"], ["nmean"])
                              ts(av[:, 0, :], oc[:, 0, 0:128], st1[:, 17:18], ALU.add, [("oc", 0), "nmean"], [("av", 0)])
                              ttr(junk[:, 0:128], av[:, 0, :], av[:, 0, :], st1[:, 18:19], [("av", 0)], ["ssq", "junk"])
                              act(st1[:, 19:20], st1[:, 18:19], AF.Ln, ["ssq", "eps_t"], ["lnv"], scale=1.0 / 128, bias=eps_t[:])
                              act(st1[:, 20:21], st1[:, 19:20], AF.Exp, ["lnv"], ["rstd1"], scale=-0.5)
                              stt(mix[:, tb, 512 + h * 128:512 + (h + 1) * 128], av[:, 0, :], st1[:, 20:21], rgh[s][:, tb, :],
                                  ALU.mult, ALU.mult, [("av", 0), "rstd1", ("rgh", s)], [("mix", tb, hh)])
                  def emit_qk(qb, kc, si):
                      qsl = slice(qb * 256, (qb + 1) * 256)
                      ksl = slice(kc * 128, (kc + 1) * 128)
                      if is_diff:
                          mm(sT[si][:, 0:256], kT[s][:, ksl], qA[s][:, qsl], True, True, OPS, [("sT", si)])
                          mm(sT[si][:, 256:512], kT[s][:, ksl], qB[s][:, qsl], True, True, OPS, [("sT", si)])
                      else:
                          mm(sT[si][:, 0:256], kT[s][:, ksl], (qA if h % 2 == 0 else qB)[s][:, qsl], True, True, OPS, [("sT", si)])
                  def emit_mid_av(qb, kc, si, ei):
                      if is_diff:
                          act(et[ei][:], sT[si][:], AF.Exp, [("sT", si)], [("et", ei)], scale=0.125)
                          for m in range(2):
                              for qs in range(2):
                                  a = m * 2 + qs
                                  mm(oacc[a][:, 0:129], et[ei][:, m * 256 + qs * 128:m * 256 + (qs + 1) * 128], vv[s][:, kc, :],
                                     kc == 0, kc == NTB - 1, [("et", ei)] + OPS, [("oacc", a)])
                      else:
                          nn = 2 * qb - kc
                          use_pool = (kc % 3 == 2) and DBG.get("retpool", False)
                          if (nn >= 1 or nn <= -2) and use_pool:
                              d_ = 0 if nn >= 1 else 1
                              pcol = pw[:, 0, nn:nn + 1] if nn >= 1 else pw[:, 1, -nn - 1:-nn]
                              tp_ = rtmp[kc % 2]
                              act(tp_[:], sT[si][:, 0:256], AF.Copy, [("sT", si), ("pw", d_)], [("rtmp", kc % 2)], scale=pcol)
                              tt(et[ei][:, 0:256], tp_[:], Wm[:, d_, :], ALU.mult, [("rtmp", kc % 2), ("Wm", d_)], [("et", ei)], eng="pool")
                          elif nn >= 1:
                              stt(et[ei][:, 0:256], sT[si][:, 0:256], pw[:, 0, nn:nn + 1], Wm[:, 0, :], ALU.mult, ALU.mult,
                                  [("sT", si), ("pw", 0), ("Wm", 0)], [("et", ei)])
                          elif nn <= -2:
                              stt(et[ei][:, 0:256], sT[si][:, 0:256], pw[:, 1, -nn - 1:-nn], Wm[:, 1, :], ALU.mult, ALU.mult,
                                  [("sT", si), ("pw", 1), ("Wm", 1)], [("et", ei)])
                          else:
                              r_ = kc - 2 * qb
                              tt(et[ei][:, 0:256], sT[si][:, 0:256], Wm[:, 2 + r_, :], ALU.mult,
                                 [("sT", si), ("Wm", 2 + r_)], [("et", ei)])
                          for qs in range(2):
                              mm(oacc[qs][:, 0:128], et[ei][:, qs * 128:(qs + 1) * 128], vv[s][:, kc, 0:128],
                                 kc == 0, kc == NTB - 1, [("et", ei)] + OPS, [("oacc", qs)])
                  steps = [(qb_, kc_) for qb_ in range(DBG["nqb"]) for kc_ in range(NTB)]
                  LA = 2
                  for j_ in range(min(LA, len(steps))):
                      emit_qk(steps[j_][0], steps[j_][1], (step + j_) % 4)
                  for n_, (qb_, kc_) in enumerate(steps):
                      if n_ + LA < len(steps):
                          emit_qk(steps[n_ + LA][0], steps[n_ + LA][1], (step + n_ + LA) % 4)
                      emit_mid_av(qb_, kc_, (step + n_) % 4, (step + n_) % 3)
                      if kc_ == NTB - 1:
                          emit_post(qb_)
                          conv_eng[0] = "dve" if is_diff else "act"
                          run_gen(cgen, 1)
                  if hh == DBG["heads"][-1]:
                      run_gen(cgen, 10 ** 6)
                  step += len(steps)
        S.barrier()

        with ExitStack() as p2:
            wo_bf = sbt(p2, "wo_bf", [128, 8, D], BF16)
            wst2 = [sbt(p2, "wst2_%d" % i, [128, D], F32) for i in range(2)]
            gmix = sbt(p2, "gmix", [128, 8], F32)
            mixT = sbt(p2, "mixT", [128, 8, 128], BF16)
            xt2 = [sbt(p2, "xt2_%d" % i, [128, D], F32) for i in range(2)]
            x2 = [sbt(p2, "x2_%d" % i, [128, D], F32) for i in range(2)]
            pM = pst(p2, "pM", [128, 8, 128], BF16)
            pX = [pst(p2, "pX%d" % i, [128, 512], F32) for i in range(2)]
            dma(gmix[:], gmix_d, (), ["gmix"], "c_gm")
            ts(gmix[:, 0:4], gmix[:, 0:4], 1.0 - LAMBDA_INIT, ALU.mult, ["gmix"], ["gmix"])
            for k in range(8):
                s = k % 2
                dma(wst2[s][:], w_out_d[k * 128:(k + 1) * 128, :], (), [("wst2", s)], "c_wo%d" % s)
                act(wo_bf[:, k, :], wst2[s][:], AF.Copy, [("wst2", s), "gmix"], [("wo_bf", k)], scale=gmix[:, k:k + 1])
            WO = [("wo_bf", k) for k in range(8)]
            for tb in range(NTB if stage in ("A", "full") else 0):
                s = tb % 2
                dma(xt2[s][:], x_d[tb * 128:(tb + 1) * 128, :], (), [("xt2", s)], "c_x2%d" % s)
                MIXK = [("mix", tb, hh) for hh in range(8)]
                for k in range(8):
                    tr(pM[:, k, :], mix[:, tb, k * 128:(k + 1) * 128], identb[:], MIXK + ["identb"], ["pM"])
                vcopy(mixT[:], pM[:], ["pM"], ["mixT"])
                for dh in range(2):
                    for k in range(8):
                        mm(pX[dh][:], mixT[:, k, :], wo_bf[:, k, dh * 512:(dh + 1) * 512], k == 0, k == 7,
                           ["mixT"] + WO, [("pX", dh)])
                    tt(x2[s][:, dh * 512:(dh + 1) * 512], pX[dh][:], xt2[s][:, dh * 512:(dh + 1) * 512], ALU.add,
                       [("pX", dh), ("xt2", s)], [("x2", s, dh)])
                if stage == "A":
                    dma(out_d[tb * 128:(tb + 1) * 128, :], x2[s][:], [("x2", s, 0), ("x2", s, 1)], ["out"], "s_o%d" % s)
                else:
                    dma(X2s[tb * 128:(tb + 1) * 128, :], x2[s][:], [("x2", s, 0), ("x2", s, 1)], ["X2s"], "s_o%d" % s)
        cm.close()
        if stage == "full":
            S.barrier()
            PEER_PHASE()
        if stage not in ("A", "full"):
            dma(out_d[0:128, 0:128], identf[:], ["identf"], ["out"], "s_o0")
        S.emit()
    return nc


def host_inputs(inputs, b, shared=None):
    if shared is None:
        shared = host_shared(inputs)
    f32 = np.float32
    x = np.ascontiguousarray(inputs["x"][b], dtype=f32)
    pos = np.arange(S_TOK, dtype=f32)
    rot_dim = 16
    rope_inv = np.power(f32(500000.0), -np.arange(rot_dim // 2, dtype=f32) * f32(2.0) / f32(rot_dim)).astype(f32)
    ret_inv = (f32(1.0) / np.power(f32(10000.0), np.linspace(0.0, 1.0, 32, dtype=f32))).astype(f32)

    def tab(inv):
        ang = (pos[:, None] * inv[None, :]).astype(f32)
        c = np.cos(ang).astype(f32).reshape(NTB, 128, -1).transpose(1, 0, 2)
        s_ = np.sin(ang).astype(f32).reshape(NTB, 128, -1).transpose(1, 0, 2)
        return np.ascontiguousarray(np.stack([c, s_], axis=1))

    jl = np.arange(128, dtype=f32)[:, None]
    xx = np.arange(256, dtype=f32)[None, :]
    rt = np.zeros((128, 10, 256), f32)
    rt[:, 0] = xx - jl
    rt[:, 1] = jl - xx + 128.0
    for r in range(2):
        dd = xx - 128.0 * r - jl
        rt[:, 2 + r] = np.maximum(dd, 0)
        rt[:, 4 + r] = np.maximum(-dd, 0)
        rt[:, 6 + r] = (dd >= 0).astype(f32)
        rt[:, 8 + r] = (dd < 0).astype(f32)
    gm = np.concatenate([np.tile(inputs["diff_norm_g"][0][:, None], (1, 4)), inputs["ret_norm_g"][0].T], axis=1)
    return {
        "x": x,
        "w_in": np.ascontiguousarray(inputs["w_in"][0], dtype=f32),
        "gattn": np.ascontiguousarray(inputs["attn_norm_g"][0].reshape(8, 128).T, dtype=f32),
        "ropd": tab(rope_inv),
        "ropr": tab(ret_inv),
        "dlam": np.ascontiguousarray(np.tile(inputs["diff_lambda"][0].reshape(1, 256), (128, 1)), dtype=f32),
        "rld": np.ascontiguousarray(np.tile(inputs["ret_log_decay"][0].reshape(1, 8), (128, 1)), dtype=f32),
        "rtab": rt,
        "e128": np.ascontiguousarray(np.tile((128.0 * np.arange(32, dtype=f32))[None, :], (128, 1))),
        "ident": np.eye(128, dtype=f32),
        "w_out": np.ascontiguousarray(inputs["w_out"][0], dtype=f32),
        "gmix": np.ascontiguousarray(gm, dtype=f32),
        "wq": np.ascontiguousarray(inputs["peer_w_query"][0], dtype=f32),
        "gffn": np.ascontiguousarray(inputs["ffn_norm_g"][0].reshape(8, 128).T, dtype=f32),
        "skT": np.ascontiguousarray(inputs["peer_sub_keys"][0].reshape(16, 128, 128).transpose(2, 0, 1), dtype=f32),
        "uT": shared["uT"],
        "pv": shared["pv"],
        "gfin": np.ascontiguousarray(np.tile(inputs["final_norm_g"].reshape(1, D), (128, 1)), dtype=f32),
        "iota": np.ascontiguousarray(np.tile(np.arange(128, dtype=f32)[None, :], (128, 1))),
        "io4": np.ascontiguousarray(np.tile(np.arange(16, dtype=f32)[None, :], (128, 128))),
    }


def host_shared(inputs):
    return {"uT": np.ascontiguousarray(np.asarray(inputs["peer_u"][0], dtype=np.float32).T),
            "pv": np.ascontiguousarray(inputs["peer_v"][0], dtype=np.float32)}


def kernel(**inputs):
    inputs = {k: np.asarray(v) for k, v in inputs.items()}
    nc = build("full")
    shared = host_shared(inputs)
    in_maps = [host_inputs(inputs, b, shared) for b in range(8)]
    res = run_bass_kernel_spmd(nc, in_maps, core_ids=list(range(8)))
    return np.stack([np.asarray(r["out"], dtype=np.float32) for r in res.results], axis=0)
```

```python
from contextlib import ExitStack
import math
import numpy as np
import concourse.bass as bass
import concourse.mybir as mybir
from concourse.bass_utils import run_bass_kernel_spmd

F32 = mybir.dt.float32
BF16 = mybir.dt.bfloat16
U32 = mybir.dt.uint32
AF = mybir.ActivationFunctionType
ALU = mybir.AluOpType
AX = mybir.AxisListType

ENGS = ("pe", "act", "dve", "pool", "sp")
S_TOK = 4096
D = 1024
NTB = S_TOK // 128
EPS = 1e-6
LAMBDA_INIT = 0.8 - 0.6 * math.exp(-0.3 * 0)
LN8 = math.log(0.125)
DBG = {"heads": list(range(8)), "nqb": 16, "post": True, "steps": True}


class Sched:
    def __init__(self, nc):
        self.nc = nc
        self.ops = []
        self.last_w = {}
        self.readers = {}
        self.chan_last = {}

    def add(self, eng, fn, reads=(), writes=(), chan=None):
        i = len(self.ops)
        deps = {}
        for k in reads:
            w = self.last_w.get(k)
            if w is not None:
                deps[w] = True
        for k in writes:
            w = self.last_w.get(k)
            if w is not None:
                deps.setdefault(w, False)
            for r in self.readers.get(k, ()):
                deps.setdefault(r, False)
        if chan is not None:
            p = self.chan_last.get(chan)
            if p is not None:
                deps[p] = True
            self.chan_last[chan] = i
        self.ops.append((eng, fn, deps, chan))
        for k in writes:
            self.last_w[k] = i
            self.readers[k] = []
        for k in reads:
            self.readers.setdefault(k, []).append(i)
        return i

    def barrier(self):
        self.ops.append(("BARRIER", None, {}, None))
        self.last_w.clear()
        self.readers.clear()
        self.chan_last.clear()

    def _skip(self, eng, chan, de, raw):
        return de == eng and chan is None and (eng == "pe" or not raw)

    def emit(self):
        nc = self.nc
        ops = self.ops
        n = len(ops)
        need = [False] * n
        for i, (eng, fn, deps, chan) in enumerate(ops):
            for d, raw in deps.items():
                de, _, _, dchan = ops[d]
                if dchan is not None:
                    continue
                if self._skip(eng, chan, de, raw):
                    continue
                need[d] = True
        cnt = {e: 0 for e in ENGS}
        chan_cnt = {}
        sig = [0] * n
        bar_snap = {}
        for i, (eng, fn, deps, chan) in enumerate(ops):
            if eng == "BARRIER":
                bar_snap[i] = dict(chan_cnt)
                continue
            if chan is not None:
                chan_cnt[chan] = chan_cnt.get(chan, 0) + 16
                sig[i] = chan_cnt[chan]
            elif need[i]:
                cnt[eng] += 1
                sig[i] = cnt[eng]
        with ExitStack() as es:
            engsem = {e: es.enter_context(nc.semaphore("sem_" + e)) for e in ENGS}
            bsem = es.enter_context(nc.semaphore("sem_bar"))
            chansem = {c: es.enter_context(nc.semaphore("dsem_%d" % j))
                       for j, c in enumerate(chan_cnt)}
            block = es.enter_context(nc.Block())

            def make(eng):
                def body(e):
                    waited = {}
                    nbar = 0
                    for i, (oeng, fn, deps, chan) in enumerate(ops):
                        if oeng == "BARRIER":
                            nbar += 1
                            if eng == "sp":
                                for c, v in bar_snap[i].items():
                                    if waited.get(("c", c), 0) < v:
                                        e.wait_ge(chansem[c], v)
                                        waited[("c", c)] = v
                            e.drain().then_inc(bsem, 1)
                            e.wait_ge(bsem, len(ENGS) * nbar)
                            continue
                        if oeng != eng:
                            continue
                        want = {}
                        for d, raw in deps.items():
                            de, _, _, dchan = ops[d]
                            if dchan is not None:
                                key = ("c", dchan)
                                s = chansem[dchan]
                            else:
                                if self._skip(eng, chan, de, raw):
                                    continue
                                key = ("e", de)
                                s = engsem[de]
                            v = sig[d]
                            if waited.get(key, 0) >= v:
                                continue
                            if key not in want or want[key][1] < v:
                                want[key] = (s, v)
                        for key, (s, v) in want.items():
                            e.wait_ge(s, v)
                            waited[key] = v
                        ins = fn(e)
                        if chan is not None:
                            ins.then_inc(chansem[chan], 16)
                        elif need[i]:
                            ins.then_inc(engsem[eng], 1)
                    if eng == "sp":
                        for c, v in chan_cnt.items():
                            if waited.get(("c", c), 0) < v:
                                e.wait_ge(chansem[c], v)
                return body

            block.tensor(make("pe"))
            block.scalar(make("act"))
            block.vector(make("dve"))
            block.gpsimd(make("pool"))
            block.sync(make("sp"))


def build(stage="full"):
    nc = bass.Bass("TRN2", target_bir_lowering=False)
    S = Sched(nc)

    def din(name, shape, dt=F32):
        return nc.dram_tensor(name, list(shape), dt, kind="ExternalInput").ap()

    x_d = din("x", [S_TOK, D])
    w_in_d = din("w_in", [D, 3072])
    gattn_d = din("gattn", [128, 8])
    ropd_d = din("ropd", [128, 2, NTB, 8])
    ropr_d = din("ropr", [128, 2, NTB, 32])
    dlam_d = din("dlam", [128, 256])
    rld_d = din("rld", [128, 8])
    rtab_d = din("rtab", [128, 10, 256])
    e128_d = din("e128", [128, 32])
    ident_d = din("ident", [128, 128])
    w_out_d = din("w_out", [D, D])
    gmix_d = din("gmix", [128, 8])
    wq_d = din("wq", [D, 2048])
    gffn_d = din("gffn", [128, 8])
    skT_d = din("skT", [128, 16, 128])
    uT_d = din("uT", [D, 16384])
    pv_d = din("pv", [16384, D])
    gfin_d = din("gfin", [128, D])
    iota_d = din("iota", [128, 128])
    io4_d = din("io4", [128, 2048])
    out_d = nc.dram_tensor("out", [S_TOK, D], F32, kind="ExternalOutput").ap()

    QKT = nc.dram_tensor("scr_qkt", [12, 128, S_TOK], BF16).ap()
    Vs = nc.dram_tensor("scr_v", [S_TOK, 512], BF16).ap()
    RVs = nc.dram_tensor("scr_rv", [S_TOK, 512], BF16).ap()
    RGs = nc.dram_tensor("scr_rg", [S_TOK, 512], BF16).ap()
    X2s = nc.dram_tensor("scr_x2", [S_TOK, D], F32).ap()
    UT2 = nc.dram_tensor("scr_ut2", [128, 128, 8, 128], BF16).ap()
    WQ2 = nc.dram_tensor("scr_wq2", [16, 128, 8, 128], BF16).ap()
    Vb = nc.dram_tensor("scr_vb", [16384, D], BF16).ap()

    def dma(out, in_, r, w, chan, eng="sp"):
        S.add(eng, lambda e: e.dma_start(out=out, in_=in_), r, w, chan=chan)

    def act(out, in_, func, r, w, scale=None, bias=None, accum=None):
        kw = {}
        if scale is not None:
            kw["scale"] = scale
        if bias is not None:
            kw["bias"] = bias
        if accum is not None:
            kw["accum_out"] = accum
        S.add("act", lambda e: e.activation(out=out, in_=in_, func=func, **kw), r, w)

    def vcopy(out, in_, r, w, eng="dve"):
        S.add(eng, lambda e: e.tensor_copy(out=out, in_=in_), r, w)

    def tt(out, in0, in1, op, r, w, eng="dve"):
        S.add(eng, lambda e: e.tensor_tensor(out=out, in0=in0, in1=in1, op=op), r, w)

    def ts(out, in0, s1, op0, r, w, s2=None, op1=None, eng="dve"):
        if op1 is None:
            S.add(eng, lambda e: e.tensor_scalar(out=out, in0=in0, scalar1=s1, scalar2=None, op0=op0), r, w)
        else:
            S.add(eng, lambda e: e.tensor_scalar(out=out, in0=in0, scalar1=s1, scalar2=s2, op0=op0, op1=op1), r, w)

    def stt(out, in0, scalar, in1, op0, op1, r, w):
        S.add("dve", lambda e: e.scalar_tensor_tensor(out=out, in0=in0, scalar=scalar, in1=in1, op0=op0, op1=op1), r, w)

    def ttr(out, in0, in1, accum, r, w):
        S.add("dve", lambda e: e.scalar_tensor_tensor(out=out, in0=in0, scalar=1.0, in1=in1, op0=ALU.mult,
                                                      op1=ALU.mult, accum_out=accum), r, w)

    def mm(out, lhsT, rhs, start, stop, r, w):
        S.add("pe", lambda e: e.matmul(out, lhsT=lhsT, rhs=rhs, start=start, stop=stop), r, w)

    def tr(out, in_, ident, r, w):
        S.add("pe", lambda e: e.transpose(out=out, in_=in_, identity=ident), r, w)

    def run_gen(gen, n):
        if gen is None:
            return
        for _ in range(n):
            try:
                next(gen)
            except StopIteration:
                return

    def recip(out, in_, r, w):
        S.add("dve", lambda e: e.reciprocal(out=out, in_=in_), r, w)

    def memset(ap, val, w, eng="dve"):
        S.add(eng, lambda e: e.memset(ap, val), (), w)

    with ExitStack() as top:
        def sbt(es, name, shape, dt):
            return es.enter_context(nc.sbuf_tensor("sb_" + name, list(shape), dt))

        def pst(es, name, shape, dt):
            return es.enter_context(nc.psum_tensor("ps_" + name, list(shape), dt))

        identf = sbt(top, "identf", [128, 128], F32)
        identb = sbt(top, "identb", [128, 128], BF16)
        eps_t = sbt(top, "eps_t", [128, 1], F32)
        ln8_t = sbt(top, "ln8_t", [128, 1], F32)
        junk = sbt(top, "junk", [128, D], BF16)
        small = sbt(top, "small", [128, 64], F32)
        dma(identf[:], ident_d, (), ["identf"], "c_id")
        vcopy(identb[:], identf[:], ["identf"], ["identb"])
        memset(eps_t[:], EPS, ["eps_t"])
        memset(ln8_t[:], LN8, ["ln8_t"])
        cm = ExitStack()
        mix = sbt(cm, "mix", [128, NTB, D], BF16)


        def PEER_PHASE():
          with ExitStack() as p3:
            gffn = sbt(p3, "gffn", [128, 8], F32)
            gfin = sbt(p3, "gfin", [128, D], BF16)
            skT = sbt(p3, "skT", [128, 16, 128], BF16)
            iota = sbt(p3, "iota", [128, 128], F32)
            io4 = sbt(p3, "io4", [128, 1024], BF16)
            dma(gffn[:], gffn_d, (), ["gffn"], "c_gf")
            dma(iota[:], iota_d, (), ["iota"], "c_io")
            with ExitStack() as pc:
                cf = [sbt(pc, "cf%d" % i, [128, 2048], F32) for i in range(2)]
                cb = [sbt(pc, "cb%d" % i, [128, 2048], BF16) for i in range(2)]
                j = 0

                def conv(s_, scale_ap, dst, even):
                    if scale_ap is None:
                        if even:
                            act(dst, cf[s_][:], AF.Copy, [("cf", s_)], [("cb", s_)])
                        else:
                            vcopy(dst, cf[s_][:], [("cf", s_)], [("cb", s_)])
                    elif even:
                        act(dst, cf[s_][:], AF.Copy, [("cf", s_), "gffn"], [("cb", s_)], scale=scale_ap)
                    else:
                        ts(dst, cf[s_][:], scale_ap, ALU.mult, [("cf", s_), "gffn"], [("cb", s_)])

                dma(cf[0][:, 0:D], gfin_d, (), [("cf", 0)], "c_cf0")
                vcopy(gfin[:], cf[0][:, 0:D], [("cf", 0)], ["gfin"])
                dma(cf[0][:], io4_d, (), [("cf", 0)], "c_cf0")
                vcopy(io4[:], cf[0][:, 0:1024], [("cf", 0)], ["io4"])
                dma(cf[1][:], skT_d.rearrange("p g n -> p (g n)"), (), [("cf", 1)], "c_cf1")
                vcopy(skT[:].rearrange("p g n -> p (g n)"), cf[1][:], [("cf", 1)], ["skT"])
                for k in range(8):
                    s_ = j % 2
                    dma(cf[s_][:], wq_d[k * 128:(k + 1) * 128, :], (), [("cf", s_)], "c_cf%d" % s_)
                    conv(s_, gffn[:, k:k + 1], cb[s_][:], j % 2 == 0)
                    dma(WQ2[:, :, k, :].rearrange("g p e -> p g e"), cb[s_][:].rearrange("p (g e) -> p g e", e=128),
                        [("cb", s_)], ["WQ2"], "s_cb%d" % s_)
                    j += 1
            S.barrier()
            with ExitStack() as pb:
                G = [sbt(pb, "G%d" % i, [128, 256, 128], BF16) for i in range(2)]
                NSL = 5
                ut8 = [sbt(pb, "ut8_%d" % i, [128, 8, 128], BF16) for i in range(NSL)]
                v8 = [sbt(pb, "v8_%d" % i, [128, D], BF16) for i in range(NSL)]
                wqp = [sbt(pb, "wqp%d" % i, [128, 8, 128], BF16) for i in range(2)]
                xnT = [sbt(pb, "xnT%d" % i, [128, 8, 256], BF16) for i in range(2)]
                x2s = sbt(pb, "x2s", [128, D], F32)
                xnb = sbt(pb, "xnb", [128, D], BF16)
                qTs = sbt(pb, "qTs", [128, 16, 128], BF16)
                buf1 = sbt(pb, "buf1", [128, 2048], F32)
                s2g = [sbt(pb, "s2g%d" % i, [128, 256], F32) for i in range(2)]
                eqt = sbt(pb, "eqt", [128, 1024], BF16)
                topv = sbt(pb, "topv", [128, 16, 16], F32)
                idxu = sbt(pb, "idxu", [128, 16, 16], U32)
                idxf = sbt(pb, "idxf", [128, 16, 16], F32)
                best = sbt(pb, "best", [128, 8, 16], F32)
                posu = sbt(pb, "posu", [128, 8, 16], U32)
                abu = sbt(pb, "abu", [128, 2, 128], U32)
                abf = sbt(pb, "abf", [128, 2, 128], F32)
                ijg = sbt(pb, "ijg", [128, 3, 128], F32)
                gsm = sbt(pb, "gsm", [128, 16], F32)
                ijgT = sbt(pb, "ijgT", [128, 3, 128], F32)
                At = [sbt(pb, "At%d" % i, [128, 128], BF16) for i in range(4)]
                Bt = [sbt(pb, "Bt%d" % i, [128, 128], BF16) for i in range(4)]
                ga = [sbt(pb, "ga%d" % i, [128, 256], BF16) for i in range(2)]
                gw = [sbt(pb, "gw%d" % i, [128, 256], BF16) for i in range(2)]
                st3 = sbt(pb, "st3", [128, 8], F32)
                big = [pst(pb, "big%d" % i, [128, 512], F32) for i in range(4)]
                pa2 = [pst(pb, "pa%d" % i, [128, 512], F32) for i in range(2)]
                pp = [pst(pb, "ppx%d" % i, [128, 512], F32) for i in range(2)]
                io4v = io4[:].rearrange("p (h k a) -> p h k a", h=4, k=16)
                eq4 = eqt[:].rearrange("p (h k a) -> p h k a", h=4, k=16)
                cand4 = buf1[:].rearrange("p (h a b) -> p h a b", h=8, a=16)
                SK = [("s", b4) for b4 in range(4)]
                NBLK = DBG.get("nblk", 16)
                ppc = [0]

                def nextpp():
                    ppc[0] += 1
                    return ppc[0] % 2

                def prologue(blk):
                    gb = blk % 2
                    for sub in range(2):
                        tb = blk * 2 + sub
                        dma(x2s[:], X2s[tb * 128:(tb + 1) * 128, :], (), ["x2s"], "l_x2s", eng=DBG.get("pdma", "sp"))
                        ttr(junk[:], x2s[:], x2s[:], st3[:, 0:1], ["x2s"], ["ss3", "junk"])
                        act(st3[:, 1:2], st3[:, 0:1], AF.Sqrt, ["ss3", "eps_t"], ["rs3"], scale=1.0 / D, bias=eps_t[:])
                        recip(st3[:, 2:3], st3[:, 1:2], ["rs3"], ["rstd3"])
                        act(xnb[:], x2s[:], AF.Copy, ["x2s", "rstd3"], ["xnb"], scale=st3[:, 2:3])
                        q_ = nextpp()
                        pT3 = pp[q_][:].bitcast(BF16).rearrange("p (k t) -> p k t", k=8)
                        for k in range(8):
                            tr(pT3[:, k, :], xnb[:, k * 128:(k + 1) * 128], identb[:], ["xnb", "identb"], [("pp", q_)])
                        vcopy(xnT[gb][:, :, sub * 128:(sub + 1) * 128], pT3, [("pp", q_)], [("xnT", gb, sub)])
                        yield
                    XN = [("xnT", gb, 0), ("xnT", gb, 1)]
                    def sub_gen(sub):
                        tsl = slice(sub * 128, (sub + 1) * 128)
                        for g in range(16):
                            ws = g % 2
                            dma(wqp[ws][:], WQ2[g], (), [("wqp", ws)], "l_wq%d" % ws, eng=DBG.get("pdma", "sp"))
                            q_ = nextpp()
                            for k in range(8):
                                mm(pp[q_][:, 0:128], wqp[ws][:, k, :], xnT[gb][:, k, tsl], k == 0, k == 7,
                                   XN + [("wqp", ws)], [("pp", q_)])
                            act(qTs[:, g, :], pp[q_][:, 0:128], AF.Copy, [("pp", q_)], [("qTs", g)])
                            if g % 4 == 3:
                                yield ("proj_done" if g == 15 else "proj")
                        for b4 in range(4):
                            q_ = nextpp()
                            for g in range(b4 * 4, b4 * 4 + 4):
                                mm(pp[q_][:, (g % 4) * 128:(g % 4 + 1) * 128], qTs[:, g, :], skT[:, g, :], True, True,
                                   [("qTs", g), "skT"], [("pp", q_)])
                            act(buf1[:, b4 * 512:(b4 + 1) * 512], pp[q_][:], AF.Copy, [("pp", q_)], [("s", b4), "cand"])
                            yield ("scores_done" if b4 == 3 else "scores")
                        for g2 in range(8):
                            gs = (2 * g2, 2 * g2 + 1)
                            sgs = [buf1[:, g * 128:(g + 1) * 128] for g in gs]
                            kks = [("s", g // 4) for g in gs]
                            zs = [s2g[j_][:, 0:128] for j_ in range(2)]
                            zks = [("s2g", j_) for j_ in range(2)]
                            for j_, g in enumerate(gs):
                                S.add("dve", lambda e, g=g, sg=sgs[j_]: e.max(out=topv[:, g, 0:8], in_=sg), [kks[j_]], [("topv", g, 0)])
                            for j_, g in enumerate(gs):
                                S.add("dve", lambda e, g=g, sg=sgs[j_]: e.max_index(out=idxu[:, g, 0:8], in_max=topv[:, g, 0:8], in_values=sg),
                                      [kks[j_], ("topv", g, 0)], [("idxu", g, 0)])
                            for j_, g in enumerate(gs):
                                S.add("dve", lambda e, g=g, sg=sgs[j_], z=zs[j_]: e.match_replace(out=z, in_to_replace=topv[:, g, 0:8], in_values=sg, imm_value=-1e30),
                                      [kks[j_], ("topv", g, 0)], [zks[j_]])
                            for j_, g in enumerate(gs):
                                S.add("dve", lambda e, g=g, z=zs[j_]: e.max(out=topv[:, g, 8:16], in_=z), [zks[j_]], [("topv", g, 1)])
                            for j_, g in enumerate(gs):
                                S.add("dve", lambda e, g=g, z=zs[j_]: e.max_index(out=idxu[:, g, 8:16], in_max=topv[:, g, 8:16], in_values=z),
                                      [zks[j_], ("topv", g, 1)], [("idxu", g, 1)])
                            yield ("stage1_done" if g2 == 7 else "stage1")
                        TOPV = [("topv", g, q) for g in range(16) for q in range(2)]
                        IDXU = [("idxu", g, q) for g in range(16) for q in range(2)]
                        vcopy(idxf[:], idxu[:], IDXU, ["idxf"])
                        tv = topv[:].rearrange("p (h q) a -> p h q a", q=2)
                        idf = idxf[:].rearrange("p (h q) a -> p h q a", q=2)
                        tt(cand4, tv[:, :, 0, :].unsqueeze(3).to_broadcast([128, 8, 16, 16]),
                           tv[:, :, 1, :].unsqueeze(2).to_broadcast([128, 8, 16, 16]), ALU.add, TOPV, ["cand"] + SK)
                        yield "x"
                        for h2 in range(4):
                            hs_ = (2 * h2, 2 * h2 + 1)
                            chs = [buf1[:, h * 256:(h + 1) * 256] for h in hs_]
                            zs = [s2g[j_][:, 0:256] for j_ in range(2)]
                            zks = [("s2g", j_) for j_ in range(2)]
                            for j_, h in enumerate(hs_):
                                S.add("dve", lambda e, h=h, ch=chs[j_]: e.max(out=best[:, h, 0:8], in_=ch), ["cand"], [("best", h, 0)])
                            for j_, h in enumerate(hs_):
                                S.add("dve", lambda e, h=h, ch=chs[j_]: e.max_index(out=posu[:, h, 0:8], in_max=best[:, h, 0:8], in_values=ch),
                                      ["cand", ("best", h, 0)], [("posu", h, 0)])
                            for j_, h in enumerate(hs_):
                                S.add("dve", lambda e, h=h, ch=chs[j_], z=zs[j_]: e.match_replace(out=z, in_to_replace=best[:, h, 0:8], in_values=ch, imm_value=-1e30),
                                      ["cand", ("best", h, 0)], [zks[j_]])
                            for j_, h in enumerate(hs_):
                                S.add("dve", lambda e, h=h, z=zs[j_]: e.max(out=best[:, h, 8:16], in_=z), [zks[j_]], [("best", h, 1)])
                            for j_, h in enumerate(hs_):
                                S.add("dve", lambda e, h=h, z=zs[j_]: e.max_index(out=posu[:, h, 8:16], in_max=best[:, h, 8:16], in_values=z),
                                      [zks[j_], ("best", h, 1)], [("posu", h, 1)])
                            yield ("stage2_done" if h2 == 3 else "stage2")
                        BEST = [("best", h, q) for h in range(8) for q in range(2)]
                        POSU = [("posu", h, q) for h in range(8) for q in range(2)]
                        posf = posu[:].rearrange("p h k -> p (h k)")
                        ts(abu[:, 0, :], posf, 4, ALU.arith_shift_right, POSU, ["abu0"])
                        ts(abu[:, 1, :], posf, 15, ALU.bitwise_and, POSU, ["abu1"])
                        vcopy(abf[:], abu[:], ["abu0", "abu1"], ["abf"])
                        for q in range(2):
                            for hf in range(2):
                                hs = slice(hf * 4, hf * 4 + 4)
                                a_b = abf[:, q, :].rearrange("p (h k) -> p h k", h=8)[:, hs, :].unsqueeze(3).to_broadcast([128, 4, 16, 16])
                                tt(eq4, a_b, io4v, ALU.is_equal, ["abf", "io4"], ["eqt"])
                                tt(eq4, eq4, idf[:, hs, q, :].unsqueeze(2).to_broadcast([128, 4, 16, 16]), ALU.mult, ["eqt", "idxf"], ["eqt"])
                                S.add("dve", lambda e, q=q, hs=hs: e.tensor_reduce(
                                    out=ijg[:, q, :].rearrange("p (h k) -> p h k", h=8)[:, hs, :], in_=eq4,
                                    axis=AX.X, op=ALU.add), ["eqt"], [("ijg", q)])
                            yield "x"
                        g3 = ijg[:, 2, :].rearrange("p (h k) -> p h k", h=8)
                        tt(g3, best[:], best[:, :, 0:1].to_broadcast([128, 8, 16]), ALU.subtract, BEST, [("ijg", 2)])
                        act(g3, g3, AF.Exp, [("ijg", 2)], [("ijg", 2)])
                        S.add("dve", lambda e, g3=g3: e.tensor_reduce(out=gsm[:, 0:8], in_=g3, axis=AX.X, op=ALU.add), [("ijg", 2)], ["gsm"])
                        recip(gsm[:, 8:16], gsm[:, 0:8], ["gsm"], ["grc"])
                        tt(g3, g3, gsm[:, 8:16].unsqueeze(2).to_broadcast([128, 8, 16]), ALU.mult, [("ijg", 2), "grc"], [("ijg", 2)])
                        for _e in range(DBG.get("eyield", 4)):
                            yield "decode"
                        yield "decode_done"
                        q_ = nextpp()
                        for q in range(3):
                            tr(pp[q_][:, q * 128:(q + 1) * 128], ijg[:, q, :], identf[:], [("ijg", q), "identf"], [("pp", q_)])
                        vcopy(ijgT[:], pp[q_][:, 0:384].rearrange("p (q t) -> p q t", q=3), [("pp", q_)], ["ijgT"])
                        yield "x"
                        for t in range(128):
                            u4 = t % 4
                            u8 = t % 4
                            if u4 == 0:
                                q_ = nextpp()
                            S.add("dve", lambda e, t=t, u8=u8, sub=sub: e.tensor_scalar(
                                out=At[u8][:], in0=iota[:], scalar1=ijgT[:, 0, t:t + 1], scalar2=ijgT[:, 2, t:t + 1],
                                op0=ALU.is_equal, op1=ALU.mult), ["ijgT", "iota"], [("At", u8)])
                            S.add("dve", lambda e, t=t, u8=u8, sub=sub: e.tensor_scalar(
                                out=Bt[u8][:], in0=iota[:], scalar1=ijgT[:, 1, t:t + 1], scalar2=None,
                                op0=ALU.is_equal), ["ijgT", "iota"], [("Bt", u8)])
                            mm(pp[q_][:, u4 * 128:(u4 + 1) * 128], Bt[u8][:], At[u8][:], True, True,
                               [("At", u8), ("Bt", u8)], [("pp", q_)])
                            if u4 == 3:
                                t0 = sub * 128 + t - 3
                                act(G[gb][:, t0:t0 + 4, :], pp[q_][:].rearrange("p (t i) -> p t i", i=128), AF.Copy,
                                    [("pp", q_)], [("G", gb)])
                                yield "x"

                    def drive(g, until):
                        for m in g:
                            yield
                            if m == until:
                                return

                    def inter(a, b):
                        da = db = False
                        while not (da and db):
                            if not da:
                                try:
                                    next(a)
                                except StopIteration:
                                    da = True
                            if not db:
                                try:
                                    next(b)
                                except StopIteration:
                                    db = True
                            yield

                    g0 = sub_gen(0)
                    g1 = sub_gen(1)
                    yield from drive(g0, "scores_done")
                    yield from inter(drive(g0, "stage1_done"), drive(g1, "proj_done"))
                    yield from drive(g0, "stage2_done")
                    yield from inter(drive(g0, "decode_done"), drive(g1, "scores_done"))
                    yield from drive(g0, None)
                    yield from drive(g1, None)

                def run(gen, n):
                    if gen is None:
                        return
                    for _ in range(n):
                        try:
                            next(gen)
                        except StopIteration:
                            return

                run(prologue(0), 10 ** 6)
                for blk in range(NBLK):
                    gb = blk % 2
                    XN = [("xnT", gb, 0), ("xnT", gb, 1)]
                    gen = prologue(blk + 1) if blk + 1 < NBLK else None
                    def emit_u(i):
                        sl = i % NSL
                        pg = i % 2
                        pah = pa2[pg][:, 0:256]
                        for k in range(8):
                            mm(pah, ut8[sl][:, k, :], xnT[gb][:, k, :], k == 0, k == 7,
                               XN + [("ut8", sl)], [("pa", pg)])

                    def emit_mid(i):
                        pg = i % 2
                        pah = pa2[pg][:, 0:256]
                        act(ga[pg][:], pah, AF.Gelu, [("pa", pg)], [("ga", pg)])
                        tt(gw[pg][:], ga[pg][:], G[gb][:, :, i], ALU.mult, [("ga", pg), ("G", gb)], [("gw", pg)], eng=DBG.get("gweng", "pool"))

                    def emit_v(i):
                        sl = i % NSL
                        pg = i % 2
                        for sub in range(2):
                            for dh in range(2):
                                mm(big[sub * 2 + dh][:], gw[pg][:, sub * 128:(sub + 1) * 128], v8[sl][:, dh * 512:(dh + 1) * 512],
                                   i == 0, i == 127, [("gw", pg), ("v8", sl)], [("big", sub * 2 + dh)])

                    def emit_load(i):
                        sl = i % NSL
                        dma(ut8[sl][:], UT2[i], (), [("ut8", sl)], "l_ut%d" % sl)
                        dma(v8[sl][:], Vb[i * 128:(i + 1) * 128, :], (), [("v8", sl)], "l_v8%d" % sl)

                    for i in range(NSL - 2):
                        emit_load(i)
                    emit_u(0)
                    for i in range(128):
                        if i + NSL - 2 < 128:
                            emit_load(i + NSL - 2)
                        if i + 1 < 128:
                            emit_u(i + 1)
                        if i == DBG.get("reload_at", 122):
                            xe_ = buf1[:].rearrange("p (a d) -> p a d", d=D)
                            for sub_ in range(2):
                                tb_ = blk * 2 + sub_
                                dma(xe_[:, sub_, :], X2s[tb_ * 128:(tb_ + 1) * 128, :], (), [("xe", sub_)] + SK + ["cand"], "l_xe%d" % sub_)
                        emit_mid(i)
                        if i >= 1:
                            emit_v(i - 1)
                        run(gen, 1)
                    emit_v(127)
                    xe = buf1[:].rearrange("p (a d) -> p a d", d=D)
                    for sub in range(2):
                        tb = blk * 2 + sub
                        for dh in range(2):
                            tt(xe[:, sub, dh * 512:(dh + 1) * 512], big[sub * 2 + dh][:], xe[:, sub, dh * 512:(dh + 1) * 512], ALU.add,
                               [("big", sub * 2 + dh), ("xe", sub)], [("xe", sub)])
                        ttr(junk[:], xe[:, sub, :], xe[:, sub, :], st3[:, 4:5], [("xe", sub)], ["ss4", "junk"])
                        act(st3[:, 5:6], st3[:, 4:5], AF.Sqrt, ["ss4", "eps_t"], ["rs4"], scale=1.0 / D, bias=eps_t[:])
                        recip(st3[:, 6:7], st3[:, 5:6], ["rs4"], ["rstd4"])
                        stt(xe[:, sub, :], xe[:, sub, :], st3[:, 6:7], gfin[:], ALU.mult, ALU.mult, [("xe", sub), "rstd4", "gfin"], [("xe", sub)])
                        dma(out_d[tb * 128:(tb + 1) * 128, :], xe[:, sub, :], [("xe", sub)], ["out"] + SK + ["cand"], "s_out%d" % sub)
                    run(gen, 10 ** 6)

        with ExitStack() as p0:
            w_bf = sbt(p0, "w_bf", [128, 8, 3072], BF16)
            wst = [sbt(p0, "wst%d" % i, [128, 1024], F32) for i in range(2)]
            gattn = sbt(p0, "gattn", [128, 8], F32)
            ropd = sbt(p0, "ropd", [128, 2, NTB, 8], F32)
            ropr = sbt(p0, "ropr", [128, 2, NTB, 32], F32)
            xt = [sbt(p0, "xt%d" % i, [128, D], F32) for i in range(2)]
            hb = [sbt(p0, "hb%d" % i, [128, D], BF16) for i in range(2)]
            hT = [sbt(p0, "hT%d" % i, [128, 8, 128], BF16) for i in range(2)]
            qk32 = [sbt(p0, "qk32_%d" % i, [128, 1536], F32) for i in range(2)]
            qkb = [sbt(p0, "qkb_%d" % i, [128, 1536], BF16) for i in range(2)]
            rt = [sbt(p0, "rt%d" % i, [128, 256], F32) for i in range(4)]
            qkTst = sbt(p0, "qkTst", [128, 12, 512], BF16)
            vst = [sbt(p0, "vst%d" % i, [128, 512], BF16) for i in range(2)]
            rvst = [sbt(p0, "rvst%d" % i, [128, 512], BF16) for i in range(2)]
            rgst = [sbt(p0, "rgst%d" % i, [128, 512], BF16) for i in range(2)]
            st0 = sbt(p0, "st0", [128, 8], F32)
            pT = [pst(p0, "pT%d" % i, [128, 8, 128], BF16) for i in range(2)]
            pp = [pst(p0, "pp%d" % i, [128, 512], F32) for i in range(2)]
            pQ1 = pst(p0, "pQ1", [128, 8, 128], BF16)
            pQ2 = pst(p0, "pQ2", [128, 4, 128], BF16)

            dma(gattn[:], gattn_d, (), ["gattn"], "c_g")
            dma(ropd[:], ropd_d, (), ["ropd"], "c_rd")
            dma(ropr[:], ropr_d, (), ["ropr"], "c_rr")
            j = 0
            for k in range(8):
                for c in range(3):
                    s = j % 2
                    dma(wst[s][:], w_in_d[k * 128:(k + 1) * 128, c * 1024:(c + 1) * 1024], (), [("wst", s)], "c_w%d" % s)
                    if j % 2 == 0:
                        act(w_bf[:, k, c * 1024:(c + 1) * 1024], wst[s][:], AF.Copy, [("wst", s), "gattn"], [("w_bf", k, c)],
                            scale=gattn[:, k:k + 1])
                    else:
                        ts(w_bf[:, k, c * 1024:(c + 1) * 1024], wst[s][:], gattn[:, k:k + 1], ALU.mult,
                           [("wst", s), "gattn"], [("w_bf", k, c)])
                    j += 1
            WALL = [("w_bf", k, c) for k in range(8) for c in range(3)]

            def stageA(tb):
                s = tb % 2
                dma(xt[s][:], x_d[tb * 128:(tb + 1) * 128, :], (), [("xt", s)], "c_x%d" % s)
                ss = st0[:, s:s + 1]
                rs = st0[:, 2 + s:3 + s]
                rstd = st0[:, 4 + s:5 + s]
                ttr(junk[:], xt[s][:], xt[s][:], ss, [("xt", s)], [("ss", s), "junk"])
                act(rs, ss, AF.Sqrt, [("ss", s), "eps_t"], [("rs", s)], scale=1.0 / D, bias=eps_t[:])
                recip(rstd, rs, [("rs", s)], [("rstd", s)])
                act(hb[s][:], xt[s][:], AF.Copy, [("xt", s), ("rstd", s)], [("hb", s)], scale=rstd)
                for k in range(8):
                    tr(pT[s][:, k, :], hb[s][:, k * 128:(k + 1) * 128], identb[:], [("hb", s), "identb"], [("pT", s)])
                vcopy(hT[s][:], pT[s][:], [("pT", s)], [("hT", s)])
                for cg in range(6):
                    ps = pp[cg % 2]
                    pk = ("pp", cg % 2)
                    for k in range(8):
                        mm(ps[:], hT[s][:, k, :], w_bf[:, k, cg * 512:(cg + 1) * 512], k == 0, k == 7,
                           [("hT", s)] + WALL, [pk])
                    if cg == 0:
                        act(qk32[s][:, 0:512], ps[:], AF.Copy, [pk], [("qk32", s, 0)])
                    elif cg == 1:
                        vcopy(qk32[s][:, 512:1024], ps[:], [pk], [("qk32", s, 1)])
                    elif cg == 2:
                        act(vst[s][:], ps[:], AF.Copy, [pk], [("vst", s)])
                    elif cg == 3:
                        vcopy(qk32[s][:, 1024:1536], ps[:], [pk], [("qk32", s, 2)])
                    elif cg == 4:
                        vcopy(rvst[s][:], ps[:], [pk], [("rvst", s)])
                    else:
                        act(rgst[s][:], ps[:], AF.Silu, [pk], [("rgst", s)])
            def stageB(tb):
                s = tb % 2
                QK32 = [("qk32", s, i) for i in range(3)]
                v_d = qk32[s][:, 0:1024].rearrange("p (g d) -> p g d", d=64)
                o_d = qkb[s][:, 0:1024].rearrange("p (g d) -> p g d", d=64)
                cd = ropd[:, 0, tb:tb + 1, :].to_broadcast([128, 16, 8])
                sd = ropd[:, 1, tb:tb + 1, :].to_broadcast([128, 16, 8])
                t = [rt[i][:, 0:128].rearrange("p (g d) -> p g d", d=8) for i in range(4)]
                tt(t[0], v_d[:, :, 0:8], cd, ALU.mult, QK32 + ["ropd"], [("rt", 0)])
                tt(t[1], v_d[:, :, 8:16], sd, ALU.mult, QK32 + ["ropd"], [("rt", 1)])
                tt(o_d[:, :, 0:8], t[0], t[1], ALU.subtract, [("rt", 0), ("rt", 1)], [("qkb", s, 0)])
                tt(t[2], v_d[:, :, 8:16], cd, ALU.mult, QK32 + ["ropd"], [("rt", 2)])
                tt(t[3], v_d[:, :, 0:8], sd, ALU.mult, QK32 + ["ropd"], [("rt", 3)])
                tt(o_d[:, :, 8:16], t[2], t[3], ALU.add, [("rt", 2), ("rt", 3)], [("qkb", s, 1)])
                act(o_d[:, :, 16:64], v_d[:, :, 16:64], AF.Copy, QK32, [("qkb", s, 2)])
                v_r = qk32[s][:, 1024:1536].rearrange("p (g d) -> p g d", d=64)
                o_r = qkb[s][:, 1024:1536].rearrange("p (g d) -> p g d", d=64)
                cr = ropr[:, 0, tb:tb + 1, :].to_broadcast([128, 8, 32])
                sr = ropr[:, 1, tb:tb + 1, :].to_broadcast([128, 8, 32])
                u = [rt[i][:, 0:256].rearrange("p (g d) -> p g d", d=32) for i in range(4)]
                tt(u[0], v_r[:, :, 0:32], cr, ALU.mult, QK32 + ["ropr"], [("rt", 0)])
                tt(u[1], v_r[:, :, 32:64], sr, ALU.mult, QK32 + ["ropr"], [("rt", 1)])
                tt(o_r[:, :, 0:32], u[0], u[1], ALU.subtract, [("rt", 0), ("rt", 1)], [("qkb", s, 3)])
                tt(u[2], v_r[:, :, 32:64], cr, ALU.mult, QK32 + ["ropr"], [("rt", 2)])
                tt(u[3], v_r[:, :, 0:32], sr, ALU.mult, QK32 + ["ropr"], [("rt", 3)])
                tt(o_r[:, :, 32:64], u[2], u[3], ALU.add, [("rt", 2), ("rt", 3)], [("qkb", s, 4)])
                QKB = [("qkb", s, i) for i in range(5)]
                for c in range(8):
                    tr(pQ1[:, c, :], qkb[s][:, c * 128:(c + 1) * 128], identb[:], QKB + ["identb"], ["pQ1"])
                for c in range(4):
                    tr(pQ2[:, c, :], qkb[s][:, (8 + c) * 128:(9 + c) * 128], identb[:], QKB + ["identb"], ["pQ2"])
                q4 = tb % 4
                act(qkTst[:, 0:8, q4 * 128:(q4 + 1) * 128], pQ1[:], AF.Copy, ["pQ1"], [("qkTst", q4, 0)])
                vcopy(qkTst[:, 8:12, q4 * 128:(q4 + 1) * 128], pQ2[:], ["pQ2"], [("qkTst", q4, 1)])
                dma(Vs[tb * 128:(tb + 1) * 128, :], vst[s][:], [("vst", s)], ["Vs"], "s_v%d" % s)
                dma(RVs[tb * 128:(tb + 1) * 128, :], rvst[s][:], [("rvst", s)], ["RVs"], "s_rv%d" % s)
                dma(RGs[tb * 128:(tb + 1) * 128, :], rgst[s][:], [("rgst", s)], ["RGs"], "s_rg%d" % s)
                if q4 == 3:
                    t4 = tb // 4
                    for c in range(12):
                        dma(QKT[c, :, t4 * 512:(t4 + 1) * 512], qkTst[:, c, :],
                            [("qkTst", a, b) for a in range(4) for b in range(2)], ["QKT"], "s_qkt")

            stageA(0)
            for tb in range(NTB):
                if tb + 1 < NTB:
                    stageA(tb + 1)
                stageB(tb)
        S.barrier()

        with ExitStack() as p1:
          if stage != "P0":
              qA = [sbt(p1, "qA%d" % i, [128, S_TOK], BF16) for i in range(2)]
              qB = [sbt(p1, "qB%d" % i, [128, S_TOK], BF16) for i in range(2)]
              kT = [sbt(p1, "kT%d" % i, [128, S_TOK], BF16) for i in range(2)]
              vv = [sbt(p1, "vv%d" % i, [128, NTB, 129], BF16) for i in range(2)]
              rgh = [sbt(p1, "rgh%d" % i, [128, NTB, 128], BF16) for i in range(2)]
              et = [sbt(p1, "et%d" % i, [128, 512], BF16) for i in range(3)]
              dlam = sbt(p1, "dlam", [128, 256], F32)
              rld = sbt(p1, "rld", [128, 8], F32)
              lg = sbt(p1, "lg", [128, 8], F32)
              rtab = sbt(p1, "rtab", [128, 10, 256], F32)
              e128 = sbt(p1, "e128", [128, 32], F32)
              Wm = sbt(p1, "Wm", [128, 4, 256], F32)
              wtmp = sbt(p1, "wtmp", [128, 2, 256], F32)
              pw = sbt(p1, "pw", [128, 2, 32], F32)
              oc = sbt(p1, "oc", [128, 2, 129], F32)
              rtmp = [sbt(p1, "rtmp%d" % i, [128, 256], F32) for i in range(2)]
              av = sbt(p1, "av", [128, 2, 128], F32)
              st1 = sbt(p1, "st1", [128, 32], F32)
              sT = [pst(p1, "sT%d" % i, [128, 512], F32) for i in range(4)]
              oacc = [pst(p1, "oacc%d" % i, [128, 512], F32) for i in range(4)]

              conv_eng = ["dve"]
              cgen = None
              if stage == "full":
                  cf1 = [sbt(p1, "cf1_%d" % i, [128, 2048], F32) for i in range(2)]
                  cb1 = [sbt(p1, "cb1_%d" % i, [128, 2048], BF16) for i in range(2)]
                  gffn1 = sbt(p1, "gffn1", [128, 8], F32)
                  dma(gffn1[:], gffn_d, (), ["gffn1"], "c_gf1")

                  def conv_gen():
                      j = 0
                      for k in range(8):
                          for cg in range(8):
                              s_ = j % 2
                              dma(cf1[s_][:], uT_d[k * 128:(k + 1) * 128, cg * 2048:(cg + 1) * 2048], (), [("cf1", s_)], "c_cf1%d" % s_)
                              if conv_eng[0] == "act":
                                  act(cb1[s_][:], cf1[s_][:], AF.Copy, [("cf1", s_), "gffn1"], [("cb1", s_)], scale=gffn1[:, k:k + 1])
                              else:
                                  ts(cb1[s_][:], cf1[s_][:], gffn1[:, k:k + 1], ALU.mult, [("cf1", s_), "gffn1"], [("cb1", s_)])
                              for hf in range(2):
                                  dma(UT2[cg * 16 + hf * 8:cg * 16 + (hf + 1) * 8, :, k, :].rearrange("c p e -> p c e"),
                                      cb1[s_][:, hf * 1024:(hf + 1) * 1024].rearrange("p (c e) -> p c e", e=128),
                                      [("cb1", s_)], ["UT2"], "s_cb1%d" % s_)
                              j += 1
                              yield
                      for r in range(64):
                          s_ = j % 2
                          dma(cf1[s_][:].rearrange("p (a d) -> p a d", d=D),
                              pv_d[r * 256:(r + 1) * 256, :].rearrange("(a p) d -> p a d", p=128), (), [("cf1", s_)], "c_cf1%d" % s_)
                          if conv_eng[0] == "act":
                              act(cb1[s_][:], cf1[s_][:], AF.Copy, [("cf1", s_)], [("cb1", s_)])
                          else:
                              vcopy(cb1[s_][:], cf1[s_][:], [("cf1", s_)], [("cb1", s_)])
                          dma(Vb[r * 256:(r + 1) * 256, :].rearrange("(a p) d -> p a d", p=128),
                              cb1[s_][:].rearrange("p (a d) -> p a d", d=D), [("cb1", s_)], ["Vb"], "s_cb1%d" % s_)
                          j += 1
                          yield

                  cgen = conv_gen()
              dma(dlam[:], dlam_d, (), ["dlam"], "c_dl")
              dma(rld[:], rld_d, (), ["rld"], "c_rl")
              dma(rtab[:], rtab_d, (), ["rtab"], "c_rt")
              dma(e128[:], e128_d, (), ["e128"], "c_e1")
              for i in range(2):
                  memset(vv[i][:, :, 128:129], 1.0, [("vv", i)])
                  memset(qA[i][64:128, :], 0.0, [("qT", i)], eng="pool")
                  memset(qB[i][0:64, :], 0.0, [("qT", i)], eng="pool")
              ttr(junk[:, 0:64], dlam[:, 0:64], dlam[:, 64:128], st1[:, 0:1], ["dlam"], ["l1", "junk"])
              ttr(junk[:, 0:64], dlam[:, 128:192], dlam[:, 192:256], st1[:, 1:2], ["dlam"], ["l2", "junk"])
              act(st1[:, 2:4], st1[:, 0:2], AF.Exp, ["l1", "l2"], ["l12e"])
              tt(st1[:, 4:5], st1[:, 3:4], st1[:, 2:3], ALU.subtract, ["l12e"], ["nl0"])
              ts(st1[:, 5:6], st1[:, 4:5], -LAMBDA_INIT, ALU.add, ["nl0"], ["neglam"])
              neglam = st1[:, 5:6]
              act(lg[:], rld[:], AF.Exp, ["rld"], ["lg0"])
              ts(lg[:], lg[:], -1.0, ALU.mult, ["lg0"], ["lg"])

              step = 0
              def emit_head_loads(hh):
                  is_diff = hh < 4
                  h = hh % 4
                  s = hh % 2
                  if is_diff:
                      dma(qA[s][0:64, :], QKT[h, 0:64, :], (), [("qT", s)], "l_q%d" % s)
                      dma(qB[s][64:128, :], QKT[h, 64:128, :], (), [("qT", s)], "l_q%d" % s)
                      dma(kT[s][:], QKT[4 + h], (), [("kT", s)], "l_k%d" % s)
                      for t8 in range(8):
                          dma(vv[s][:, t8 * 4:(t8 + 1) * 4, 0:128],
                              Vs.rearrange("(t p) c -> p t c", p=128)[:, t8 * 4:(t8 + 1) * 4, h * 128:(h + 1) * 128],
                              (), [("vv", s)], "l_v%d" % s)
                  else:
                      if h % 2 == 0:
                          dma(qA[s][0:64, :], QKT[8 + h // 2, 0:64, :], (), [("qT", s)], "l_q%d" % s)
                      else:
                          dma(qB[s][64:128, :], QKT[8 + h // 2, 64:128, :], (), [("qT", s)], "l_q%d" % s)
                      dma(kT[s][:], QKT[10 + h // 2], (), [("kT", s)], "l_k%d" % s)
                      for t8 in range(8):
                          dma(vv[s][:, t8 * 4:(t8 + 1) * 4, 0:128],
                              RVs.rearrange("(t p) c -> p t c", p=128)[:, t8 * 4:(t8 + 1) * 4, h * 128:(h + 1) * 128],
                              (), [("vv", s)], "l_v%d" % s)
                          dma(rgh[s][:, t8 * 4:(t8 + 1) * 4, :],
                              RGs.rearrange("(t p) c -> p t c", p=128)[:, t8 * 4:(t8 + 1) * 4, h * 128:(h + 1) * 128],
                              (), [("rgh", s)], "l_g%d" % s)

              HEADS = DBG["heads"]
              if HEADS:
                  emit_head_loads(HEADS[0])
              for hidx, hh in enumerate(HEADS):
                  is_diff = hh < 4
                  h = hh % 4
                  s = hh % 2
                  OPS = [("qT", s), ("kT", s), ("vv", s)]
                  if hidx + 1 < len(HEADS):
                      emit_head_loads(HEADS[hidx + 1])
                  if not is_diff:
                      pb = (h % 2) * 64
                      lgf = lg[:, h:h + 1]
                      lgb = lg[:, 4 + h:5 + h]
                      act(Wm[:, 0, :], rtab[:, 0, :], AF.Exp, ["rtab", "lg", "ln8_t"], [("Wm", 0)], scale=lgf, bias=ln8_t[:])
                      act(Wm[:, 1, :], rtab[:, 1, :], AF.Exp, ["rtab", "lg", "ln8_t"], [("Wm", 1)], scale=lgb, bias=ln8_t[:])
                      for r_ in range(2):
                          act(wtmp[:, 0, :], rtab[:, 2 + r_, :], AF.Exp, ["rtab", "lg", "ln8_t"], [("wtmp", 0)], scale=lgf, bias=ln8_t[:])
                          act(wtmp[:, 1, :], rtab[:, 4 + r_, :], AF.Exp, ["rtab", "lg", "ln8_t"], [("wtmp", 1)], scale=lgb, bias=ln8_t[:])
                          tt(wtmp[:, 0, :], wtmp[:, 0, :], rtab[:, 6 + r_, :], ALU.mult, [("wtmp", 0), "rtab"], [("wtmp", 0)])
                          tt(wtmp[:, 1, :], wtmp[:, 1, :], rtab[:, 8 + r_, :], ALU.mult, [("wtmp", 1), "rtab"], [("wtmp", 1)])
                          tt(Wm[:, 2 + r_, :], wtmp[:, 0, :], wtmp[:, 1, :], ALU.add, [("wtmp", 0), ("wtmp", 1)], [("Wm", 2 + r_)])
                      act(pw[:, 0, :], e128[:], AF.Exp, ["e128", "lg"], [("pw", 0)], scale=lgf)
                      act(pw[:, 1, :], e128[:], AF.Exp, ["e128", "lg"], [("pw", 1)], scale=lgb)
                  def emit_post(qb):
                      for qs in (range(2) if DBG["post"] else []):
                          tb = qb * 2 + qs
                          if is_diff:
                              vcopy(oc[:, 0, :], oacc[qs][:, 0:129], [("oacc", qs)], [("oc", 0)])
                              act(oc[:, 1, :], oacc[2 + qs][:, 0:129], AF.Copy, [("oacc", 2 + qs)], [("oc", 1)])
                              recip(st1[:, 8:10], oc[:, :, 128], [("oc", 0), ("oc", 1)], ["rs2"])
                              tt(st1[:, 10:11], st1[:, 9:10], neglam, ALU.mult, ["rs2", "neglam"], ["nl"])
                              ts(av[:, 0, :], oc[:, 0, 0:128], st1[:, 8:9], ALU.mult, [("oc", 0), "rs2"], [("av", 0)])
                              stt(av[:, 1, :], oc[:, 1, 0:128], st1[:, 10:11], av[:, 0, :], ALU.mult, ALU.add,
                                  [("oc", 1), "nl", ("av", 0)], [("av", 1)])
                              ttr(junk[:, 0:128], av[:, 1, :], av[:, 1, :], st1[:, 11:12], [("av", 1)], ["ssq", "junk"])
                              act(st1[:, 12:13], st1[:, 11:12], AF.Ln, ["ssq", "eps_t"], ["lnv"], scale=1.0 / 128, bias=eps_t[:])
                              act(st1[:, 13:14], st1[:, 12:13], AF.Exp, ["lnv"], ["rstd1"], scale=-0.5)
                              ts(mix[:, tb, h * 128:(h + 1) * 128], av[:, 1, :], st1[:, 13:14], ALU.mult,
                                 [("av", 1), "rstd1"], [("mix", tb, hh)])
                          else:
                              vcopy(oc[:, 0, 0:128], oacc[qs][:, 0:128], [("oacc", qs)], [("oc", 0)])
                              S.add("dve", lambda e: e.tensor_reduce(out=st1[:, 16:17], in_=oc[:, 0, 0:128], axis=AX.X, op=ALU.add),
                                    [("oc", 0)], ["rsum"])
                              ts(st1[:, 17:18], st1[:, 16:17], -1.0 / 128, ALU.mult, ["rsum"], ["nmean"])
                              ts(av[:, 0, :], oc[:, 0, 0:128], st1[:, 17:18], ALU.add, [("oc", 0), "nmean"], [("av", 0)])
                              ttr(junk[:, 0:128], av[:, 0, :], av[:, 0, :], st1[:, 18:19], [("av", 0)], ["ssq", "junk"])
                              act(st1[:, 19:20], st1[:, 18:19], AF.Ln, ["ssq", "eps_t"], ["lnv"], scale=1.0 / 128, bias=eps_t[:])
                              act(st1[:, 20:21], st1[:, 19:20], AF.Exp, ["lnv"], ["rstd1"], scale=-0.5)
                              stt(mix[:, tb, 512 + h * 128:512 + (h + 1) * 128], av[:, 0, :], st1[:, 20:21], rgh[s][:, tb, :],
                                  ALU.mult, ALU.mult, [("av", 0), "rstd1", ("rgh", s)], [("mix", tb, hh)])
                  def emit_qk(qb, kc, si):
                      qsl = slice(qb * 256, (qb + 1) * 256)
                      ksl = slice(kc * 128, (kc + 1) * 128)
                      if is_diff:
                          mm(sT[si][:, 0:256], kT[s][:, ksl], qA[s][:, qsl], True, True, OPS, [("sT", si)])
                          mm(sT[si][:, 256:512], kT[s][:, ksl], qB[s][:, qsl], True, True, OPS, [("sT", si)])
                      else:
                          mm(sT[si][:, 0:256], kT[s][:, ksl], (qA if h % 2 == 0 else qB)[s][:, qsl], True, True, OPS, [("sT", si)])
                  def emit_mid_av(qb, kc, si, ei):
                      if is_diff:
                          act(et[ei][:], sT[si][:], AF.Exp, [("sT", si)], [("et", ei)], scale=0.125)
                          for m in range(2):
                              for qs in range(2):
                                  a = m * 2 + qs
                                  mm(oacc[a][:, 0:129], et[ei][:, m * 256 + qs * 128:m * 256 + (qs + 1) * 128], vv[s][:, kc, :],
                                     kc == 0, kc == NTB - 1, [("et", ei)] + OPS, [("oacc", a)])
                      else:
                          nn = 2 * qb - kc
                          use_pool = (kc % 3 == 2) and DBG.get("retpool", False)
                          if (nn >= 1 or nn <= -2) and use_pool:
                              d_ = 0 if nn >= 1 else 1
                              pcol = pw[:, 0, nn:nn + 1] if nn >= 1 else pw[:, 1, -nn - 1:-nn]
                              tp_ = rtmp[kc % 2]
                              act(tp_[:], sT[si][:, 0:256], AF.Copy, [("sT", si), ("pw", d_)], [("rtmp", kc % 2)], scale=pcol)
                              tt(et[ei][:, 0:256], tp_[:], Wm[:, d_, :], ALU.mult, [("rtmp", kc % 2), ("Wm", d_)], [("et", ei)], eng="pool")
                          elif nn >= 1:
                              stt(et[ei][:, 0:256], sT[si][:, 0:256], pw[:, 0, nn:nn + 1], Wm[:, 0, :], ALU.mult, ALU.mult,
                                  [("sT", si), ("pw", 0), ("Wm", 0)], [("et", ei)])
                          elif nn <= -2:
                              stt(et[ei][:, 0:256], sT[si][:, 0:256], pw[:, 1, -nn - 1:-nn], Wm[:, 1, :], ALU.mult, ALU.mult,
                                  [("sT", si), ("pw", 1), ("Wm", 1)], [("et", ei)])
                          else:
                              r_ = kc - 2 * qb
                              tt(et[ei][:, 0:256], sT[si][:, 0:256], Wm[:, 2 + r_, :], ALU.mult,
                                 [("sT", si), ("Wm", 2 + r_)], [("et", ei)])
                          for qs in range(2):
                              mm(oacc[qs][:, 0:128], et[ei][:, qs * 128:(qs + 1) * 128], vv[s][:, kc, 0:128],
                                 kc == 0, kc == NTB - 1, [("et", ei)] + OPS, [("oacc", qs)])
                  steps = [(qb_, kc_) for qb_ in range(DBG["nqb"]) for kc_ in range(NTB)]
                  LA = 2
                  for j_ in range(min(LA, len(steps))):
                      emit_qk(steps[j_][0], steps[j_][1], (step + j_) % 4)
                  for n_, (qb_, kc_) in enumerate(steps):
                      if n_ + LA < len(steps):
                          emit_qk(steps[n_ + LA][0], steps[n_ + LA][1], (step + n_ + LA) % 4)
                      emit_mid_av(qb_, kc_, (step + n_) % 4, (step + n_) % 3)
                      if kc_ == NTB - 1:
                          emit_post(qb_)
                          conv_eng[0] = "dve" if is_diff else "act"
                          run_gen(cgen, 1)
                  if hh == DBG["heads"][-1]:
                      run_gen(cgen, 10 ** 6)
                  step += len(steps)
        S.barrier()

        with ExitStack() as p2:
            wo_bf = sbt(p2, "wo_bf", [128, 8, D], BF16)
            wst2 = [sbt(p2, "wst2_%d" % i, [128, D], F32) for i in range(2)]
            gmix = sbt(p2, "gmix", [128, 8], F32)
            mixT = sbt(p2, "mixT", [128, 8, 128], BF16)
            xt2 = [sbt(p2, "xt2_%d" % i, [128, D], F32) for i in range(2)]
            x2 = [sbt(p2, "x2_%d" % i, [128, D], F32) for i in range(2)]
            pM = pst(p2, "pM", [128, 8, 128], BF16)
            pX = [pst(p2, "pX%d" % i, [128, 512], F32) for i in range(2)]
            dma(gmix[:], gmix_d, (), ["gmix"], "c_gm")
            ts(gmix[:, 0:4], gmix[:, 0:4], 1.0 - LAMBDA_INIT, ALU.mult, ["gmix"], ["gmix"])
            for k in range(8):
                s = k % 2
                dma(wst2[s][:], w_out_d[k * 128:(k + 1) * 128, :], (), [("wst2", s)], "c_wo%d" % s)
                act(wo_bf[:, k, :], wst2[s][:], AF.Copy, [("wst2", s), "gmix"], [("wo_bf", k)], scale=gmix[:, k:k + 1])
            WO = [("wo_bf", k) for k in range(8)]
            for tb in range(NTB if stage in ("A", "full") else 0):
                s = tb % 2
                dma(xt2[s][:], x_d[tb * 128:(tb + 1) * 128, :], (), [("xt2", s)], "c_x2%d" % s)
                MIXK = [("mix", tb, hh) for hh in range(8)]
                for k in range(8):
                    tr(pM[:, k, :], mix[:, tb, k * 128:(k + 1) * 128], identb[:], MIXK + ["identb"], ["pM"])
                vcopy(mixT[:], pM[:], ["pM"], ["mixT"])
                for dh in range(2):
                    for k in range(8):
                        mm(pX[dh][:], mixT[:, k, :], wo_bf[:, k, dh * 512:(dh + 1) * 512], k == 0, k == 7,
                           ["mixT"] + WO, [("pX", dh)])
                    tt(x2[s][:, dh * 512:(dh + 1) * 512], pX[dh][:], xt2[s][:, dh * 512:(dh + 1) * 512], ALU.add,
                       [("pX", dh), ("xt2", s)], [("x2", s, dh)])
                if stage == "A":
                    dma(out_d[tb * 128:(tb + 1) * 128, :], x2[s][:], [("x2", s, 0), ("x2", s, 1)], ["out"], "s_o%d" % s)
                else:
                    dma(X2s[tb * 128:(tb + 1) * 128, :], x2[s][:], [("x2", s, 0), ("x2", s, 1)], ["X2s"], "s_o%d" % s)
        cm.close()
        if stage == "full":
            S.barrier()
            PEER_PHASE()
        if stage not in ("A", "full"):
            dma(out_d[0:128, 0:128], identf[:], ["identf"], ["out"], "s_o0")
        S.emit()
    return nc


def host_inputs(inputs, b, shared=None):
    if shared is None:
        shared = host_shared(inputs)
    f32 = np.float32
    x = np.ascontiguousarray(inputs["x"][b], dtype=f32)
    pos = np.arange(S_TOK, dtype=f32)
    rot_dim = 16
    rope_inv = np.power(f32(500000.0), -np.arange(rot_dim // 2, dtype=f32) * f32(2.0) / f32(rot_dim)).astype(f32)
    ret_inv = (f32(1.0) / np.power(f32(10000.0), np.linspace(0.0, 1.0, 32, dtype=f32))).astype(f32)

    def tab(inv):
        ang = (pos[:, None] * inv[None, :]).astype(f32)
        c = np.cos(ang).astype(f32).reshape(NTB, 128, -1).transpose(1, 0, 2)
        s_ = np.sin(ang).astype(f32).reshape(NTB, 128, -1).transpose(1, 0, 2)
        return np.ascontiguousarray(np.stack([c, s_], axis=1))

    jl = np.arange(128, dtype=f32)[:, None]
    xx = np.arange(256, dtype=f32)[None, :]
    rt = np.zeros((128, 10, 256), f32)
    rt[:, 0] = xx - jl
    rt[:, 1] = jl - xx + 128.0
    for r in range(2):
        dd = xx - 128.0 * r - jl
        rt[:, 2 + r] = np.maximum(dd, 0)
        rt[:, 4 + r] = np.maximum(-dd, 0)
        rt[:, 6 + r] = (dd >= 0).astype(f32)
        rt[:, 8 + r] = (dd < 0).astype(f32)
    gm = np.concatenate([np.tile(inputs["diff_norm_g"][0][:, None], (1, 4)), inputs["ret_norm_g"][0].T], axis=1)
    return {
        "x": x,
        "w_in": np.ascontiguousarray(inputs["w_in"][0], dtype=f32),
        "gattn": np.ascontiguousarray(inputs["attn_norm_g"][0].reshape(8, 128).T, dtype=f32),
        "ropd": tab(rope_inv),
        "ropr": tab(ret_inv),
        "dlam": np.ascontiguousarray(np.tile(inputs["diff_lambda"][0].reshape(1, 256), (128, 1)), dtype=f32),
        "rld": np.ascontiguousarray(np.tile(inputs["ret_log_decay"][0].reshape(1, 8), (128, 1)), dtype=f32),
        "rtab": rt,
        "e128": np.ascontiguousarray(np.tile((128.0 * np.arange(32, dtype=f32))[None, :], (128, 1))),
        "ident": np.eye(128, dtype=f32),
        "w_out": np.ascontiguousarray(inputs["w_out"][0], dtype=f32),
        "gmix": np.ascontiguousarray(gm, dtype=f32),
        "wq": np.ascontiguousarray(inputs["peer_w_query"][0], dtype=f32),
        "gffn": np.ascontiguousarray(inputs["ffn_norm_g"][0].reshape(8, 128).T, dtype=f32),
        "skT": np.ascontiguousarray(inputs["peer_sub_keys"][0].reshape(16, 128, 128).transpose(2, 0, 1), dtype=f32),
        "uT": shared["uT"],
        "pv": shared["pv"],
        "gfin": np.ascontiguousarray(np.tile(inputs["final_norm_g"].reshape(1, D), (128, 1)), dtype=f32),
        "iota": np.ascontiguousarray(np.tile(np.arange(128, dtype=f32)[None, :], (128, 1))),
        "io4": np.ascontiguousarray(np.tile(np.arange(16, dtype=f32)[None, :], (128, 128))),
    }


def host_shared(inputs):
    return {"uT": np.ascontiguousarray(np.asarray(inputs["peer_u"][0], dtype=np.float32).T),
            "pv": np.ascontiguousarray(inputs["peer_v"][0], dtype=np.float32)}


def kernel(**inputs):
    inputs = {k: np.asarray(v) for k, v in inputs.items()}
    nc = build("full")
    shared = host_shared(inputs)
    in_maps = [host_inputs(inputs, b, shared) for b in range(8)]
    res = run_bass_kernel_spmd(nc, in_maps, core_ids=list(range(8)))
    return np.stack([np.asarray(r["out"], dtype=np.float32) for r in res.results], axis=0)
```

```python
from contextlib import ExitStack
import math
import numpy as np
import concourse.bass as bass
import concourse.mybir as mybir
from concourse.bass_utils import run_bass_kernel_spmd

F32 = mybir.dt.float32
BF16 = mybir.dt.bfloat16
U32 = mybir.dt.uint32
AF = mybir.ActivationFunctionType
ALU = mybir.AluOpType
AX = mybir.AxisListType

ENGS = ("pe", "act", "dve", "pool", "sp")
S_TOK = 4096
D = 1024
NTB = S_TOK // 128
EPS = 1e-6
LAMBDA_INIT = 0.8 - 0.6 * math.exp(-0.3 * 0)
LN8 = math.log(0.125)
DBG = {"heads": list(range(8)), "nqb": 16, "post": True, "steps": True}


class Sched:
    def __init__(self, nc):
        self.nc = nc
        self.ops = []
        self.last_w = {}
        self.readers = {}
        self.chan_last = {}

    def add(self, eng, fn, reads=(), writes=(), chan=None):
        i = len(self.ops)
        deps = {}
        for k in reads:
            w = self.last_w.get(k)
            if w is not None:
                deps[w] = True
        for k in writes:
            w = self.last_w.get(k)
            if w is not None:
                deps.setdefault(w, False)
            for r in self.readers.get(k, ()):
                deps.setdefault(r, False)
        if chan is not None:
            p = self.chan_last.get(chan)
            if p is not None:
                deps[p] = True
            self.chan_last[chan] = i
        self.ops.append((eng, fn, deps, chan))
        for k in writes:
            self.last_w[k] = i
            self.readers[k] = []
        for k in reads:
            self.readers.setdefault(k, []).append(i)
        return i

    def barrier(self):
        self.ops.append(("BARRIER", None, {}, None))
        self.last_w.clear()
        self.readers.clear()
        self.chan_last.clear()

    def _skip(self, eng, chan, de, raw):
        return de == eng and chan is None and (eng == "pe" or not raw)

    def emit(self):
        nc = self.nc
        ops = self.ops
        n = len(ops)
        need = [False] * n
        for i, (eng, fn, deps, chan) in enumerate(ops):
            for d, raw in deps.items():
                de, _, _, dchan = ops[d]
                if dchan is not None:
                    continue
                if self._skip(eng, chan, de, raw):
                    continue
                need[d] = True
        cnt = {e: 0 for e in ENGS}
        chan_cnt = {}
        sig = [0] * n
        bar_snap = {}
        for i, (eng, fn, deps, chan) in enumerate(ops):
            if eng == "BARRIER":
                bar_snap[i] = dict(chan_cnt)
                continue
            if chan is not None:
                chan_cnt[chan] = chan_cnt.get(chan, 0) + 16
                sig[i] = chan_cnt[chan]
            elif need[i]:
                cnt[eng] += 1
                sig[i] = cnt[eng]
        with ExitStack() as es:
            engsem = {e: es.enter_context(nc.semaphore("sem_" + e)) for e in ENGS}
            bsem = es.enter_context(nc.semaphore("sem_bar"))
            chansem = {c: es.enter_context(nc.semaphore("dsem_%d" % j))
                       for j, c in enumerate(chan_cnt)}
            block = es.enter_context(nc.Block())

            def make(eng):
                def body(e):
                    waited = {}
                    nbar = 0
                    for i, (oeng, fn, deps, chan) in enumerate(ops):
                        if oeng == "BARRIER":
                            nbar += 1
                            if eng == "sp":
                                for c, v in bar_snap[i].items():
                                    if waited.get(("c", c), 0) < v:
                                        e.wait_ge(chansem[c], v)
                                        waited[("c", c)] = v
                            e.drain().then_inc(bsem, 1)
                            e.wait_ge(bsem, len(ENGS) * nbar)
                            continue
                        if oeng != eng:
                            continue
                        want = {}
                        for d, raw in deps.items():
                            de, _, _, dchan = ops[d]
                            if dchan is not None:
                                key = ("c", dchan)
                                s = chansem[dchan]
                            else:
                                if self._skip(eng, chan, de, raw):
                                    continue
                                key = ("e", de)
                                s = engsem[de]
                            v = sig[d]
                            if waited.get(key, 0) >= v:
                                continue
                            if key not in want or want[key][1] < v:
                                want[key] = (s, v)
                        for key, (s, v) in want.items():
                            e.wait_ge(s, v)
                            waited[key] = v
                        ins = fn(e)
                        if chan is not None:
                            ins.then_inc(chansem[chan], 16)
                        elif need[i]:
                            ins.then_inc(engsem[eng], 1)
                    if eng == "sp":
                        for c, v in chan_cnt.items():
                            if waited.get(("c", c), 0) < v:
                                e.wait_ge(chansem[c], v)
                return body

            block.tensor(make("pe"))
            block.scalar(make("act"))
            block.vector(make("dve"))
            block.gpsimd(make("pool"))
            block.sync(make("sp"))


def build(stage="full"):
    nc = bass.Bass("TRN2", target_bir_lowering=False)
    S = Sched(nc)

    def din(name, shape, dt=F32):
        return nc.dram_tensor(name, list(shape), dt, kind="ExternalInput").ap()

    x_d = din("x", [S_TOK, D])
    w_in_d = din("w_in", [D, 3072])
    gattn_d = din("gattn", [128, 8])
    ropd_d = din("ropd", [128, 2, NTB, 8])
    ropr_d = din("ropr", [128, 2, NTB, 32])
    dlam_d = din("dlam", [128, 256])
    rld_d = din("rld", [128, 8])
    rtab_d = din("rtab", [128, 10, 256])
    e128_d = din("e128", [128, 32])
    ident_d = din("ident", [128, 128])
    w_out_d = din("w_out", [D, D])
    gmix_d = din("gmix", [128, 8])
    wq_d = din("wq", [D, 2048])
    gffn_d = din("gffn", [128, 8])
    skT_d = din("skT", [128, 16, 128])
    uT_d = din("uT", [D, 16384])
    pv_d = din("pv", [16384, D])
    gfin_d = din("gfin", [128, D])
    iota_d = din("iota", [128, 128])
    io4_d = din("io4", [128, 2048])
    out_d = nc.dram_tensor("out", [S_TOK, D], F32, kind="ExternalOutput").ap()

    QKT = nc.dram_tensor("scr_qkt", [12, 128, S_TOK], BF16).ap()
    Vs = nc.dram_tensor("scr_v", [S_TOK, 512], BF16).ap()
    RVs = nc.dram_tensor("scr_rv", [S_TOK, 512], BF16).ap()
    RGs = nc.dram_tensor("scr_rg", [S_TOK, 512], BF16).ap()
    X2s = nc.dram_tensor("scr_x2", [S_TOK, D], F32).ap()
    UT2 = nc.dram_tensor("scr_ut2", [128, 128, 8, 128], BF16).ap()
    WQ2 = nc.dram_tensor("scr_wq2", [16, 128, 8, 128], BF16).ap()
    Vb = nc.dram_tensor("scr_vb", [16384, D], BF16).ap()

    def dma(out, in_, r, w, chan, eng="sp"):
        S.add(eng, lambda e: e.dma_start(out=out, in_=in_), r, w, chan=chan)

    def act(out, in_, func, r, w, scale=None, bias=None, accum=None):
        kw = {}
        if scale is not None:
            kw["scale"] = scale
        if bias is not None:
            kw["bias"] = bias
        if accum is not None:
            kw["accum_out"] = accum
        S.add("act", lambda e: e.activation(out=out, in_=in_, func=func, **kw), r, w)

    def vcopy(out, in_, r, w, eng="dve"):
        S.add(eng, lambda e: e.tensor_copy(out=out, in_=in_), r, w)

    def tt(out, in0, in1, op, r, w, eng="dve"):
        S.add(eng, lambda e: e.tensor_tensor(out=out, in0=in0, in1=in1, op=op), r, w)

    def ts(out, in0, s1, op0, r, w, s2=None, op1=None, eng="dve"):
        if op1 is None:
            S.add(eng, lambda e: e.tensor_scalar(out=out, in0=in0, scalar1=s1, scalar2=None, op0=op0), r, w)
        else:
            S.add(eng, lambda e: e.tensor_scalar(out=out, in0=in0, scalar1=s1, scalar2=s2, op0=op0, op1=op1), r, w)

    def stt(out, in0, scalar, in1, op0, op1, r, w):
        S.add("dve", lambda e: e.scalar_tensor_tensor(out=out, in0=in0, scalar=scalar, in1=in1, op0=op0, op1=op1), r, w)

    def ttr(out, in0, in1, accum, r, w):
        S.add("dve", lambda e: e.scalar_tensor_tensor(out=out, in0=in0, scalar=1.0, in1=in1, op0=ALU.mult,
                                                      op1=ALU.mult, accum_out=accum), r, w)

    def mm(out, lhsT, rhs, start, stop, r, w):
        S.add("pe", lambda e: e.matmul(out, lhsT=lhsT, rhs=rhs, start=start, stop=stop), r, w)

    def tr(out, in_, ident, r, w):
        S.add("pe", lambda e: e.transpose(out=out, in_=in_, identity=ident), r, w)

    def run_gen(gen, n):
        if gen is None:
            return
        for _ in range(n):
            try:
                next(gen)
            except StopIteration:
                return

    def recip(out, in_, r, w):
        S.add("dve", lambda e: e.reciprocal(out=out, in_=in_), r, w)

    def memset(ap, val, w, eng="dve"):
        S.add(eng, lambda e: e.memset(ap, val), (), w)

    with ExitStack() as top:
        def sbt(es, name, shape, dt):
            return es.enter_context(nc.sbuf_tensor("sb_" + name, list(shape), dt))

        def pst(es, name, shape, dt):
            return es.enter_context(nc.psum_tensor("ps_" + name, list(shape), dt))

        identf = sbt(top, "identf", [128, 128], F32)
        identb = sbt(top, "identb", [128, 128], BF16)
        eps_t = sbt(top, "eps_t", [128, 1], F32)
        ln8_t = sbt(top, "ln8_t", [128, 1], F32)
        junk = sbt(top, "junk", [128, D], BF16)
        small = sbt(top, "small", [128, 64], F32)
        dma(identf[:], ident_d, (), ["identf"], "c_id")
        vcopy(identb[:], identf[:], ["identf"], ["identb"])
        memset(eps_t[:], EPS, ["eps_t"])
        memset(ln8_t[:], LN8, ["ln8_t"])
        cm = ExitStack()
        mix = sbt(cm, "mix", [128, NTB, D], BF16)


        def PEER_PHASE():
          with ExitStack() as p3:
            gffn = sbt(p3, "gffn", [128, 8], F32)
            gfin = sbt(p3, "gfin", [128, D], BF16)
            skT = sbt(p3, "skT", [128, 16, 128], BF16)
            iota = sbt(p3, "iota", [128, 128], F32)
            io4 = sbt(p3, "io4", [128, 1024], BF16)
            dma(gffn[:], gffn_d, (), ["gffn"], "c_gf")
            dma(iota[:], iota_d, (), ["iota"], "c_io")
            with ExitStack() as pc:
                cf = [sbt(pc, "cf%d" % i, [128, 2048], F32) for i in range(2)]
                cb = [sbt(pc, "cb%d" % i, [128, 2048], BF16) for i in range(2)]
                j = 0

                def conv(s_, scale_ap, dst, even):
                    if scale_ap is None:
                        if even:
                            act(dst, cf[s_][:], AF.Copy, [("cf", s_)], [("cb", s_)])
                        else:
                            vcopy(dst, cf[s_][:], [("cf", s_)], [("cb", s_)])
                    elif even:
                        act(dst, cf[s_][:], AF.Copy, [("cf", s_), "gffn"], [("cb", s_)], scale=scale_ap)
                    else:
                        ts(dst, cf[s_][:], scale_ap, ALU.mult, [("cf", s_), "gffn"], [("cb", s_)])

                dma(cf[0][:, 0:D], gfin_d, (), [("cf", 0)], "c_cf0")
                vcopy(gfin[:], cf[0][:, 0:D], [("cf", 0)], ["gfin"])
                dma(cf[0][:], io4_d, (), [("cf", 0)], "c_cf0")
                vcopy(io4[:], cf[0][:, 0:1024], [("cf", 0)], ["io4"])
                dma(cf[1][:], skT_d.rearrange("p g n -> p (g n)"), (), [("cf", 1)], "c_cf1")
                vcopy(skT[:].rearrange("p g n -> p (g n)"), cf[1][:], [("cf", 1)], ["skT"])
                for k in range(8):
                    s_ = j % 2
                    dma(cf[s_][:], wq_d[k * 128:(k + 1) * 128, :], (), [("cf", s_)], "c_cf%d" % s_)
                    conv(s_, gffn[:, k:k + 1], cb[s_][:], j % 2 == 0)
                    dma(WQ2[:, :, k, :].rearrange("g p e -> p g e"), cb[s_][:].rearrange("p (g e) -> p g e", e=128),
                        [("cb", s_)], ["WQ2"], "s_cb%d" % s_)
                    j += 1
            S.barrier()
            with ExitStack() as pb:
                G = [sbt(pb, "G%d" % i, [128, 256, 128], BF16) for i in range(2)]
                NSL = 5
                ut8 = [sbt(pb, "ut8_%d" % i, [128, 8, 128], BF16) for i in range(NSL)]
                v8 = [sbt(pb, "v8_%d" % i, [128, D], BF16) for i in range(NSL)]
                wqp = [sbt(pb, "wqp%d" % i, [128, 8, 128], BF16) for i in range(2)]
                xnT = [sbt(pb, "xnT%d" % i, [128, 8, 256], BF16) for i in range(2)]
                x2s = sbt(pb, "x2s", [128, D], F32)
                xnb = sbt(pb, "xnb", [128, D], BF16)
                qTs = sbt(pb, "qTs", [128, 16, 128], BF16)
                buf1 = sbt(pb, "buf1", [128, 2048], F32)
                s2g = [sbt(pb, "s2g%d" % i, [128, 256], F32) for i in range(2)]
                eqt = sbt(pb, "eqt", [128, 1024], BF16)
                topv = sbt(pb, "topv", [128, 16, 16], F32)
                idxu = sbt(pb, "idxu", [128, 16, 16], U32)
                idxf = sbt(pb, "idxf", [128, 16, 16], F32)
                best = sbt(pb, "best", [128, 8, 16], F32)
                posu = sbt(pb, "posu", [128, 8, 16], U32)
                abu = sbt(pb, "abu", [128, 2, 128], U32)
                abf = sbt(pb, "abf", [128, 2, 128], F32)
                ijg = sbt(pb, "ijg", [128, 3, 128], F32)
                gsm = sbt(pb, "gsm", [128, 16], F32)
                ijgT = sbt(pb, "ijgT", [128, 3, 128], F32)
                At = [sbt(pb, "At%d" % i, [128, 128], BF16) for i in range(4)]
                Bt = [sbt(pb, "Bt%d" % i, [128, 128], BF16) for i in range(4)]
                ga = [sbt(pb, "ga%d" % i, [128, 256], BF16) for i in range(2)]
                gw = [sbt(pb, "gw%d" % i, [128, 256], BF16) for i in range(2)]
                st3 = sbt(pb, "st3", [128, 8], F32)
                big = [pst(pb, "big%d" % i, [128, 512], F32) for i in range(4)]
                pa2 = [pst(pb, "pa%d" % i, [128, 512], F32) for i in range(2)]
                pp = [pst(pb, "ppx%d" % i, [128, 512], F32) for i in range(2)]
                io4v = io4[:].rearrange("p (h k a) -> p h k a", h=4, k=16)
                eq4 = eqt[:].rearrange("p (h k a) -> p h k a", h=4, k=16)
                cand4 = buf1[:].rearrange("p (h a b) -> p h a b", h=8, a=16)
                SK = [("s", b4) for b4 in range(4)]
                NBLK = DBG.get("nblk", 16)
                ppc = [0]

                def nextpp():
                    ppc[0] += 1
                    return ppc[0] % 2

                def prologue(blk):
                    gb = blk % 2
                    for sub in range(2):
                        tb = blk * 2 + sub
                        dma(x2s[:], X2s[tb * 128:(tb + 1) * 128, :], (), ["x2s"], "l_x2s", eng=DBG.get("pdma", "sp"))
                        ttr(junk[:], x2s[:], x2s[:], st3[:, 0:1], ["x2s"], ["ss3", "junk"])
                        act(st3[:, 1:2], st3[:, 0:1], AF.Sqrt, ["ss3", "eps_t"], ["rs3"], scale=1.0 / D, bias=eps_t[:])
                        recip(st3[:, 2:3], st3[:, 1:2], ["rs3"], ["rstd3"])
                        act(xnb[:], x2s[:], AF.Copy, ["x2s", "rstd3"], ["xnb"], scale=st3[:, 2:3])
                        q_ = nextpp()
                        pT3 = pp[q_][:].bitcast(BF16).rearrange("p (k t) -> p k t", k=8)
                        for k in range(8):
                            tr(pT3[:, k, :], xnb[:, k * 128:(k + 1) * 128], identb[:], ["xnb", "identb"], [("pp", q_)])
                        vcopy(xnT[gb][:, :, sub * 128:(sub + 1) * 128], pT3, [("pp", q_)], [("xnT", gb, sub)])
                        yield
                    XN = [("xnT", gb, 0), ("xnT", gb, 1)]
                    def sub_gen(sub):
                        tsl = slice(sub * 128, (sub + 1) * 128)
                        for g in range(16):
                            ws = g % 2
                            dma(wqp[ws][:], WQ2[g], (), [("wqp", ws)], "l_wq%d" % ws, eng=DBG.get("pdma", "sp"))
                            q_ = nextpp()
                            for k in range(8):
                                mm(pp[q_][:, 0:128], wqp[ws][:, k, :], xnT[gb][:, k, tsl], k == 0, k == 7,
                                   XN + [("wqp", ws)], [("pp", q_)])
                            act(qTs[:, g, :], pp[q_][:, 0:128], AF.Copy, [("pp", q_)], [("qTs", g)])
                            if g % 4 == 3:
                                yield ("proj_done" if g == 15 else "proj")
                        for b4 in range(4):
                            q_ = nextpp()
                            for g in range(b4 * 4, b4 * 4 + 4):
                                mm(pp[q_][:, (g % 4) * 128:(g % 4 + 1) * 128], qTs[:, g, :], skT[:, g, :], True, True,
                                   [("qTs", g), "skT"], [("pp", q_)])
                            act(buf1[:, b4 * 512:(b4 + 1) * 512], pp[q_][:], AF.Copy, [("pp", q_)], [("s", b4), "cand"])
                            yield ("scores_done" if b4 == 3 else "scores")
                        for g2 in range(8):
                            gs = (2 * g2, 2 * g2 + 1)
                            sgs = [buf1[:, g * 128:(g + 1) * 128] for g in gs]
                            kks = [("s", g // 4) for g in gs]
                            zs = [s2g[j_][:, 0:128] for j_ in range(2)]
                            zks = [("s2g", j_) for j_ in range(2)]
                            for j_, g in enumerate(gs):
                                S.add("dve", lambda e, g=g, sg=sgs[j_]: e.max(out=topv[:, g, 0:8], in_=sg), [kks[j_]], [("topv", g, 0)])
                            for j_, g in enumerate(gs):
                                S.add("dve", lambda e, g=g, sg=sgs[j_]: e.max_index(out=idxu[:, g, 0:8], in_max=topv[:, g, 0:8], in_values=sg),
                                      [kks[j_], ("topv", g, 0)], [("idxu", g, 0)])
                            for j_, g in enumerate(gs):
                                S.add("dve", lambda e, g=g, sg=sgs[j_], z=zs[j_]: e.match_replace(out=z, in_to_replace=topv[:, g, 0:8], in_values=sg, imm_value=-1e30),
                                      [kks[j_], ("topv", g, 0)], [zks[j_]])
                            for j_, g in enumerate(gs):
                                S.add("dve", lambda e, g=g, z=zs[j_]: e.max(out=topv[:, g, 8:16], in_=z), [zks[j_]], [("topv", g, 1)])
                            for j_, g in enumerate(gs):
                                S.add("dve", lambda e, g=g, z=zs[j_]: e.max_index(out=idxu[:, g, 8:16], in_max=topv[:, g, 8:16], in_values=z),
                                      [zks[j_], ("topv", g, 1)], [("idxu", g, 1)])
                            yield ("stage1_done" if g2 == 7 else "stage1")
                        TOPV = [("topv", g, q) for g in range(16) for q in range(2)]
                        IDXU = [("idxu", g, q) for g in range(16) for q in range(2)]
                        vcopy(idxf[:], idxu[:], IDXU, ["idxf"])
                        tv = topv[:].rearrange("p (h q) a -> p h q a", q=2)
                        idf = idxf[:].rearrange("p (h q) a -> p h q a", q=2)
                        tt(cand4, tv[:, :, 0, :].unsqueeze(3).to_broadcast([128, 8, 16, 16]),
                           tv[:, :, 1, :].unsqueeze(2).to_broadcast([128, 8, 16, 16]), ALU.add, TOPV, ["cand"] + SK)
                        yield "x"
                        for h2 in range(4):
                            hs_ = (2 * h2, 2 * h2 + 1)
                            chs = [buf1[:, h * 256:(h + 1) * 256] for h in hs_]
                            zs = [s2g[j_][:, 0:256] for j_ in range(2)]
                            zks = [("s2g", j_) for j_ in range(2)]
                            for j_, h in enumerate(hs_):
                                S.add("dve", lambda e, h=h, ch=chs[j_]: e.max(out=best[:, h, 0:8], in_=ch), ["cand"], [("best", h, 0)])
                            for j_, h in enumerate(hs_):
                                S.add("dve", lambda e, h=h, ch=chs[j_]: e.max_index(out=posu[:, h, 0:8], in_max=best[:, h, 0:8], in_values=ch),
                                      ["cand", ("best", h, 0)], [("posu", h, 0)])
                            for j_, h in enumerate(hs_):
                                S.add("dve", lambda e, h=h, ch=chs[j_], z=zs[j_]: e.match_replace(out=z, in_to_replace=best[:, h, 0:8], in_values=ch, imm_value=-1e30),
                                      ["cand", ("best", h, 0)], [zks[j_]])
                            for j_, h in enumerate(hs_):
                                S.add("dve", lambda e, h=h, z=zs[j_]: e.max(out=best[:, h, 8:16], in_=z), [zks[j_]], [("best", h, 1)])
                            for j_, h in enumerate(hs_):
                                S.add("dve", lambda e, h=h, z=zs[j_]: e.max_index(out=posu[:, h, 8:16], in_max=best[:, h, 8:16], in_values=z),
                                      [zks[j_], ("best", h, 1)], [("posu", h, 1)])
                            yield ("stage2_done" if h2 == 3 else "stage2")
                        BEST = [("best", h, q) for h in range(8) for q in range(2)]
                        POSU = [("posu", h, q) for h in range(8) for q in range(2)]
                        posf = posu[:].rearrange("p h k -> p (h k)")
                        ts(abu[:, 0, :], posf, 4, ALU.arith_shift_right, POSU, ["abu0"])
                        ts(abu[:, 1, :], posf, 15, ALU.bitwise_and, POSU, ["abu1"])
                        vcopy(abf[:], abu[:], ["abu0", "abu1"], ["abf"])
                        for q in range(2):
                            for hf in range(2):
                                hs = slice(hf * 4, hf * 4 + 4)
                                a_b = abf[:, q, :].rearrange("p (h k) -> p h k", h=8)[:, hs, :].unsqueeze(3).to_broadcast([128, 4, 16, 16])
                                tt(eq4, a_b, io4v, ALU.is_equal, ["abf", "io4"], ["eqt"])
                                tt(eq4, eq4, idf[:, hs, q, :].unsqueeze(2).to_broadcast([128, 4, 16, 16]), ALU.mult, ["eqt", "idxf"], ["eqt"])
                                S.add("dve", lambda e, q=q, hs=hs: e.tensor_reduce(
                                    out=ijg[:, q, :].rearrange("p (h k) -> p h k", h=8)[:, hs, :], in_=eq4,
                                    axis=AX.X, op=ALU.add), ["eqt"], [("ijg", q)])
                            yield "x"
                        g3 = ijg[:, 2, :].rearrange("p (h k) -> p h k", h=8)
                        tt(g3, best[:], best[:, :, 0:1].to_broadcast([128, 8, 16]), ALU.subtract, BEST, [("ijg", 2)])
                        act(g3, g3, AF.Exp, [("ijg", 2)], [("ijg", 2)])
                        S.add("dve", lambda e, g3=g3: e.tensor_reduce(out=gsm[:, 0:8], in_=g3, axis=AX.X, op=ALU.add), [("ijg", 2)], ["gsm"])
                        recip(gsm[:, 8:16], gsm[:, 0:8], ["gsm"], ["grc"])
                        tt(g3, g3, gsm[:, 8:16].unsqueeze(2).to_broadcast([128, 8, 16]), ALU.mult, [("ijg", 2), "grc"], [("ijg", 2)])
                        for _e in range(DBG.get("eyield", 4)):
                            yield "decode"
                        yield "decode_done"
                        q_ = nextpp()
                        for q in range(3):
                            tr(pp[q_][:, q * 128:(q + 1) * 128], ijg[:, q, :], identf[:], [("ijg", q), "identf"], [("pp", q_)])
                        vcopy(ijgT[:], pp[q_][:, 0:384].rearrange("p (q t) -> p q t", q=3), [("pp", q_)], ["ijgT"])
                        yield "x"
                        for t in range(128):
                            u4 = t % 4
                            u8 = t % 4
                            if u4 == 0:
                                q_ = nextpp()
                            S.add("dve", lambda e, t=t, u8=u8, sub=sub: e.tensor_scalar(
                                out=At[u8][:], in0=iota[:], scalar1=ijgT[:, 0, t:t + 1], scalar2=ijgT[:, 2, t:t + 1],
                                op0=ALU.is_equal, op1=ALU.mult), ["ijgT", "iota"], [("At", u8)])
                            S.add("dve", lambda e, t=t, u8=u8, sub=sub: e.tensor_scalar(
                                out=Bt[u8][:], in0=iota[:], scalar1=ijgT[:, 1, t:t + 1], scalar2=None,
                                op0=ALU.is_equal), ["ijgT", "iota"], [("Bt", u8)])
                            mm(pp[q_][:, u4 * 128:(u4 + 1) * 128], Bt[u8][:], At[u8][:], True, True,
                               [("At", u8), ("Bt", u8)], [("pp", q_)])
                            if u4 == 3:
                                t0 = sub * 128 + t - 3
                                act(G[gb][:, t0:t0 + 4, :], pp[q_][:].rearrange("p (t i) -> p t i", i=128), AF.Copy,
                                    [("pp", q_)], [("G", gb)])
                                yield "x"

                    def drive(g, until):
                        for m in g:
                            yield
                            if m == until:
                                return

                    def inter(a, b):
                        da = db = False
                        while not (da and db):
                            if not da:
                                try:
                                    next(a)
                                except StopIteration:
                                    da = True
                            if not db:
                                try:
                                    next(b)
                                except StopIteration:
                                    db = True
                            yield

                    g0 = sub_gen(0)
                    g1 = sub_gen(1)
                    yield from drive(g0, "scores_done")
                    yield from inter(drive(g0, "stage1_done"), drive(g1, "proj_done"))
                    yield from drive(g0, "stage2_done")
                    yield from inter(drive(g0, "decode_done"), drive(g1, "scores_done"))
                    yield from drive(g0, None)
                    yield from drive(g1, None)

                def run(gen, n):
                    if gen is None:
                        return
                    for _ in range(n):
                        try:
                            next(gen)
                        except StopIteration:
                            return

                run(prologue(0), 10 ** 6)
                for blk in range(NBLK):
                    gb = blk % 2
                    XN = [("xnT", gb, 0), ("xnT", gb, 1)]
                    gen = prologue(blk + 1) if blk + 1 < NBLK else None
                    def emit_u(i):
                        sl = i % NSL
                        pg = i % 2
                        pah = pa2[pg][:, 0:256]
                        for k in range(8):
                            mm(pah, ut8[sl][:, k, :], xnT[gb][:, k, :], k == 0, k == 7,
                               XN + [("ut8", sl)], [("pa", pg)])

                    def emit_mid(i):
                        pg = i % 2
                        pah = pa2[pg][:, 0:256]
                        act(ga[pg][:], pah, AF.Gelu, [("pa", pg)], [("ga", pg)])
                        tt(gw[pg][:], ga[pg][:], G[gb][:, :, i], ALU.mult, [("ga", pg), ("G", gb)], [("gw", pg)], eng=DBG.get("gweng", "pool"))

                    def emit_v(i):
                        sl = i % NSL
                        pg = i % 2
                        for sub in range(2):
                            for dh in range(2):
                                mm(big[sub * 2 + dh][:], gw[pg][:, sub * 128:(sub + 1) * 128], v8[sl][:, dh * 512:(dh + 1) * 512],
                                   i == 0, i == 127, [("gw", pg), ("v8", sl)], [("big", sub * 2 + dh)])

                    def emit_load(i):
                        sl = i % NSL
                        dma(ut8[sl][:], UT2[i], (), [("ut8", sl)], "l_ut%d" % sl)
                        dma(v8[sl][:], Vb[i * 128:(i + 1) * 128, :], (), [("v8", sl)], "l_v8%d" % sl)

                    for i in range(NSL - 2):
                        emit_load(i)
                    emit_u(0)
                    for i in range(128):
                        if i + NSL - 2 < 128:
                            emit_load(i + NSL - 2)
                        if i + 1 < 128:
                            emit_u(i + 1)
                        if i == DBG.get("reload_at", 122):
                            xe_ = buf1[:].rearrange("p (a d) -> p a d", d=D)
                            for sub_ in range(2):
                                tb_ = blk * 2 + sub_
                                dma(xe_[:, sub_, :], X2s[tb_ * 128:(tb_ + 1) * 128, :], (), [("xe", sub_)] + SK + ["cand"], "l_xe%d" % sub_)
                        emit_mid(i)
                        if i >= 1:
                            emit_v(i - 1)
                        run(gen, 1)
                    emit_v(127)
                    xe = buf1[:].rearrange("p (a d) -> p a d", d=D)
                    for sub in range(2):
                        tb = blk * 2 + sub
                        for dh in range(2):
                            tt(xe[:, sub, dh * 512:(dh + 1) * 512], big[sub * 2 + dh][:], xe[:, sub, dh * 512:(dh + 1) * 512], ALU.add,
                               [("big", sub * 2 + dh), ("xe", sub)], [("xe", sub)])
                        ttr(junk[:], xe[:, sub, :], xe[:, sub, :], st3[:, 4:5], [("xe", sub)], ["ss4", "junk"])
                        act(st3[:, 5:6], st3[:, 4:5], AF.Sqrt, ["ss4", "eps_t"], ["rs4"], scale=1.0 / D, bias=eps_t[:])
                        recip(st3[:, 6:7], st3[:, 5:6], ["rs4"], ["rstd4"])
                        stt(xe[:, sub, :], xe[:, sub, :], st3[:, 6:7], gfin[:], ALU.mult, ALU.mult, [("xe", sub), "rstd4", "gfin"], [("xe", sub)])
                        dma(out_d[tb * 128:(tb + 1) * 128, :], xe[:, sub, :], [("xe", sub)], ["out"] + SK + ["cand"], "s_out%d" % sub)
                    run(gen, 10 ** 6)

        with ExitStack() as p0:
            w_bf = sbt(p0, "w_bf", [128, 8, 3072], BF16)
            wst = [sbt(p0, "wst%d" % i, [128, 1024], F32) for i in range(2)]
            gattn = sbt(p0, "gattn", [128, 8], F32)
            ropd = sbt(p0, "ropd", [128, 2, NTB, 8], F32)
            ropr = sbt(p0, "ropr", [128, 2, NTB, 32], F32)
            xt = [sbt(p0, "xt%d" % i, [128, D], F32) for i in range(2)]
            hb = [sbt(p0, "hb%d" % i, [128, D], BF16) for i in range(2)]
            hT = [sbt(p0, "hT%d" % i, [128, 8, 128], BF16) for i in range(2)]
            qk32 = [sbt(p0, "qk32_%d" % i, [128, 1536], F32) for i in range(2)]
            qkb = [sbt(p0, "qkb_%d" % i, [128, 1536], BF16) for i in range(2)]
            rt = [sbt(p0, "rt%d" % i, [128, 256], F32) for i in range(4)]
            qkTst2 = [sbt(p0, "qkTst%d" % i, [128, 12, 512], BF16) for i in range(2)]
            vst = [sbt(p0, "vst%d" % i, [128, 512], BF16) for i in range(2)]
            rvst = [sbt(p0, "rvst%d" % i, [128, 512], BF16) for i in range(2)]
            rgst = [sbt(p0, "rgst%d" % i, [128, 512], BF16) for i in range(2)]
            st0 = sbt(p0, "st0", [128, 8], F32)
            pT = [pst(p0, "pT%d" % i, [128, 8, 128], BF16) for i in range(2)]
            pp = [pst(p0, "pp%d" % i, [128, 512], F32) for i in range(2)]
            pQ1 = pst(p0, "pQ1", [128, 8, 128], BF16)
            pQ2 = pst(p0, "pQ2", [128, 4, 128], BF16)

            dma(gattn[:], gattn_d, (), ["gattn"], "c_g")
            dma(ropd[:], ropd_d, (), ["ropd"], "c_rd")
            dma(ropr[:], ropr_d, (), ["ropr"], "c_rr")
            j = 0
            for k in range(8):
                for c in range(3):
                    s = j % 2
                    dma(wst[s][:], w_in_d[k * 128:(k + 1) * 128, c * 1024:(c + 1) * 1024], (), [("wst", s)], "c_w%d" % s)
                    if j % 2 == 0:
                        act(w_bf[:, k, c * 1024:(c + 1) * 1024], wst[s][:], AF.Copy, [("wst", s), "gattn"], [("w_bf", k, c)],
                            scale=gattn[:, k:k + 1])
                    else:
                        ts(w_bf[:, k, c * 1024:(c + 1) * 1024], wst[s][:], gattn[:, k:k + 1], ALU.mult,
                           [("wst", s), "gattn"], [("w_bf", k, c)])
                    j += 1
            WALL = [("w_bf", k, c) for k in range(8) for c in range(3)]

            def stageA(tb):
                s = tb % 2
                dma(xt[s][:], x_d[tb * 128:(tb + 1) * 128, :], (), [("xt", s)], "c_x%d" % s)
                ss = st0[:, s:s + 1]
                rs = st0[:, 2 + s:3 + s]
                rstd = st0[:, 4 + s:5 + s]
                ttr(junk[:], xt[s][:], xt[s][:], ss, [("xt", s)], [("ss", s), "junk"])
                act(rs, ss, AF.Sqrt, [("ss", s), "eps_t"], [("rs", s)], scale=1.0 / D, bias=eps_t[:])
                recip(rstd, rs, [("rs", s)], [("rstd", s)])
                act(hb[s][:], xt[s][:], AF.Copy, [("xt", s), ("rstd", s)], [("hb", s)], scale=rstd)
                for k in range(8):
                    tr(pT[s][:, k, :], hb[s][:, k * 128:(k + 1) * 128], identb[:], [("hb", s), "identb"], [("pT", s)])
                vcopy(hT[s][:], pT[s][:], [("pT", s)], [("hT", s)])
                for cg in range(6):
                    ps = pp[cg % 2]
                    pk = ("pp", cg % 2)
                    for k in range(8):
                        mm(ps[:], hT[s][:, k, :], w_bf[:, k, cg * 512:(cg + 1) * 512], k == 0, k == 7,
                           [("hT", s)] + WALL, [pk])
                    if cg == 0:
                        act(qk32[s][:, 0:512], ps[:], AF.Copy, [pk], [("qk32", s, 0)])
                    elif cg == 1:
                        vcopy(qk32[s][:, 512:1024], ps[:], [pk], [("qk32", s, 1)])
                    elif cg == 2:
                        act(vst[s][:], ps[:], AF.Copy, [pk], [("vst", s)])
                    elif cg == 3:
                        vcopy(qk32[s][:, 1024:1536], ps[:], [pk], [("qk32", s, 2)])
                    elif cg == 4:
                        vcopy(rvst[s][:], ps[:], [pk], [("rvst", s)])
                    else:
                        act(rgst[s][:], ps[:], AF.Silu, [pk], [("rgst", s)])
            def stageB(tb):
                s = tb % 2
                QK32 = [("qk32", s, i) for i in range(3)]
                v_d = qk32[s][:, 0:1024].rearrange("p (g d) -> p g d", d=64)
                o_d = qkb[s][:, 0:1024].rearrange("p (g d) -> p g d", d=64)
                cd = ropd[:, 0, tb:tb + 1, :].to_broadcast([128, 16, 8])
                sd = ropd[:, 1, tb:tb + 1, :].to_broadcast([128, 16, 8])
                t = [rt[i][:, 0:128].rearrange("p (g d) -> p g d", d=8) for i in range(4)]
                tt(t[0], v_d[:, :, 0:8], cd, ALU.mult, QK32 + ["ropd"], [("rt", 0)])
                tt(t[1], v_d[:, :, 8:16], sd, ALU.mult, QK32 + ["ropd"], [("rt", 1)])
                tt(o_d[:, :, 0:8], t[0], t[1], ALU.subtract, [("rt", 0), ("rt", 1)], [("qkb", s, 0)])
                tt(t[2], v_d[:, :, 8:16], cd, ALU.mult, QK32 + ["ropd"], [("rt", 2)])
                tt(t[3], v_d[:, :, 0:8], sd, ALU.mult, QK32 + ["ropd"], [("rt", 3)])
                tt(o_d[:, :, 8:16], t[2], t[3], ALU.add, [("rt", 2), ("rt", 3)], [("qkb", s, 1)])
                act(o_d[:, :, 16:64], v_d[:, :, 16:64], AF.Copy, QK32, [("qkb", s, 2)])
                v_r = qk32[s][:, 1024:1536].rearrange("p (g d) -> p g d", d=64)
                o_r = qkb[s][:, 1024:1536].rearrange("p (g d) -> p g d", d=64)
                cr = ropr[:, 0, tb:tb + 1, :].to_broadcast([128, 8, 32])
                sr = ropr[:, 1, tb:tb + 1, :].to_broadcast([128, 8, 32])
                u = [rt[i][:, 0:256].rearrange("p (g d) -> p g d", d=32) for i in range(4)]
                tt(u[0], v_r[:, :, 0:32], cr, ALU.mult, QK32 + ["ropr"], [("rt", 0)])
                tt(u[1], v_r[:, :, 32:64], sr, ALU.mult, QK32 + ["ropr"], [("rt", 1)])
                tt(o_r[:, :, 0:32], u[0], u[1], ALU.subtract, [("rt", 0), ("rt", 1)], [("qkb", s, 3)])
                tt(u[2], v_r[:, :, 32:64], cr, ALU.mult, QK32 + ["ropr"], [("rt", 2)])
                tt(u[3], v_r[:, :, 0:32], sr, ALU.mult, QK32 + ["ropr"], [("rt", 3)])
                tt(o_r[:, :, 32:64], u[2], u[3], ALU.add, [("rt", 2), ("rt", 3)], [("qkb", s, 4)])
                QKB = [("qkb", s, i) for i in range(5)]
                for c in range(8):
                    tr(pQ1[:, c, :], qkb[s][:, c * 128:(c + 1) * 128], identb[:], QKB + ["identb"], ["pQ1"])
                for c in range(4):
                    tr(pQ2[:, c, :], qkb[s][:, (8 + c) * 128:(9 + c) * 128], identb[:], QKB + ["identb"], ["pQ2"])
                q4 = tb % 4
                qs_ = (tb // 4) % 2
                qkTst = qkTst2[qs_]
                act(qkTst[:, 0:8, q4 * 128:(q4 + 1) * 128], pQ1[:], AF.Copy, ["pQ1"], [("qkTst", qs_, q4, 0)])
                vcopy(qkTst[:, 8:12, q4 * 128:(q4 + 1) * 128], pQ2[:], ["pQ2"], [("qkTst", qs_, q4, 1)])
                dma(Vs[tb * 128:(tb + 1) * 128, :], vst[s][:], [("vst", s)], ["Vs"], "s_v%d" % s)
                dma(RVs[tb * 128:(tb + 1) * 128, :], rvst[s][:], [("rvst", s)], ["RVs"], "s_rv%d" % s)
                dma(RGs[tb * 128:(tb + 1) * 128, :], rgst[s][:], [("rgst", s)], ["RGs"], "s_rg%d" % s)
                if q4 == 3:
                    t4 = tb // 4
                    for c in range(12):
                        dma(QKT[c, :, t4 * 512:(t4 + 1) * 512], qkTst[:, c, :],
                            [("qkTst", qs_, a, b) for a in range(4) for b in range(2)], ["QKT"], "s_qkt%d_%d" % (qs_, c))

            stageA(0)
            for tb in range(NTB):
                if tb + 1 < NTB:
                    stageA(tb + 1)
                stageB(tb)
        S.barrier()

        with ExitStack() as p1:
          if stage != "P0":
              qA = [sbt(p1, "qA%d" % i, [128, S_TOK], BF16) for i in range(2)]
              qB = [sbt(p1, "qB%d" % i, [128, S_TOK], BF16) for i in range(2)]
              kT = [sbt(p1, "kT%d" % i, [128, S_TOK], BF16) for i in range(2)]
              vv = [sbt(p1, "vv%d" % i, [128, NTB, 129], BF16) for i in range(2)]
              rgh = [sbt(p1, "rgh%d" % i, [128, NTB, 128], BF16) for i in range(2)]
              et = [sbt(p1, "et%d" % i, [128, 512], BF16) for i in range(3)]
              dlam = sbt(p1, "dlam", [128, 256], F32)
              rld = sbt(p1, "rld", [128, 8], F32)
              lg = sbt(p1, "lg", [128, 8], F32)
              rtab = sbt(p1, "rtab", [128, 10, 256], F32)
              e128 = sbt(p1, "e128", [128, 32], F32)
              Wm = sbt(p1, "Wm", [128, 4, 256], F32)
              wtmp = sbt(p1, "wtmp", [128, 2, 256], F32)
              pw = sbt(p1, "pw", [128, 2, 32], F32)
              oc = sbt(p1, "oc", [128, 2, 129], F32)
              rtmp = [sbt(p1, "rtmp%d" % i, [128, 256], F32) for i in range(2)]
              av = sbt(p1, "av", [128, 2, 128], F32)
              st1 = sbt(p1, "st1", [128, 32], F32)
              sT = [pst(p1, "sT%d" % i, [128, 512], F32) for i in range(4)]
              oacc = [pst(p1, "oacc%d" % i, [128, 512], F32) for i in range(4)]

              conv_eng = ["dve"]
              cgen = None
              if stage == "full":
                  cf1 = [sbt(p1, "cf1_%d" % i, [128, 2048], F32) for i in range(2)]
                  cb1 = [sbt(p1, "cb1_%d" % i, [128, 2048], BF16) for i in range(2)]
                  gffn1 = sbt(p1, "gffn1", [128, 8], F32)
                  dma(gffn1[:], gffn_d, (), ["gffn1"], "c_gf1")

                  def conv_gen():
                      j = 0
                      for k in range(8):
                          for cg in range(8):
                              s_ = j % 2
                              dma(cf1[s_][:], uT_d[k * 128:(k + 1) * 128, cg * 2048:(cg + 1) * 2048], (), [("cf1", s_)], "c_cf1%d" % s_)
                              if conv_eng[0] == "act":
                                  act(cb1[s_][:], cf1[s_][:], AF.Copy, [("cf1", s_), "gffn1"], [("cb1", s_)], scale=gffn1[:, k:k + 1])
                              else:
                                  ts(cb1[s_][:], cf1[s_][:], gffn1[:, k:k + 1], ALU.mult, [("cf1", s_), "gffn1"], [("cb1", s_)])
                              for hf in range(2):
                                  dma(UT2[cg * 16 + hf * 8:cg * 16 + (hf + 1) * 8, :, k, :].rearrange("c p e -> p c e"),
                                      cb1[s_][:, hf * 1024:(hf + 1) * 1024].rearrange("p (c e) -> p c e", e=128),
                                      [("cb1", s_)], ["UT2"], "s_cb1%d" % s_)
                              j += 1
                              yield
                      for r in range(64):
                          s_ = j % 2
                          dma(cf1[s_][:].rearrange("p (a d) -> p a d", d=D),
                              pv_d[r * 256:(r + 1) * 256, :].rearrange("(a p) d -> p a d", p=128), (), [("cf1", s_)], "c_cf1%d" % s_)
                          if conv_eng[0] == "act":
                              act(cb1[s_][:], cf1[s_][:], AF.Copy, [("cf1", s_)], [("cb1", s_)])
                          else:
                              vcopy(cb1[s_][:], cf1[s_][:], [("cf1", s_)], [("cb1", s_)])
                          dma(Vb[r * 256:(r + 1) * 256, :].rearrange("(a p) d -> p a d", p=128),
                              cb1[s_][:].rearrange("p (a d) -> p a d", d=D), [("cb1", s_)], ["Vb"], "s_cb1%d" % s_)
                          j += 1
                          yield

                  cgen = conv_gen()
              dma(dlam[:], dlam_d, (), ["dlam"], "c_dl")
              dma(rld[:], rld_d, (), ["rld"], "c_rl")
              dma(rtab[:], rtab_d, (), ["rtab"], "c_rt")
              dma(e128[:], e128_d, (), ["e128"], "c_e1")
              for i in range(2):
                  memset(vv[i][:, :, 128:129], 1.0, [("vv", i)])
                  memset(qA[i][64:128, :], 0.0, [("qT", i)], eng="pool")
                  memset(qB[i][0:64, :], 0.0, [("qT", i)], eng="pool")
              ttr(junk[:, 0:64], dlam[:, 0:64], dlam[:, 64:128], st1[:, 0:1], ["dlam"], ["l1", "junk"])
              ttr(junk[:, 0:64], dlam[:, 128:192], dlam[:, 192:256], st1[:, 1:2], ["dlam"], ["l2", "junk"])
              act(st1[:, 2:4], st1[:, 0:2], AF.Exp, ["l1", "l2"], ["l12e"])
              tt(st1[:, 4:5], st1[:, 3:4], st1[:, 2:3], ALU.subtract, ["l12e"], ["nl0"])
              ts(st1[:, 5:6], st1[:, 4:5], -LAMBDA_INIT, ALU.add, ["nl0"], ["neglam"])
              neglam = st1[:, 5:6]
              act(lg[:], rld[:], AF.Exp, ["rld"], ["lg0"])
              ts(lg[:], lg[:], -1.0, ALU.mult, ["lg0"], ["lg"])

              step = 0
              def emit_head_loads(hh):
                  is_diff = hh < 4
                  h = hh % 4
                  s = hh % 2
                  if is_diff:
                      dma(qA[s][0:64, :], QKT[h, 0:64, :], (), [("qT", s)], "l_q%d" % s)
                      dma(qB[s][64:128, :], QKT[h, 64:128, :], (), [("qT", s)], "l_q%d" % s)
                      dma(kT[s][:], QKT[4 + h], (), [("kT", s)], "l_k%d" % s)
                      for t8 in range(8):
                          dma(vv[s][:, t8 * 4:(t8 + 1) * 4, 0:128],
                              Vs.rearrange("(t p) c -> p t c", p=128)[:, t8 * 4:(t8 + 1) * 4, h * 128:(h + 1) * 128],
                              (), [("vv", s)], "l_v%d" % s)
                  else:
                      if h % 2 == 0:
                          dma(qA[s][0:64, :], QKT[8 + h // 2, 0:64, :], (), [("qT", s)], "l_q%d" % s)
                      else:
                          dma(qB[s][64:128, :], QKT[8 + h // 2, 64:128, :], (), [("qT", s)], "l_q%d" % s)
                      dma(kT[s][:], QKT[10 + h // 2], (), [("kT", s)], "l_k%d" % s)
                      for t8 in range(8):
                          dma(vv[s][:, t8 * 4:(t8 + 1) * 4, 0:128],
                              RVs.rearrange("(t p) c -> p t c", p=128)[:, t8 * 4:(t8 + 1) * 4, h * 128:(h + 1) * 128],
                              (), [("vv", s)], "l_v%d" % s)
                          dma(rgh[s][:, t8 * 4:(t8 + 1) * 4, :],
                              RGs.rearrange("(t p) c -> p t c", p=128)[:, t8 * 4:(t8 + 1) * 4, h * 128:(h + 1) * 128],
                              (), [("rgh", s)], "l_g%d" % s)

              HEADS = DBG["heads"]
              if HEADS:
                  emit_head_loads(HEADS[0])
              for hidx, hh in enumerate(HEADS):
                  is_diff = hh < 4
                  h = hh % 4
                  s = hh % 2
                  OPS = [("qT", s), ("kT", s), ("vv", s)]
                  if hidx + 1 < len(HEADS):
                      emit_head_loads(HEADS[hidx + 1])
                  if not is_diff:
                      pb = (h % 2) * 64
                      lgf = lg[:, h:h + 1]
                      lgb = lg[:, 4 + h:5 + h]
                      act(Wm[:, 0, :], rtab[:, 0, :], AF.Exp, ["rtab", "lg", "ln8_t"], [("Wm", 0)], scale=lgf, bias=ln8_t[:])
                      act(Wm[:, 1, :], rtab[:, 1, :], AF.Exp, ["rtab", "lg", "ln8_t"], [("Wm", 1)], scale=lgb, bias=ln8_t[:])
                      for r_ in range(2):
                          act(wtmp[:, 0, :], rtab[:, 2 + r_, :], AF.Exp, ["rtab", "lg", "ln8_t"], [("wtmp", 0)], scale=lgf, bias=ln8_t[:])
                          act(wtmp[:, 1, :], rtab[:, 4 + r_, :], AF.Exp, ["rtab", "lg", "ln8_t"], [("wtmp", 1)], scale=lgb, bias=ln8_t[:])
                          tt(wtmp[:, 0, :], wtmp[:, 0, :], rtab[:, 6 + r_, :], ALU.mult, [("wtmp", 0), "rtab"], [("wtmp", 0)])
                          tt(wtmp[:, 1, :], wtmp[:, 1, :], rtab[:, 8 + r_, :], ALU.mult, [("wtmp", 1), "rtab"], [("wtmp", 1)])
                          tt(Wm[:, 2 + r_, :], wtmp[:, 0, :], wtmp[:, 1, :], ALU.add, [("wtmp", 0), ("wtmp", 1)], [("Wm", 2 + r_)])
                      act(pw[:, 0, :], e128[:], AF.Exp, ["e128", "lg"], [("pw", 0)], scale=lgf)
                      act(pw[:, 1, :], e128[:], AF.Exp, ["e128", "lg"], [("pw", 1)], scale=lgb)
                  def emit_post(qb):
                      for qs in (range(2) if DBG["post"] else []):
                          tb = qb * 2 + qs
                          if is_diff:
                              vcopy(oc[:, 0, :], oacc[qs][:, 0:129], [("oacc", qs)], [("oc", 0)])
                              act(oc[:, 1, :], oacc[2 + qs][:, 0:129], AF.Copy, [("oacc", 2 + qs)], [("oc", 1)])
                              recip(st1[:, 8:10], oc[:, :, 128], [("oc", 0), ("oc", 1)], ["rs2"])
                              tt(st1[:, 10:11], st1[:, 9:10], neglam, ALU.mult, ["rs2", "neglam"], ["nl"])
                              ts(av[:, 0, :], oc[:, 0, 0:128], st1[:, 8:9], ALU.mult, [("oc", 0), "rs2"], [("av", 0)])
                              stt(av[:, 1, :], oc[:, 1, 0:128], st1[:, 10:11], av[:, 0, :], ALU.mult, ALU.add,
                                  [("oc", 1), "nl", ("av", 0)], [("av", 1)])
                              ttr(junk[:, 0:128], av[:, 1, :], av[:, 1, :], st1[:, 11:12], [("av", 1)], ["ssq", "junk"])
                              act(st1[:, 12:13], st1[:, 11:12], AF.Ln, ["ssq", "eps_t"], ["lnv"], scale=1.0 / 128, bias=eps_t[:])
                              act(st1[:, 13:14], st1[:, 12:13], AF.Exp, ["lnv"], ["rstd1"], scale=-0.5)
                              ts(mix[:, tb, h * 128:(h + 1) * 128], av[:, 1, :], st1[:, 13:14], ALU.mult,
                                 [("av", 1), "rstd1"], [("mix", tb, hh)])
                          else:
                              vcopy(oc[:, 0, 0:128], oacc[qs][:, 0:128], [("oacc", qs)], [("oc", 0)])
                              S.add("dve", lambda e: e.tensor_reduce(out=st1[:, 16:17], in_=oc[:, 0, 0:128], axis=AX.X, op=ALU.add),
                                    [("oc", 0)], ["rsum"])
                              ts(st1[:, 17:18], st1[:, 16:17], -1.0 / 128, ALU.mult, ["rsum"], ["nmean"])
                              ts(av[:, 0, :], oc[:, 0, 0:128], st1[:, 17:18], ALU.add, [("oc", 0), "nmean"], [("av", 0)])
                              ttr(junk[:, 0:128], av[:, 0, :], av[:, 0, :], st1[:, 18:19], [("av", 0)], ["ssq", "junk"])
                              act(st1[:, 19:20], st1[:, 18:19], AF.Ln, ["ssq", "eps_t"], ["lnv"], scale=1.0 / 128, bias=eps_t[:])
                              act(st1[:, 20:21], st1[:, 19:20], AF.Exp, ["lnv"], ["rstd1"], scale=-0.5)
                              stt(mix[:, tb, 512 + h * 128:512 + (h + 1) * 128], av[:, 0, :], st1[:, 20:21], rgh[s][:, tb, :],
                                  ALU.mult, ALU.mult, [("av", 0), "rstd1", ("rgh", s)], [("mix", tb, hh)])
                  def emit_qk(qb, kc, si):
                      qsl = slice(qb * 256, (qb + 1) * 256)
                      ksl = slice(kc * 128, (kc + 1) * 128)
                      if is_diff:
                          mm(sT[si][:, 0:256], kT[s][:, ksl], qA[s][:, qsl], True, True, OPS, [("sT", si)])
                          mm(sT[si][:, 256:512], kT[s][:, ksl], qB[s][:, qsl], True, True, OPS, [("sT", si)])
                      else:
                          mm(sT[si][:, 0:256], kT[s][:, ksl], (qA if h % 2 == 0 else qB)[s][:, qsl], True, True, OPS, [("sT", si)])
                  def emit_mid_av(qb, kc, si, ei):
                      if is_diff:
                          act(et[ei][:], sT[si][:], AF.Exp, [("sT", si)], [("et", ei)], scale=0.125)
                          for m in range(2):
                              for qs in range(2):
                                  a = m * 2 + qs
                                  mm(oacc[a][:, 0:129], et[ei][:, m * 256 + qs * 128:m * 256 + (qs + 1) * 128], vv[s][:, kc, :],
                                     kc == 0, kc == NTB - 1, [("et", ei)] + OPS, [("oacc", a)])
                      else:
                          nn = 2 * qb - kc
                          use_pool = (kc % 3 == 2) and DBG.get("retpool", False)
                          if (nn >= 1 or nn <= -2) and use_pool:
                              d_ = 0 if nn >= 1 else 1
                              pcol = pw[:, 0, nn:nn + 1] if nn >= 1 else pw[:, 1, -nn - 1:-nn]
                              tp_ = rtmp[kc % 2]
                              act(tp_[:], sT[si][:, 0:256], AF.Copy, [("sT", si), ("pw", d_)], [("rtmp", kc % 2)], scale=pcol)
                              tt(et[ei][:, 0:256], tp_[:], Wm[:, d_, :], ALU.mult, [("rtmp", kc % 2), ("Wm", d_)], [("et", ei)], eng="pool")
                          elif nn >= 1:
                              stt(et[ei][:, 0:256], sT[si][:, 0:256], pw[:, 0, nn:nn + 1], Wm[:, 0, :], ALU.mult, ALU.mult,
                                  [("sT", si), ("pw", 0), ("Wm", 0)], [("et", ei)])
                          elif nn <= -2:
                              stt(et[ei][:, 0:256], sT[si][:, 0:256], pw[:, 1, -nn - 1:-nn], Wm[:, 1, :], ALU.mult, ALU.mult,
                                  [("sT", si), ("pw", 1), ("Wm", 1)], [("et", ei)])
                          else:
                              r_ = kc - 2 * qb
                              tt(et[ei][:, 0:256], sT[si][:, 0:256], Wm[:, 2 + r_, :], ALU.mult,
                                 [("sT", si), ("Wm", 2 + r_)], [("et", ei)])
                          for qs in range(2):
                              mm(oacc[qs][:, 0:128], et[ei][:, qs * 128:(qs + 1) * 128], vv[s][:, kc, 0:128],
                                 kc == 0, kc == NTB - 1, [("et", ei)] + OPS, [("oacc", qs)])
                  steps = [(qb_, kc_) for qb_ in range(DBG["nqb"]) for kc_ in range(NTB)]
                  LA = 2
                  for j_ in range(min(LA, len(steps))):
                      emit_qk(steps[j_][0], steps[j_][1], (step + j_) % 4)
                  for n_, (qb_, kc_) in enumerate(steps):
                      if n_ + LA < len(steps):
                          emit_qk(steps[n_ + LA][0], steps[n_ + LA][1], (step + n_ + LA) % 4)
                      emit_mid_av(qb_, kc_, (step + n_) % 4, (step + n_) % 3)
                      if kc_ == NTB - 1:
                          emit_post(qb_)
                          conv_eng[0] = "dve" if is_diff else "act"
                          run_gen(cgen, 1)
                  if hh == DBG["heads"][-1]:
                      run_gen(cgen, 10 ** 6)
                  step += len(steps)
        S.barrier()

        with ExitStack() as p2:
            wo_bf = sbt(p2, "wo_bf", [128, 8, D], BF16)
            wst2 = [sbt(p2, "wst2_%d" % i, [128, D], F32) for i in range(2)]
            gmix = sbt(p2, "gmix", [128, 8], F32)
            mixT = sbt(p2, "mixT", [128, 8, 128], BF16)
            xt2 = [sbt(p2, "xt2_%d" % i, [128, D], F32) for i in range(2)]
            x2 = [sbt(p2, "x2_%d" % i, [128, D], F32) for i in range(2)]
            pM = pst(p2, "pM", [128, 8, 128], BF16)
            pX = [pst(p2, "pX%d" % i, [128, 512], F32) for i in range(2)]
            dma(gmix[:], gmix_d, (), ["gmix"], "c_gm")
            ts(gmix[:, 0:4], gmix[:, 0:4], 1.0 - LAMBDA_INIT, ALU.mult, ["gmix"], ["gmix"])
            for k in range(8):
                s = k % 2
                dma(wst2[s][:], w_out_d[k * 128:(k + 1) * 128, :], (), [("wst2", s)], "c_wo%d" % s)
                act(wo_bf[:, k, :], wst2[s][:], AF.Copy, [("wst2", s), "gmix"], [("wo_bf", k)], scale=gmix[:, k:k + 1])
            WO = [("wo_bf", k) for k in range(8)]
            for tb in range(NTB if stage in ("A", "full") else 0):
                s = tb % 2
                dma(xt2[s][:], x_d[tb * 128:(tb + 1) * 128, :], (), [("xt2", s)], "c_x2%d" % s)
                MIXK = [("mix", tb, hh) for hh in range(8)]
                for k in range(8):
                    tr(pM[:, k, :], mix[:, tb, k * 128:(k + 1) * 128], identb[:], MIXK + ["identb"], ["pM"])
                vcopy(mixT[:], pM[:], ["pM"], ["mixT"])
                for dh in range(2):
                    for k in range(8):
                        mm(pX[dh][:], mixT[:, k, :], wo_bf[:, k, dh * 512:(dh + 1) * 512], k == 0, k == 7,
                           ["mixT"] + WO, [("pX", dh)])
                    tt(x2[s][:, dh * 512:(dh + 1) * 512], pX[dh][:], xt2[s][:, dh * 512:(dh + 1) * 512], ALU.add,
                       [("pX", dh), ("xt2", s)], [("x2", s, dh)])
                if stage == "A":
                    dma(out_d[tb * 128:(tb + 1) * 128, :], x2[s][:], [("x2", s, 0), ("x2", s, 1)], ["out"], "s_o%d" % s)
                else:
                    dma(X2s[tb * 128:(tb + 1) * 128, :], x2[s][:], [("x2", s, 0), ("x2", s, 1)], ["X2s"], "s_o%d" % s)
        cm.close()
        if stage == "full":
            S.barrier()
            PEER_PHASE()
        if stage not in ("A", "full"):
            dma(out_d[0:128, 0:128], identf[:], ["identf"], ["out"], "s_o0")
        S.emit()
    return nc


def host_inputs(inputs, b, shared=None):
    if shared is None:
        shared = host_shared(inputs)
    f32 = np.float32
    x = np.ascontiguousarray(inputs["x"][b], dtype=f32)
    pos = np.arange(S_TOK, dtype=f32)
    rot_dim = 16
    rope_inv = np.power(f32(500000.0), -np.arange(rot_dim // 2, dtype=f32) * f32(2.0) / f32(rot_dim)).astype(f32)
    ret_inv = (f32(1.0) / np.power(f32(10000.0), np.linspace(0.0, 1.0, 32, dtype=f32))).astype(f32)

    def tab(inv):
        ang = (pos[:, None] * inv[None, :]).astype(f32)
        c = np.cos(ang).astype(f32).reshape(NTB, 128, -1).transpose(1, 0, 2)
        s_ = np.sin(ang).astype(f32).reshape(NTB, 128, -1).transpose(1, 0, 2)
        return np.ascontiguousarray(np.stack([c, s_], axis=1))

    jl = np.arange(128, dtype=f32)[:, None]
    xx = np.arange(256, dtype=f32)[None, :]
    rt = np.zeros((128, 10, 256), f32)
    rt[:, 0] = xx - jl
    rt[:, 1] = jl - xx + 128.0
    for r in range(2):
        dd = xx - 128.0 * r - jl
        rt[:, 2 + r] = np.maximum(dd, 0)
        rt[:, 4 + r] = np.maximum(-dd, 0)
        rt[:, 6 + r] = (dd >= 0).astype(f32)
        rt[:, 8 + r] = (dd < 0).astype(f32)
    gm = np.concatenate([np.tile(inputs["diff_norm_g"][0][:, None], (1, 4)), inputs["ret_norm_g"][0].T], axis=1)
    return {
        "x": x,
        "w_in": np.ascontiguousarray(inputs["w_in"][0], dtype=f32),
        "gattn": np.ascontiguousarray(inputs["attn_norm_g"][0].reshape(8, 128).T, dtype=f32),
        "ropd": tab(rope_inv),
        "ropr": tab(ret_inv),
        "dlam": np.ascontiguousarray(np.tile(inputs["diff_lambda"][0].reshape(1, 256), (128, 1)), dtype=f32),
        "rld": np.ascontiguousarray(np.tile(inputs["ret_log_decay"][0].reshape(1, 8), (128, 1)), dtype=f32),
        "rtab": rt,
        "e128": np.ascontiguousarray(np.tile((128.0 * np.arange(32, dtype=f32))[None, :], (128, 1))),
        "ident": np.eye(128, dtype=f32),
        "w_out": np.ascontiguousarray(inputs["w_out"][0], dtype=f32),
        "gmix": np.ascontiguousarray(gm, dtype=f32),
        "wq": np.ascontiguousarray(inputs["peer_w_query"][0], dtype=f32),
        "gffn": np.ascontiguousarray(inputs["ffn_norm_g"][0].reshape(8, 128).T, dtype=f32),
        "skT": np.ascontiguousarray(inputs["peer_sub_keys"][0].reshape(16, 128, 128).transpose(2, 0, 1), dtype=f32),
        "uT": shared["uT"],
        "pv": shared["pv"],
        "gfin": np.ascontiguousarray(np.tile(inputs["final_norm_g"].reshape(1, D), (128, 1)), dtype=f32),
        "iota": np.ascontiguousarray(np.tile(np.arange(128, dtype=f32)[None, :], (128, 1))),
        "io4": np.ascontiguousarray(np.tile(np.arange(16, dtype=f32)[None, :], (128, 128))),
    }


def host_shared(inputs):
    return {"uT": np.ascontiguousarray(np.asarray(inputs["peer_u"][0], dtype=np.float32).T),
            "pv": np.ascontiguousarray(inputs["peer_v"][0], dtype=np.float32)}


def kernel(**inputs):
    inputs = {k: np.asarray(v) for k, v in inputs.items()}
    nc = build("full")
    shared = host_shared(inputs)
    in_maps = [host_inputs(inputs, b, shared) for b in range(8)]
    res = run_bass_kernel_spmd(nc, in_maps, core_ids=list(range(8)))
    return np.stack([np.asarray(r["out"], dtype=np.float32) for r in res.results], axis=0)
```

```python
from contextlib import ExitStack
import math
import numpy as np
import concourse.bass as bass
import concourse.mybir as mybir
from concourse.bass_utils import run_bass_kernel_spmd

F32 = mybir.dt.float32
BF16 = mybir.dt.bfloat16
U32 = mybir.dt.uint32
AF = mybir.ActivationFunctionType
ALU = mybir.AluOpType
AX = mybir.AxisListType

ENGS = ("pe", "act", "dve", "pool", "sp")
S_TOK = 4096
D = 1024
NTB = S_TOK // 128
EPS = 1e-6
LAMBDA_INIT = 0.8 - 0.6 * math.exp(-0.3 * 0)
LN8 = math.log(0.125)
DBG = {"heads": list(range(8)), "nqb": 16, "post": True, "steps": True}


class Sched:
    def __init__(self, nc):
        self.nc = nc
        self.ops = []
        self.last_w = {}
        self.readers = {}
        self.chan_last = {}

    def add(self, eng, fn, reads=(), writes=(), chan=None):
        i = len(self.ops)
        deps = {}
        for k in reads:
            w = self.last_w.get(k)
            if w is not None:
                deps[w] = True
        for k in writes:
            w = self.last_w.get(k)
            if w is not None:
                deps.setdefault(w, False)
            for r in self.readers.get(k, ()):
                deps.setdefault(r, False)
        if chan is not None:
            p = self.chan_last.get(chan)
            if p is not None:
                deps[p] = True
            self.chan_last[chan] = i
        self.ops.append((eng, fn, deps, chan))
        for k in writes:
            self.last_w[k] = i
            self.readers[k] = []
        for k in reads:
            self.readers.setdefault(k, []).append(i)
        return i

    def barrier(self):
        self.ops.append(("BARRIER", None, {}, None))
        self.last_w.clear()
        self.readers.clear()
        self.chan_last.clear()

    def _skip(self, eng, chan, de, raw):
        return de == eng and chan is None and (eng == "pe" or not raw)

    def emit(self):
        nc = self.nc
        ops = self.ops
        n = len(ops)
        need = [False] * n
        for i, (eng, fn, deps, chan) in enumerate(ops):
            for d, raw in deps.items():
                de, _, _, dchan = ops[d]
                if dchan is not None:
                    continue
                if self._skip(eng, chan, de, raw):
                    continue
                need[d] = True
        cnt = {e: 0 for e in ENGS}
        chan_cnt = {}
        sig = [0] * n
        bar_snap = {}
        for i, (eng, fn, deps, chan) in enumerate(ops):
            if eng == "BARRIER":
                bar_snap[i] = dict(chan_cnt)
                continue
            if chan is not None:
                chan_cnt[chan] = chan_cnt.get(chan, 0) + 16
                sig[i] = chan_cnt[chan]
            elif need[i]:
                cnt[eng] += 1
                sig[i] = cnt[eng]
        with ExitStack() as es:
            engsem = {e: es.enter_context(nc.semaphore("sem_" + e)) for e in ENGS}
            bsem = es.enter_context(nc.semaphore("sem_bar"))
            chansem = {c: es.enter_context(nc.semaphore("dsem_%d" % j))
                       for j, c in enumerate(chan_cnt)}
            block = es.enter_context(nc.Block())

            def make(eng):
                def body(e):
                    waited = {}
                    nbar = 0
                    for i, (oeng, fn, deps, chan) in enumerate(ops):
                        if oeng == "BARRIER":
                            nbar += 1
                            if eng == "sp":
                                for c, v in bar_snap[i].items():
                                    if waited.get(("c", c), 0) < v:
                                        e.wait_ge(chansem[c], v)
                                        waited[("c", c)] = v
                            e.drain().then_inc(bsem, 1)
                            e.wait_ge(bsem, len(ENGS) * nbar)
                            continue
                        if oeng != eng:
                            continue
                        want = {}
                        for d, raw in deps.items():
                            de, _, _, dchan = ops[d]
                            if dchan is not None:
                                key = ("c", dchan)
                                s = chansem[dchan]
                            else:
                                if self._skip(eng, chan, de, raw):
                                    continue
                                key = ("e", de)
                                s = engsem[de]
                            v = sig[d]
                            if waited.get(key, 0) >= v:
                                continue
                            if key not in want or want[key][1] < v:
                                want[key] = (s, v)
                        for key, (s, v) in want.items():
                            e.wait_ge(s, v)
                            waited[key] = v
                        ins = fn(e)
                        if chan is not None:
                            ins.then_inc(chansem[chan], 16)
                        elif need[i]:
                            ins.then_inc(engsem[eng], 1)
                    if eng == "sp":
                        for c, v in chan_cnt.items():
                            if waited.get(("c", c), 0) < v:
                                e.wait_ge(chansem[c], v)
                return body

            block.tensor(make("pe"))
            block.scalar(make("act"))
            block.vector(make("dve"))
            block.gpsimd(make("pool"))
            block.sync(make("sp"))


def build(stage="full"):
    nc = bass.Bass("TRN2", target_bir_lowering=False)
    S = Sched(nc)

    def din(name, shape, dt=F32):
        return nc.dram_tensor(name, list(shape), dt, kind="ExternalInput").ap()

    x_d = din("x", [S_TOK, D])
    w_in_d = din("w_in", [D, 3072])
    gattn_d = din("gattn", [128, 8])
    ropd_d = din("ropd", [128, 2, NTB, 8])
    ropr_d = din("ropr", [128, 2, NTB, 32])
    dlam_d = din("dlam", [128, 256])
    rld_d = din("rld", [128, 8])
    rtab_d = din("rtab", [128, 10, 256])
    e128_d = din("e128", [128, 32])
    ident_d = din("ident", [128, 128])
    w_out_d = din("w_out", [D, D])
    gmix_d = din("gmix", [128, 8])
    wq_d = din("wq", [D, 2048])
    gffn_d = din("gffn", [128, 8])
    skT_d = din("skT", [128, 16, 128])
    uT_d = din("uT", [D, 16384])
    pv_d = din("pv", [16384, D])
    gfin_d = din("gfin", [128, D])
    iota_d = din("iota", [128, 128])
    io4_d = din("io4", [128, 2048])
    out_d = nc.dram_tensor("out", [S_TOK, D], F32, kind="ExternalOutput").ap()

    QKT = nc.dram_tensor("scr_qkt", [12, 128, S_TOK], BF16).ap()
    Vs = nc.dram_tensor("scr_v", [S_TOK, 512], BF16).ap()
    RVs = nc.dram_tensor("scr_rv", [S_TOK, 512], BF16).ap()
    RGs = nc.dram_tensor("scr_rg", [S_TOK, 512], BF16).ap()
    X2s = nc.dram_tensor("scr_x2", [S_TOK, D], F32).ap()
    UT2 = nc.dram_tensor("scr_ut2", [128, 128, 8, 128], BF16).ap()
    WQ2 = nc.dram_tensor("scr_wq2", [16, 128, 8, 128], BF16).ap()
    Vb = nc.dram_tensor("scr_vb", [16384, D], BF16).ap()

    def dma(out, in_, r, w, chan, eng="sp"):
        S.add(eng, lambda e: e.dma_start(out=out, in_=in_), r, w, chan=chan)

    def act(out, in_, func, r, w, scale=None, bias=None, accum=None):
        kw = {}
        if scale is not None:
            kw["scale"] = scale
        if bias is not None:
            kw["bias"] = bias
        if accum is not None:
            kw["accum_out"] = accum
        S.add("act", lambda e: e.activation(out=out, in_=in_, func=func, **kw), r, w)

    def vcopy(out, in_, r, w, eng="dve"):
        S.add(eng, lambda e: e.tensor_copy(out=out, in_=in_), r, w)

    def tt(out, in0, in1, op, r, w, eng="dve"):
        S.add(eng, lambda e: e.tensor_tensor(out=out, in0=in0, in1=in1, op=op), r, w)

    def ts(out, in0, s1, op0, r, w, s2=None, op1=None, eng="dve"):
        if op1 is None:
            S.add(eng, lambda e: e.tensor_scalar(out=out, in0=in0, scalar1=s1, scalar2=None, op0=op0), r, w)
        else:
            S.add(eng, lambda e: e.tensor_scalar(out=out, in0=in0, scalar1=s1, scalar2=s2, op0=op0, op1=op1), r, w)

    def stt(out, in0, scalar, in1, op0, op1, r, w):
        S.add("dve", lambda e: e.scalar_tensor_tensor(out=out, in0=in0, scalar=scalar, in1=in1, op0=op0, op1=op1), r, w)

    def ttr(out, in0, in1, accum, r, w):
        S.add("dve", lambda e: e.scalar_tensor_tensor(out=out, in0=in0, scalar=1.0, in1=in1, op0=ALU.mult,
                                                      op1=ALU.mult, accum_out=accum), r, w)

    def mm(out, lhsT, rhs, start, stop, r, w):
        S.add("pe", lambda e: e.matmul(out, lhsT=lhsT, rhs=rhs, start=start, stop=stop), r, w)

    def tr(out, in_, ident, r, w):
        S.add("pe", lambda e: e.transpose(out=out, in_=in_, identity=ident), r, w)

    def run_gen(gen, n):
        if gen is None:
            return
        for _ in range(n):
            try:
                next(gen)
            except StopIteration:
                return

    def recip(out, in_, r, w):
        S.add("dve", lambda e: e.reciprocal(out=out, in_=in_), r, w)

    def memset(ap, val, w, eng="dve"):
        S.add(eng, lambda e: e.memset(ap, val), (), w)

    with ExitStack() as top:
        def sbt(es, name, shape, dt):
            return es.enter_context(nc.sbuf_tensor("sb_" + name, list(shape), dt))

        def pst(es, name, shape, dt):
            return es.enter_context(nc.psum_tensor("ps_" + name, list(shape), dt))

        identf = sbt(top, "identf", [128, 128], F32)
        identb = sbt(top, "identb", [128, 128], BF16)
        eps_t = sbt(top, "eps_t", [128, 1], F32)
        ln8_t = sbt(top, "ln8_t", [128, 1], F32)
        junk = sbt(top, "junk", [128, D], BF16)
        small = sbt(top, "small", [128, 64], F32)
        dma(identf[:], ident_d, (), ["identf"], "c_id")
        vcopy(identb[:], identf[:], ["identf"], ["identb"])
        memset(eps_t[:], EPS, ["eps_t"])
        memset(ln8_t[:], LN8, ["ln8_t"])
        cm = ExitStack()
        mix = sbt(cm, "mix", [128, NTB, D], BF16)


        def PEER_PHASE():
          with ExitStack() as p3:
            gffn = sbt(p3, "gffn", [128, 8], F32)
            gfin = sbt(p3, "gfin", [128, D], BF16)
            skT = sbt(p3, "skT", [128, 16, 128], BF16)
            iota = sbt(p3, "iota", [128, 128], F32)
            io4 = sbt(p3, "io4", [128, 1024], BF16)
            dma(gffn[:], gffn_d, (), ["gffn"], "c_gf")
            dma(iota[:], iota_d, (), ["iota"], "c_io")
            with ExitStack() as pc:
                cf = [sbt(pc, "cf%d" % i, [128, 2048], F32) for i in range(2)]
                cb = [sbt(pc, "cb%d" % i, [128, 2048], BF16) for i in range(2)]
                j = 0

                def conv(s_, scale_ap, dst, even):
                    if scale_ap is None:
                        if even:
                            act(dst, cf[s_][:], AF.Copy, [("cf", s_)], [("cb", s_)])
                        else:
                            vcopy(dst, cf[s_][:], [("cf", s_)], [("cb", s_)])
                    elif even:
                        act(dst, cf[s_][:], AF.Copy, [("cf", s_), "gffn"], [("cb", s_)], scale=scale_ap)
                    else:
                        ts(dst, cf[s_][:], scale_ap, ALU.mult, [("cf", s_), "gffn"], [("cb", s_)])

                dma(cf[0][:, 0:D], gfin_d, (), [("cf", 0)], "c_cf0")
                vcopy(gfin[:], cf[0][:, 0:D], [("cf", 0)], ["gfin"])
                dma(cf[0][:], io4_d, (), [("cf", 0)], "c_cf0")
                vcopy(io4[:], cf[0][:, 0:1024], [("cf", 0)], ["io4"])
                dma(cf[1][:], skT_d.rearrange("p g n -> p (g n)"), (), [("cf", 1)], "c_cf1")
                vcopy(skT[:].rearrange("p g n -> p (g n)"), cf[1][:], [("cf", 1)], ["skT"])
                for k in range(8):
                    s_ = j % 2
                    dma(cf[s_][:], wq_d[k * 128:(k + 1) * 128, :], (), [("cf", s_)], "c_cf%d" % s_)
                    conv(s_, gffn[:, k:k + 1], cb[s_][:], j % 2 == 0)
                    dma(WQ2[:, :, k, :].rearrange("g p e -> p g e"), cb[s_][:].rearrange("p (g e) -> p g e", e=128),
                        [("cb", s_)], (), "s_cb%d" % s_)
                    j += 1
            S.barrier()
            with ExitStack() as pb:
                G = [sbt(pb, "G%d" % i, [128, 256, 128], BF16) for i in range(2)]
                NSL = 5
                ut8 = [sbt(pb, "ut8_%d" % i, [128, 8, 128], BF16) for i in range(NSL)]
                v8 = [sbt(pb, "v8_%d" % i, [128, D], BF16) for i in range(NSL)]
                wqp = [sbt(pb, "wqp%d" % i, [128, 8, 128], BF16) for i in range(2)]
                xnT = [sbt(pb, "xnT%d" % i, [128, 8, 256], BF16) for i in range(2)]
                x2s = sbt(pb, "x2s", [128, D], F32)
                xnb = sbt(pb, "xnb", [128, D], BF16)
                qTs = sbt(pb, "qTs", [128, 16, 128], BF16)
                buf1 = sbt(pb, "buf1", [128, 2048], F32)
                s2g = [sbt(pb, "s2g%d" % i, [128, 256], F32) for i in range(2)]
                eqt = sbt(pb, "eqt", [128, 1024], BF16)
                topv = sbt(pb, "topv", [128, 16, 16], F32)
                idxu = sbt(pb, "idxu", [128, 16, 16], U32)
                idxf = sbt(pb, "idxf", [128, 16, 16], F32)
                best = sbt(pb, "best", [128, 8, 16], F32)
                posu = sbt(pb, "posu", [128, 8, 16], U32)
                abu = sbt(pb, "abu", [128, 2, 128], U32)
                abf = sbt(pb, "abf", [128, 2, 128], F32)
                ijg = sbt(pb, "ijg", [128, 3, 128], F32)
                gsm = sbt(pb, "gsm", [128, 16], F32)
                ijgT = sbt(pb, "ijgT", [128, 3, 128], F32)
                At = [sbt(pb, "At%d" % i, [128, 128], BF16) for i in range(4)]
                Bt = [sbt(pb, "Bt%d" % i, [128, 128], BF16) for i in range(4)]
                ga = [sbt(pb, "ga%d" % i, [128, 256], BF16) for i in range(2)]
                gw = [sbt(pb, "gw%d" % i, [128, 256], BF16) for i in range(2)]
                st3 = sbt(pb, "st3", [128, 8], F32)
                big = [pst(pb, "big%d" % i, [128, 512], F32) for i in range(4)]
                pa2 = [pst(pb, "pa%d" % i, [128, 512], F32) for i in range(2)]
                pp = [pst(pb, "ppx%d" % i, [128, 512], F32) for i in range(2)]
                io4v = io4[:].rearrange("p (h k a) -> p h k a", h=4, k=16)
                eq4 = eqt[:].rearrange("p (h k a) -> p h k a", h=4, k=16)
                cand4 = buf1[:].rearrange("p (h a b) -> p h a b", h=8, a=16)
                SK = [("s", b4) for b4 in range(4)]
                NBLK = DBG.get("nblk", 16)
                ppc = [0]

                def nextpp():
                    ppc[0] += 1
                    return ppc[0] % 2

                def prologue(blk):
                    gb = blk % 2
                    for sub in range(2):
                        tb = blk * 2 + sub
                        dma(x2s[:], X2s[tb * 128:(tb + 1) * 128, :], (), ["x2s"], "l_x2s", eng=DBG.get("pdma", "sp"))
                        ttr(junk[:], x2s[:], x2s[:], st3[:, 0:1], ["x2s"], ["ss3", "junk"])
                        act(st3[:, 1:2], st3[:, 0:1], AF.Sqrt, ["ss3", "eps_t"], ["rs3"], scale=1.0 / D, bias=eps_t[:])
                        recip(st3[:, 2:3], st3[:, 1:2], ["rs3"], ["rstd3"])
                        act(xnb[:], x2s[:], AF.Copy, ["x2s", "rstd3"], ["xnb"], scale=st3[:, 2:3])
                        q_ = nextpp()
                        pT3 = pp[q_][:].bitcast(BF16).rearrange("p (k t) -> p k t", k=8)
                        for k in range(8):
                            tr(pT3[:, k, :], xnb[:, k * 128:(k + 1) * 128], identb[:], ["xnb", "identb"], [("pp", q_)])
                        vcopy(xnT[gb][:, :, sub * 128:(sub + 1) * 128], pT3, [("pp", q_)], [("xnT", gb, sub)])
                        yield
                    XN = [("xnT", gb, 0), ("xnT", gb, 1)]
                    def sub_gen(sub):
                        tsl = slice(sub * 128, (sub + 1) * 128)
                        for g in range(16):
                            ws = g % 2
                            dma(wqp[ws][:], WQ2[g], (), [("wqp", ws)], "l_wq%d" % ws, eng=DBG.get("pdma", "sp"))
                            q_ = nextpp()
                            for k in range(8):
                                mm(pp[q_][:, 0:128], wqp[ws][:, k, :], xnT[gb][:, k, tsl], k == 0, k == 7,
                                   XN + [("wqp", ws)], [("pp", q_)])
                            act(qTs[:, g, :], pp[q_][:, 0:128], AF.Copy, [("pp", q_)], [("qTs", g)])
                            if g % 4 == 3:
                                yield ("proj_done" if g == 15 else "proj")
                        for b4 in range(4):
                            q_ = nextpp()
                            for g in range(b4 * 4, b4 * 4 + 4):
                                mm(pp[q_][:, (g % 4) * 128:(g % 4 + 1) * 128], qTs[:, g, :], skT[:, g, :], True, True,
                                   [("qTs", g), "skT"], [("pp", q_)])
                            act(buf1[:, b4 * 512:(b4 + 1) * 512], pp[q_][:], AF.Copy, [("pp", q_)], [("s", b4), "cand"])
                            yield ("scores_done" if b4 == 3 else "scores")
                        for g2 in range(8):
                            gs = (2 * g2, 2 * g2 + 1)
                            sgs = [buf1[:, g * 128:(g + 1) * 128] for g in gs]
                            kks = [("s", g // 4) for g in gs]
                            zs = [s2g[j_][:, 0:128] for j_ in range(2)]
                            zks = [("s2g", j_) for j_ in range(2)]
                            for j_, g in enumerate(gs):
                                S.add("dve", lambda e, g=g, sg=sgs[j_]: e.max(out=topv[:, g, 0:8], in_=sg), [kks[j_]], [("topv", g, 0)])
                            for j_, g in enumerate(gs):
                                S.add("dve", lambda e, g=g, sg=sgs[j_]: e.max_index(out=idxu[:, g, 0:8], in_max=topv[:, g, 0:8], in_values=sg),
                                      [kks[j_], ("topv", g, 0)], [("idxu", g, 0)])
                            for j_, g in enumerate(gs):
                                S.add("dve", lambda e, g=g, sg=sgs[j_], z=zs[j_]: e.match_replace(out=z, in_to_replace=topv[:, g, 0:8], in_values=sg, imm_value=-1e30),
                                      [kks[j_], ("topv", g, 0)], [zks[j_]])
                            for j_, g in enumerate(gs):
                                S.add("dve", lambda e, g=g, z=zs[j_]: e.max(out=topv[:, g, 8:16], in_=z), [zks[j_]], [("topv", g, 1)])
                            for j_, g in enumerate(gs):
                                S.add("dve", lambda e, g=g, z=zs[j_]: e.max_index(out=idxu[:, g, 8:16], in_max=topv[:, g, 8:16], in_values=z),
                                      [zks[j_], ("topv", g, 1)], [("idxu", g, 1)])
                            yield ("stage1_done" if g2 == 7 else "stage1")
                        TOPV = [("topv", g, q) for g in range(16) for q in range(2)]
                        IDXU = [("idxu", g, q) for g in range(16) for q in range(2)]
                        vcopy(idxf[:], idxu[:], IDXU, ["idxf"])
                        tv = topv[:].rearrange("p (h q) a -> p h q a", q=2)
                        idf = idxf[:].rearrange("p (h q) a -> p h q a", q=2)
                        tt(cand4, tv[:, :, 0, :].unsqueeze(3).to_broadcast([128, 8, 16, 16]),
                           tv[:, :, 1, :].unsqueeze(2).to_broadcast([128, 8, 16, 16]), ALU.add, TOPV, ["cand"] + SK)
                        yield "x"
                        for h2 in range(4):
                            hs_ = (2 * h2, 2 * h2 + 1)
                            chs = [buf1[:, h * 256:(h + 1) * 256] for h in hs_]
                            zs = [s2g[j_][:, 0:256] for j_ in range(2)]
                            zks = [("s2g", j_) for j_ in range(2)]
                            for j_, h in enumerate(hs_):
                                S.add("dve", lambda e, h=h, ch=chs[j_]: e.max(out=best[:, h, 0:8], in_=ch), ["cand"], [("best", h, 0)])
                            for j_, h in enumerate(hs_):
                                S.add("dve", lambda e, h=h, ch=chs[j_]: e.max_index(out=posu[:, h, 0:8], in_max=best[:, h, 0:8], in_values=ch),
                                      ["cand", ("best", h, 0)], [("posu", h, 0)])
                            for j_, h in enumerate(hs_):
                                S.add("dve", lambda e, h=h, ch=chs[j_], z=zs[j_]: e.match_replace(out=z, in_to_replace=best[:, h, 0:8], in_values=ch, imm_value=-1e30),
                                      ["cand", ("best", h, 0)], [zks[j_]])
                            for j_, h in enumerate(hs_):
                                S.add("dve", lambda e, h=h, z=zs[j_]: e.max(out=best[:, h, 8:16], in_=z), [zks[j_]], [("best", h, 1)])
                            for j_, h in enumerate(hs_):
                                S.add("dve", lambda e, h=h, z=zs[j_]: e.max_index(out=posu[:, h, 8:16], in_max=best[:, h, 8:16], in_values=z),
                                      [zks[j_], ("best", h, 1)], [("posu", h, 1)])
                            yield ("stage2_done" if h2 == 3 else "stage2")
                        BEST = [("best", h, q) for h in range(8) for q in range(2)]
                        POSU = [("posu", h, q) for h in range(8) for q in range(2)]
                        posf = posu[:].rearrange("p h k -> p (h k)")
                        ts(abu[:, 0, :], posf, 4, ALU.arith_shift_right, POSU, ["abu0"])
                        ts(abu[:, 1, :], posf, 15, ALU.bitwise_and, POSU, ["abu1"])
                        vcopy(abf[:], abu[:], ["abu0", "abu1"], ["abf"])
                        for q in range(2):
                            for hf in range(2):
                                hs = slice(hf * 4, hf * 4 + 4)
                                a_b = abf[:, q, :].rearrange("p (h k) -> p h k", h=8)[:, hs, :].unsqueeze(3).to_broadcast([128, 4, 16, 16])
                                tt(eq4, a_b, io4v, ALU.is_equal, ["abf", "io4"], ["eqt"])
                                tt(eq4, eq4, idf[:, hs, q, :].unsqueeze(2).to_broadcast([128, 4, 16, 16]), ALU.mult, ["eqt", "idxf"], ["eqt"])
                                S.add("dve", lambda e, q=q, hs=hs: e.tensor_reduce(
                                    out=ijg[:, q, :].rearrange("p (h k) -> p h k", h=8)[:, hs, :], in_=eq4,
                                    axis=AX.X, op=ALU.add), ["eqt"], [("ijg", q)])
                            yield "x"
                        g3 = ijg[:, 2, :].rearrange("p (h k) -> p h k", h=8)
                        tt(g3, best[:], best[:, :, 0:1].to_broadcast([128, 8, 16]), ALU.subtract, BEST, [("ijg", 2)])
                        act(g3, g3, AF.Exp, [("ijg", 2)], [("ijg", 2)])
                        S.add("dve", lambda e, g3=g3: e.tensor_reduce(out=gsm[:, 0:8], in_=g3, axis=AX.X, op=ALU.add), [("ijg", 2)], ["gsm"])
                        recip(gsm[:, 8:16], gsm[:, 0:8], ["gsm"], ["grc"])
                        tt(g3, g3, gsm[:, 8:16].unsqueeze(2).to_broadcast([128, 8, 16]), ALU.mult, [("ijg", 2), "grc"], [("ijg", 2)])
                        for _e in range(DBG.get("eyield", 4)):
                            yield "decode"
                        yield "decode_done"
                        q_ = nextpp()
                        for q in range(3):
                            tr(pp[q_][:, q * 128:(q + 1) * 128], ijg[:, q, :], identf[:], [("ijg", q), "identf"], [("pp", q_)])
                        vcopy(ijgT[:], pp[q_][:, 0:384].rearrange("p (q t) -> p q t", q=3), [("pp", q_)], ["ijgT"])
                        yield "x"
                        for t in range(128):
                            u4 = t % 4
                            u8 = t % 4
                            if u4 == 0:
                                q_ = nextpp()
                            S.add("dve", lambda e, t=t, u8=u8, sub=sub: e.tensor_scalar(
                                out=At[u8][:], in0=iota[:], scalar1=ijgT[:, 0, t:t + 1], scalar2=ijgT[:, 2, t:t + 1],
                                op0=ALU.is_equal, op1=ALU.mult), ["ijgT", "iota"], [("At", u8)])
                            S.add("dve", lambda e, t=t, u8=u8, sub=sub: e.tensor_scalar(
                                out=Bt[u8][:], in0=iota[:], scalar1=ijgT[:, 1, t:t + 1], scalar2=None,
                                op0=ALU.is_equal), ["ijgT", "iota"], [("Bt", u8)])
                            mm(pp[q_][:, u4 * 128:(u4 + 1) * 128], Bt[u8][:], At[u8][:], True, True,
                               [("At", u8), ("Bt", u8)], [("pp", q_)])
                            if u4 == 3:
                                t0 = sub * 128 + t - 3
                                act(G[gb][:, t0:t0 + 4, :], pp[q_][:].rearrange("p (t i) -> p t i", i=128), AF.Copy,
                                    [("pp", q_)], [("G", gb)])
                                yield "x"

                    def drive(g, until):
                        for m in g:
                            yield
                            if m == until:
                                return

                    def inter(a, b):
                        da = db = False
                        while not (da and db):
                            if not da:
                                try:
                                    next(a)
                                except StopIteration:
                                    da = True
                            if not db:
                                try:
                                    next(b)
                                except StopIteration:
                                    db = True
                            yield

                    g0 = sub_gen(0)
                    g1 = sub_gen(1)
                    yield from drive(g0, "scores_done")
                    yield from inter(drive(g0, "stage1_done"), drive(g1, "proj_done"))
                    yield from drive(g0, "stage2_done")
                    yield from inter(drive(g0, "decode_done"), drive(g1, "scores_done"))
                    yield from drive(g0, None)
                    yield from drive(g1, None)

                def run(gen, n):
                    if gen is None:
                        return
                    for _ in range(n):
                        try:
                            next(gen)
                        except StopIteration:
                            return

                run(prologue(0), 10 ** 6)
                for blk in range(NBLK):
                    gb = blk % 2
                    XN = [("xnT", gb, 0), ("xnT", gb, 1)]
                    gen = prologue(blk + 1) if blk + 1 < NBLK else None
                    def emit_u(i):
                        sl = i % NSL
                        pg = i % 2
                        pah = pa2[pg][:, 0:256]
                        for k in range(8):
                            mm(pah, ut8[sl][:, k, :], xnT[gb][:, k, :], k == 0, k == 7,
                               XN + [("ut8", sl)], [("pa", pg)])

                    def emit_mid(i):
                        pg = i % 2
                        pah = pa2[pg][:, 0:256]
                        act(ga[pg][:], pah, AF.Gelu, [("pa", pg)], [("ga", pg)])
                        tt(gw[pg][:], ga[pg][:], G[gb][:, :, i], ALU.mult, [("ga", pg), ("G", gb)], [("gw", pg)], eng=DBG.get("gweng", "pool"))

                    def emit_v(i):
                        sl = i % NSL
                        pg = i % 2
                        for sub in range(2):
                            for dh in range(2):
                                mm(big[sub * 2 + dh][:], gw[pg][:, sub * 128:(sub + 1) * 128], v8[sl][:, dh * 512:(dh + 1) * 512],
                                   i == 0, i == 127, [("gw", pg), ("v8", sl)], [("big", sub * 2 + dh)])

                    def emit_load(i):
                        sl = i % NSL
                        dma(ut8[sl][:], UT2[i], (), [("ut8", sl)], "l_ut%d" % sl)
                        dma(v8[sl][:], Vb[i * 128:(i + 1) * 128, :], (), [("v8", sl)], "l_v8%d" % sl)

                    for i in range(NSL - 2):
                        emit_load(i)
                    emit_u(0)
                    for i in range(128):
                        if i + NSL - 2 < 128:
                            emit_load(i + NSL - 2)
                        if i + 1 < 128:
                            emit_u(i + 1)
                        if i == DBG.get("reload_at", 122):
                            xe_ = buf1[:].rearrange("p (a d) -> p a d", d=D)
                            for sub_ in range(2):
                                tb_ = blk * 2 + sub_
                                dma(xe_[:, sub_, :], X2s[tb_ * 128:(tb_ + 1) * 128, :], (), [("xe", sub_)] + SK + ["cand"], "l_xe%d" % sub_)
                        emit_mid(i)
                        if i >= 1:
                            emit_v(i - 1)
                        run(gen, 1)
                    emit_v(127)
                    xe = buf1[:].rearrange("p (a d) -> p a d", d=D)
                    for sub in range(2):
                        tb = blk * 2 + sub
                        for dh in range(2):
                            tt(xe[:, sub, dh * 512:(dh + 1) * 512], big[sub * 2 + dh][:], xe[:, sub, dh * 512:(dh + 1) * 512], ALU.add,
                               [("big", sub * 2 + dh), ("xe", sub)], [("xe", sub)])
                        ttr(junk[:], xe[:, sub, :], xe[:, sub, :], st3[:, 4:5], [("xe", sub)], ["ss4", "junk"])
                        act(st3[:, 5:6], st3[:, 4:5], AF.Sqrt, ["ss4", "eps_t"], ["rs4"], scale=1.0 / D, bias=eps_t[:])
                        recip(st3[:, 6:7], st3[:, 5:6], ["rs4"], ["rstd4"])
                        stt(xe[:, sub, :], xe[:, sub, :], st3[:, 6:7], gfin[:], ALU.mult, ALU.mult, [("xe", sub), "rstd4", "gfin"], [("xe", sub)])
                        dma(out_d[tb * 128:(tb + 1) * 128, :], xe[:, sub, :], [("xe", sub)], SK + ["cand"], "s_out%d" % sub)
                    run(gen, 10 ** 6)

        with ExitStack() as p0:
            w_bf = sbt(p0, "w_bf", [128, 8, 3072], BF16)
            wst = [sbt(p0, "wst%d" % i, [128, 1024], F32) for i in range(2)]
            gattn = sbt(p0, "gattn", [128, 8], F32)
            ropd = sbt(p0, "ropd", [128, 2, NTB, 8], F32)
            ropr = sbt(p0, "ropr", [128, 2, NTB, 32], F32)
            xt = [sbt(p0, "xt%d" % i, [128, D], F32) for i in range(2)]
            hb = [sbt(p0, "hb%d" % i, [128, D], BF16) for i in range(2)]
            hT = [sbt(p0, "hT%d" % i, [128, 8, 128], BF16) for i in range(2)]
            qk32 = [sbt(p0, "qk32_%d" % i, [128, 1536], F32) for i in range(2)]
            qkb = [sbt(p0, "qkb_%d" % i, [128, 1536], BF16) for i in range(2)]
            rt = [sbt(p0, "rt%d" % i, [128, 256], F32) for i in range(4)]
            qkTst2 = [sbt(p0, "qkTst%d" % i, [128, 12, 512], BF16) for i in range(2)]
            vst = [sbt(p0, "vst%d" % i, [128, 512], BF16) for i in range(2)]
            rvst = [sbt(p0, "rvst%d" % i, [128, 512], BF16) for i in range(2)]
            rgst = [sbt(p0, "rgst%d" % i, [128, 512], BF16) for i in range(2)]
            st0 = sbt(p0, "st0", [128, 8], F32)
            pT = [pst(p0, "pT%d" % i, [128, 8, 128], BF16) for i in range(2)]
            pp = [pst(p0, "pp%d" % i, [128, 512], F32) for i in range(2)]
            pQ1 = pst(p0, "pQ1", [128, 8, 128], BF16)
            pQ2 = pst(p0, "pQ2", [128, 4, 128], BF16)

            dma(gattn[:], gattn_d, (), ["gattn"], "c_g")
            dma(ropd[:], ropd_d, (), ["ropd"], "c_rd")
            dma(ropr[:], ropr_d, (), ["ropr"], "c_rr")
            j = 0
            for k in range(8):
                for c in range(3):
                    s = j % 2
                    dma(wst[s][:], w_in_d[k * 128:(k + 1) * 128, c * 1024:(c + 1) * 1024], (), [("wst", s)], "c_w%d" % s)
                    if j % 2 == 0:
                        act(w_bf[:, k, c * 1024:(c + 1) * 1024], wst[s][:], AF.Copy, [("wst", s), "gattn"], [("w_bf", k, c)],
                            scale=gattn[:, k:k + 1])
                    else:
                        ts(w_bf[:, k, c * 1024:(c + 1) * 1024], wst[s][:], gattn[:, k:k + 1], ALU.mult,
                           [("wst", s), "gattn"], [("w_bf", k, c)])
                    j += 1
            WALL = [("w_bf", k, c) for k in range(8) for c in range(3)]

            def sN1(tb):
                s = tb % 2
                dma(xt[s][:], x_d[tb * 128:(tb + 1) * 128, :], (), [("xt", s)], "c_x%d" % s)
                ss = st0[:, s:s + 1]
                rs = st0[:, 2 + s:3 + s]
                rstd = st0[:, 4 + s:5 + s]
                ttr(junk[:], xt[s][:], xt[s][:], ss, [("xt", s)], [("ss", s), "junk"])
                act(rs, ss, AF.Sqrt, [("ss", s), "eps_t"], [("rs", s)], scale=1.0 / D, bias=eps_t[:])
                recip(rstd, rs, [("rs", s)], [("rstd", s)])
                act(hb[s][:], xt[s][:], AF.Copy, [("xt", s), ("rstd", s)], [("hb", s)], scale=rstd)
            def sN2(tb):
                s = tb % 2
                for k in range(8):
                    tr(pT[s][:, k, :], hb[s][:, k * 128:(k + 1) * 128], identb[:], [("hb", s), "identb"], [("pT", s)])
                vcopy(hT[s][:], pT[s][:], [("pT", s)], [("hT", s)])
            def sP(tb):
                s = tb % 2
                for cg in range(6):
                    ps = pp[cg % 2]
                    pk = ("pp", cg % 2)
                    for k in range(8):
                        mm(ps[:], hT[s][:, k, :], w_bf[:, k, cg * 512:(cg + 1) * 512], k == 0, k == 7,
                           [("hT", s)] + WALL, [pk])
                    if cg == 0:
                        act(qk32[s][:, 0:512], ps[:], AF.Copy, [pk], [("qk32", s, 0)])
                    elif cg == 1:
                        vcopy(qk32[s][:, 512:1024], ps[:], [pk], [("qk32", s, 1)])
                    elif cg == 2:
                        act(vst[s][:], ps[:], AF.Copy, [pk], [("vst", s)])
                    elif cg == 3:
                        vcopy(qk32[s][:, 1024:1536], ps[:], [pk], [("qk32", s, 2)])
                    elif cg == 4:
                        vcopy(rvst[s][:], ps[:], [pk], [("rvst", s)])
                    else:
                        act(rgst[s][:], ps[:], AF.Silu, [pk], [("rgst", s)])
            def sB1(tb):
                s = tb % 2
                QK32 = [("qk32", s, i) for i in range(3)]
                v_d = qk32[s][:, 0:1024].rearrange("p (g d) -> p g d", d=64)
                o_d = qkb[s][:, 0:1024].rearrange("p (g d) -> p g d", d=64)
                cd = ropd[:, 0, tb:tb + 1, :].to_broadcast([128, 16, 8])
                sd = ropd[:, 1, tb:tb + 1, :].to_broadcast([128, 16, 8])
                t = [rt[i][:, 0:128].rearrange("p (g d) -> p g d", d=8) for i in range(4)]
                tt(t[0], v_d[:, :, 0:8], cd, ALU.mult, QK32 + ["ropd"], [("rt", 0)])
                tt(t[1], v_d[:, :, 8:16], sd, ALU.mult, QK32 + ["ropd"], [("rt", 1)])
                tt(o_d[:, :, 0:8], t[0], t[1], ALU.subtract, [("rt", 0), ("rt", 1)], [("qkb", s, 0)])
                tt(t[2], v_d[:, :, 8:16], cd, ALU.mult, QK32 + ["ropd"], [("rt", 2)])
                tt(t[3], v_d[:, :, 0:8], sd, ALU.mult, QK32 + ["ropd"], [("rt", 3)])
                tt(o_d[:, :, 8:16], t[2], t[3], ALU.add, [("rt", 2), ("rt", 3)], [("qkb", s, 1)])
                act(o_d[:, :, 16:64], v_d[:, :, 16:64], AF.Copy, QK32, [("qkb", s, 2)])
                v_r = qk32[s][:, 1024:1536].rearrange("p (g d) -> p g d", d=64)
                o_r = qkb[s][:, 1024:1536].rearrange("p (g d) -> p g d", d=64)
                cr = ropr[:, 0, tb:tb + 1, :].to_broadcast([128, 8, 32])
                sr = ropr[:, 1, tb:tb + 1, :].to_broadcast([128, 8, 32])
                u = [rt[i][:, 0:256].rearrange("p (g d) -> p g d", d=32) for i in range(4)]
                tt(u[0], v_r[:, :, 0:32], cr, ALU.mult, QK32 + ["ropr"], [("rt", 0)])
                tt(u[1], v_r[:, :, 32:64], sr, ALU.mult, QK32 + ["ropr"], [("rt", 1)])
                tt(o_r[:, :, 0:32], u[0], u[1], ALU.subtract, [("rt", 0), ("rt", 1)], [("qkb", s, 3)])
                tt(u[2], v_r[:, :, 32:64], cr, ALU.mult, QK32 + ["ropr"], [("rt", 2)])
                tt(u[3], v_r[:, :, 0:32], sr, ALU.mult, QK32 + ["ropr"], [("rt", 3)])
                tt(o_r[:, :, 32:64], u[2], u[3], ALU.add, [("rt", 2), ("rt", 3)], [("qkb", s, 4)])
                QKB = [("qkb", s, i) for i in range(5)]
            def sB2(tb):
                s = tb % 2
                QKB = [("qkb", s, i) for i in range(5)]
                for c in range(8):
                    tr(pQ1[:, c, :], qkb[s][:, c * 128:(c + 1) * 128], identb[:], QKB + ["identb"], ["pQ1"])
                for c in range(4):
                    tr(pQ2[:, c, :], qkb[s][:, (8 + c) * 128:(9 + c) * 128], identb[:], QKB + ["identb"], ["pQ2"])
                q4 = tb % 4
                qs_ = (tb // 4) % 2
                qkTst = qkTst2[qs_]
                act(qkTst[:, 0:8, q4 * 128:(q4 + 1) * 128], pQ1[:], AF.Copy, ["pQ1"], [("qkTst", qs_, q4, 0)])
                vcopy(qkTst[:, 8:12, q4 * 128:(q4 + 1) * 128], pQ2[:], ["pQ2"], [("qkTst", qs_, q4, 1)])
                dma(Vs[tb * 128:(tb + 1) * 128, :], vst[s][:], [("vst", s)], (), "s_v%d" % s)
                dma(RVs[tb * 128:(tb + 1) * 128, :], rvst[s][:], [("rvst", s)], (), "s_rv%d" % s)
                dma(RGs[tb * 128:(tb + 1) * 128, :], rgst[s][:], [("rgst", s)], (), "s_rg%d" % s)
                if q4 == 3:
                    t4 = tb // 4
                    for c in range(12):
                        dma(QKT[c, :, t4 * 512:(t4 + 1) * 512], qkTst[:, c, :],
                            [("qkTst", qs_, a, b) for a in range(4) for b in range(2)], (), "s_qkt%d_%d" % (qs_, c))


            sN1(0)
            sN2(0)
            for tb in range(NTB):
                if tb >= 1:
                    sB1(tb - 1)
                if tb + 1 < NTB:
                    sN1(tb + 1)
                sP(tb)
                if tb + 1 < NTB:
                    sN2(tb + 1)
                if tb >= 1:
                    sB2(tb - 1)
            sB1(NTB - 1)
            sB2(NTB - 1)
        S.barrier()

        with ExitStack() as p1:
          if stage != "P0":
              qA = [sbt(p1, "qA%d" % i, [128, S_TOK], BF16) for i in range(2)]
              qB = [sbt(p1, "qB%d" % i, [128, S_TOK], BF16) for i in range(2)]
              kT = [sbt(p1, "kT%d" % i, [128, S_TOK], BF16) for i in range(2)]
              vv = [sbt(p1, "vv%d" % i, [128, NTB, 129], BF16) for i in range(2)]
              rgh = [sbt(p1, "rgh%d" % i, [128, NTB, 128], BF16) for i in range(2)]
              et = [sbt(p1, "et%d" % i, [128, 512], BF16) for i in range(3)]
              dlam = sbt(p1, "dlam", [128, 256], F32)
              rld = sbt(p1, "rld", [128, 8], F32)
              lg = sbt(p1, "lg", [128, 8], F32)
              rtab = sbt(p1, "rtab", [128, 10, 256], F32)
              e128 = sbt(p1, "e128", [128, 32], F32)
              Wm = sbt(p1, "Wm", [128, 4, 256], F32)
              wtmp = sbt(p1, "wtmp", [128, 2, 256], F32)
              pw = sbt(p1, "pw", [128, 2, 32], F32)
              oc = sbt(p1, "oc", [128, 2, 129], F32)
              rtmp = [sbt(p1, "rtmp%d" % i, [128, 256], F32) for i in range(2)]
              av = sbt(p1, "av", [128, 2, 128], F32)
              st1 = sbt(p1, "st1", [128, 32], F32)
              sT = [pst(p1, "sT%d" % i, [128, 512], F32) for i in range(4)]
              oacc = [pst(p1, "oacc%d" % i, [128, 512], F32) for i in range(4)]

              conv_eng = ["dve"]
              cgen = None
              if stage == "full":
                  cf1 = [sbt(p1, "cf1_%d" % i, [128, 2048], F32) for i in range(2)]
                  cb1 = [sbt(p1, "cb1_%d" % i, [128, 2048], BF16) for i in range(2)]
                  gffn1 = sbt(p1, "gffn1", [128, 8], F32)
                  dma(gffn1[:], gffn_d, (), ["gffn1"], "c_gf1")

                  def conv_gen():
                      j = 0
                      for k in range(8):
                          for cg in range(8):
                              s_ = j % 2
                              dma(cf1[s_][:], uT_d[k * 128:(k + 1) * 128, cg * 2048:(cg + 1) * 2048], (), [("cf1", s_)], "c_cf1%d" % s_)
                              if conv_eng[0] == "act":
                                  act(cb1[s_][:], cf1[s_][:], AF.Copy, [("cf1", s_), "gffn1"], [("cb1", s_)], scale=gffn1[:, k:k + 1])
                              else:
                                  ts(cb1[s_][:], cf1[s_][:], gffn1[:, k:k + 1], ALU.mult, [("cf1", s_), "gffn1"], [("cb1", s_)])
                              for hf in range(2):
                                  dma(UT2[cg * 16 + hf * 8:cg * 16 + (hf + 1) * 8, :, k, :].rearrange("c p e -> p c e"),
                                      cb1[s_][:, hf * 1024:(hf + 1) * 1024].rearrange("p (c e) -> p c e", e=128),
                                      [("cb1", s_)], (), "s_cb1%d" % s_)
                              j += 1
                              yield
                      for r in range(64):
                          s_ = j % 2
                          dma(cf1[s_][:].rearrange("p (a d) -> p a d", d=D),
                              pv_d[r * 256:(r + 1) * 256, :].rearrange("(a p) d -> p a d", p=128), (), [("cf1", s_)], "c_cf1%d" % s_)
                          if conv_eng[0] == "act":
                              act(cb1[s_][:], cf1[s_][:], AF.Copy, [("cf1", s_)], [("cb1", s_)])
                          else:
                              vcopy(cb1[s_][:], cf1[s_][:], [("cf1", s_)], [("cb1", s_)])
                          dma(Vb[r * 256:(r + 1) * 256, :].rearrange("(a p) d -> p a d", p=128),
                              cb1[s_][:].rearrange("p (a d) -> p a d", d=D), [("cb1", s_)], (), "s_cb1%d" % s_)
                          j += 1
                          yield

                  cgen = conv_gen()
              dma(dlam[:], dlam_d, (), ["dlam"], "c_dl")
              dma(rld[:], rld_d, (), ["rld"], "c_rl")
              dma(rtab[:], rtab_d, (), ["rtab"], "c_rt")
              dma(e128[:], e128_d, (), ["e128"], "c_e1")
              for i in range(2):
                  memset(vv[i][:, :, 128:129], 1.0, [("vv", i)])
                  memset(qA[i][64:128, :], 0.0, [("qT", i)], eng="pool")
                  memset(qB[i][0:64, :], 0.0, [("qT", i)], eng="pool")
              ttr(junk[:, 0:64], dlam[:, 0:64], dlam[:, 64:128], st1[:, 0:1], ["dlam"], ["l1", "junk"])
              ttr(junk[:, 0:64], dlam[:, 128:192], dlam[:, 192:256], st1[:, 1:2], ["dlam"], ["l2", "junk"])
              act(st1[:, 2:4], st1[:, 0:2], AF.Exp, ["l1", "l2"], ["l12e"])
              tt(st1[:, 4:5], st1[:, 3:4], st1[:, 2:3], ALU.subtract, ["l12e"], ["nl0"])
              ts(st1[:, 5:6], st1[:, 4:5], -LAMBDA_INIT, ALU.add, ["nl0"], ["neglam"])
              neglam = st1[:, 5:6]
              act(lg[:], rld[:], AF.Exp, ["rld"], ["lg0"])
              ts(lg[:], lg[:], -1.0, ALU.mult, ["lg0"], ["lg"])

              step = 0
              def emit_head_loads(hh):
                  is_diff = hh < 4
                  h = hh % 4
                  s = hh % 2
                  if is_diff:
                      dma(qA[s][0:64, :], QKT[h, 0:64, :], (), [("qT", s)], "l_q%d" % s)
                      dma(qB[s][64:128, :], QKT[h, 64:128, :], (), [("qT", s)], "l_q%d" % s)
                      dma(kT[s][:], QKT[4 + h], (), [("kT", s)], "l_k%d" % s)
                      for t8 in range(8):
                          dma(vv[s][:, t8 * 4:(t8 + 1) * 4, 0:128],
                              Vs.rearrange("(t p) c -> p t c", p=128)[:, t8 * 4:(t8 + 1) * 4, h * 128:(h + 1) * 128],
                              (), [("vv", s)], "l_v%d" % s)
                  else:
                      if h % 2 == 0:
                          dma(qA[s][0:64, :], QKT[8 + h // 2, 0:64, :], (), [("qT", s)], "l_q%d" % s)
                      else:
                          dma(qB[s][64:128, :], QKT[8 + h // 2, 64:128, :], (), [("qT", s)], "l_q%d" % s)
                      dma(kT[s][:], QKT[10 + h // 2], (), [("kT", s)], "l_k%d" % s)
                      for t8 in range(8):
                          dma(vv[s][:, t8 * 4:(t8 + 1) * 4, 0:128],
                              RVs.rearrange("(t p) c -> p t c", p=128)[:, t8 * 4:(t8 + 1) * 4, h * 128:(h + 1) * 128],
                              (), [("vv", s)], "l_v%d" % s)
                          dma(rgh[s][:, t8 * 4:(t8 + 1) * 4, :],
                              RGs.rearrange("(t p) c -> p t c", p=128)[:, t8 * 4:(t8 + 1) * 4, h * 128:(h + 1) * 128],
                              (), [("rgh", s)], "l_g%d" % s)

              HEADS = DBG["heads"]
              if HEADS:
                  emit_head_loads(HEADS[0])
              for hidx, hh in enumerate(HEADS):
                  is_diff = hh < 4
                  h = hh % 4
                  s = hh % 2
                  OPS = [("qT", s), ("kT", s), ("vv", s)]
                  if hidx + 1 < len(HEADS):
                      emit_head_loads(HEADS[hidx + 1])
                  if not is_diff:
                      pb = (h % 2) * 64
                      lgf = lg[:, h:h + 1]
                      lgb = lg[:, 4 + h:5 + h]
                      act(Wm[:, 0, :], rtab[:, 0, :], AF.Exp, ["rtab", "lg", "ln8_t"], [("Wm", 0)], scale=lgf, bias=ln8_t[:])
                      act(Wm[:, 1, :], rtab[:, 1, :], AF.Exp, ["rtab", "lg", "ln8_t"], [("Wm", 1)], scale=lgb, bias=ln8_t[:])
                      for r_ in range(2):
                          act(wtmp[:, 0, :], rtab[:, 2 + r_, :], AF.Exp, ["rtab", "lg", "ln8_t"], [("wtmp", 0)], scale=lgf, bias=ln8_t[:])
                          act(wtmp[:, 1, :], rtab[:, 4 + r_, :], AF.Exp, ["rtab", "lg", "ln8_t"], [("wtmp", 1)], scale=lgb, bias=ln8_t[:])
                          tt(wtmp[:, 0, :], wtmp[:, 0, :], rtab[:, 6 + r_, :], ALU.mult, [("wtmp", 0), "rtab"], [("wtmp", 0)])
                          tt(wtmp[:, 1, :], wtmp[:, 1, :], rtab[:, 8 + r_, :], ALU.mult, [("wtmp", 1), "rtab"], [("wtmp", 1)])
                          tt(Wm[:, 2 + r_, :], wtmp[:, 0, :], wtmp[:, 1, :], ALU.add, [("wtmp", 0), ("wtmp", 1)], [("Wm", 2 + r_)])
                      act(pw[:, 0, :], e128[:], AF.Exp, ["e128", "lg"], [("pw", 0)], scale=lgf)
                      act(pw[:, 1, :], e128[:], AF.Exp, ["e128", "lg"], [("pw", 1)], scale=lgb)
                  def emit_post(qb):
                      for qs in (range(2) if DBG["post"] else []):
                          tb = qb * 2 + qs
                          if is_diff:
                              vcopy(oc[:, 0, :], oacc[qs][:, 0:129], [("oacc", qs)], [("oc", 0)])
                              act(oc[:, 1, :], oacc[2 + qs][:, 0:129], AF.Copy, [("oacc", 2 + qs)], [("oc", 1)])
                              recip(st1[:, 8:10], oc[:, :, 128], [("oc", 0), ("oc", 1)], ["rs2"])
                              tt(st1[:, 10:11], st1[:, 9:10], neglam, ALU.mult, ["rs2", "neglam"], ["nl"])
                              ts(av[:, 0, :], oc[:, 0, 0:128], st1[:, 8:9], ALU.mult, [("oc", 0), "rs2"], [("av", 0)])
                              stt(av[:, 1, :], oc[:, 1, 0:128], st1[:, 10:11], av[:, 0, :], ALU.mult, ALU.add,
                                  [("oc", 1), "nl", ("av", 0)], [("av", 1)])
                              ttr(junk[:, 0:128], av[:, 1, :], av[:, 1, :], st1[:, 11:12], [("av", 1)], ["ssq", "junk"])
                              act(st1[:, 12:13], st1[:, 11:12], AF.Ln, ["ssq", "eps_t"], ["lnv"], scale=1.0 / 128, bias=eps_t[:])
                              act(st1[:, 13:14], st1[:, 12:13], AF.Exp, ["lnv"], ["rstd1"], scale=-0.5)
                              ts(mix[:, tb, h * 128:(h + 1) * 128], av[:, 1, :], st1[:, 13:14], ALU.mult,
                                 [("av", 1), "rstd1"], [("mix", tb, hh)])
                          else:
                              vcopy(oc[:, 0, 0:128], oacc[qs][:, 0:128], [("oacc", qs)], [("oc", 0)])
                              S.add("dve", lambda e: e.tensor_reduce(out=st1[:, 16:17], in_=oc[:, 0, 0:128], axis=AX.X, op=ALU.add),
                                    [("oc", 0)], ["rsum"])
                              ts(st1[:, 17:18], st1[:, 16:17], -1.0 / 128, ALU.mult, ["rsum"], ["nmean"])
                              ts(av[:, 0, :], oc[:, 0, 0:128], st1[:, 17:18], ALU.add, [("oc", 0), "nmean"], [("av", 0)])
                              ttr(junk[:, 0:128], av[:, 0, :], av[:, 0, :], st1[:, 18:19], [("av", 0)], ["ssq", "junk"])
                              act(st1[:, 19:20], st1[:, 18:19], AF.Ln, ["ssq", "eps_t"], ["lnv"], scale=1.0 / 128, bias=eps_t[:])
                              act(st1[:, 20:21], st1[:, 19:20], AF.Exp, ["lnv"], ["rstd1"], scale=-0.5)
                              stt(mix[:, tb, 512 + h * 128:512 + (h + 1) * 128], av[:, 0, :], st1[:, 20:21], rgh[s][:, tb, :],
                                  ALU.mult, ALU.mult, [("av", 0), "rstd1", ("rgh", s)], [("mix", tb, hh)])
                  def emit_qk(qb, kc, si):
                      qsl = slice(qb * 256, (qb + 1) * 256)
                      ksl = slice(kc * 128, (kc + 1) * 128)
                      if is_diff:
                          mm(sT[si][:, 0:256], kT[s][:, ksl], qA[s][:, qsl], True, True, OPS, [("sT", si)])
                          mm(sT[si][:, 256:512], kT[s][:, ksl], qB[s][:, qsl], True, True, OPS, [("sT", si)])
                      else:
                          mm(sT[si][:, 0:256], kT[s][:, ksl], (qA if h % 2 == 0 else qB)[s][:, qsl], True, True, OPS, [("sT", si)])
                  def emit_mid_av(qb, kc, si, ei):
                      if is_diff:
                          act(et[ei][:], sT[si][:], AF.Exp, [("sT", si)], [("et", ei)], scale=0.125)
                          for m in range(2):
                              for qs in range(2):
                                  a = m * 2 + qs
                                  mm(oacc[a][:, 0:129], et[ei][:, m * 256 + qs * 128:m * 256 + (qs + 1) * 128], vv[s][:, kc, :],
                                     kc == 0, kc == NTB - 1, [("et", ei)] + OPS, [("oacc", a)])
                      else:
                          nn = 2 * qb - kc
                          use_pool = (kc % 3 == 2) and DBG.get("retpool", False)
                          if (nn >= 1 or nn <= -2) and use_pool:
                              d_ = 0 if nn >= 1 else 1
                              pcol = pw[:, 0, nn:nn + 1] if nn >= 1 else pw[:, 1, -nn - 1:-nn]
                              tp_ = rtmp[kc % 2]
                              act(tp_[:], sT[si][:, 0:256], AF.Copy, [("sT", si), ("pw", d_)], [("rtmp", kc % 2)], scale=pcol)
                              tt(et[ei][:, 0:256], tp_[:], Wm[:, d_, :], ALU.mult, [("rtmp", kc % 2), ("Wm", d_)], [("et", ei)], eng="pool")
                          elif nn >= 1:
                              stt(et[ei][:, 0:256], sT[si][:, 0:256], pw[:, 0, nn:nn + 1], Wm[:, 0, :], ALU.mult, ALU.mult,
                                  [("sT", si), ("pw", 0), ("Wm", 0)], [("et", ei)])
                          elif nn <= -2:
                              stt(et[ei][:, 0:256], sT[si][:, 0:256], pw[:, 1, -nn - 1:-nn], Wm[:, 1, :], ALU.mult, ALU.mult,
                                  [("sT", si), ("pw", 1), ("Wm", 1)], [("et", ei)])
                          else:
                              r_ = kc - 2 * qb
                              tt(et[ei][:, 0:256], sT[si][:, 0:256], Wm[:, 2 + r_, :], ALU.mult,
                                 [("sT", si), ("Wm", 2 + r_)], [("et", ei)])
                          for qs in range(2):
                              mm(oacc[qs][:, 0:128], et[ei][:, qs * 128:(qs + 1) * 128], vv[s][:, kc, 0:128],
                                 kc == 0, kc == NTB - 1, [("et", ei)] + OPS, [("oacc", qs)])
                  steps = [(qb_, kc_) for qb_ in range(DBG["nqb"]) for kc_ in range(NTB)]
                  LA = 2
                  for j_ in range(min(LA, len(steps))):
                      emit_qk(steps[j_][0], steps[j_][1], (step + j_) % 4)
                  for n_, (qb_, kc_) in enumerate(steps):
                      if n_ + LA < len(steps):
                          emit_qk(steps[n_ + LA][0], steps[n_ + LA][1], (step + n_ + LA) % 4)
                      emit_mid_av(qb_, kc_, (step + n_) % 4, (step + n_) % 3)
                      if kc_ == NTB - 1:
                          emit_post(qb_)
                          conv_eng[0] = "dve" if is_diff else "act"
                          run_gen(cgen, 1)
                  if hh == DBG["heads"][-1]:
                      run_gen(cgen, 10 ** 6)
                  step += len(steps)
        S.barrier()

        with ExitStack() as p2:
            wo_bf = sbt(p2, "wo_bf", [128, 8, D], BF16)
            wst2 = [sbt(p2, "wst2_%d" % i, [128, D], F32) for i in range(2)]
            gmix = sbt(p2, "gmix", [128, 8], F32)
            mixT = sbt(p2, "mixT", [128, 8, 128], BF16)
            xt2 = [sbt(p2, "xt2_%d" % i, [128, D], F32) for i in range(2)]
            x2 = [sbt(p2, "x2_%d" % i, [128, D], F32) for i in range(2)]
            pM = pst(p2, "pM", [128, 8, 128], BF16)
            pX = [pst(p2, "pX%d" % i, [128, 512], F32) for i in range(2)]
            dma(gmix[:], gmix_d, (), ["gmix"], "c_gm")
            ts(gmix[:, 0:4], gmix[:, 0:4], 1.0 - LAMBDA_INIT, ALU.mult, ["gmix"], ["gmix"])
            for k in range(8):
                s = k % 2
                dma(wst2[s][:], w_out_d[k * 128:(k + 1) * 128, :], (), [("wst2", s)], "c_wo%d" % s)
                act(wo_bf[:, k, :], wst2[s][:], AF.Copy, [("wst2", s), "gmix"], [("wo_bf", k)], scale=gmix[:, k:k + 1])
            WO = [("wo_bf", k) for k in range(8)]
            for tb in range(NTB if stage in ("A", "full") else 0):
                s = tb % 2
                dma(xt2[s][:], x_d[tb * 128:(tb + 1) * 128, :], (), [("xt2", s)], "c_x2%d" % s)
                MIXK = [("mix", tb, hh) for hh in range(8)]
                for k in range(8):
                    tr(pM[:, k, :], mix[:, tb, k * 128:(k + 1) * 128], identb[:], MIXK + ["identb"], ["pM"])
                vcopy(mixT[:], pM[:], ["pM"], ["mixT"])
                for dh in range(2):
                    for k in range(8):
                        mm(pX[dh][:], mixT[:, k, :], wo_bf[:, k, dh * 512:(dh + 1) * 512], k == 0, k == 7,
                           ["mixT"] + WO, [("pX", dh)])
                    tt(x2[s][:, dh * 512:(dh + 1) * 512], pX[dh][:], xt2[s][:, dh * 512:(dh + 1) * 512], ALU.add,
                       [("pX", dh), ("xt2", s)], [("x2", s, dh)])
                if stage == "A":
                    dma(out_d[tb * 128:(tb + 1) * 128, :], x2[s][:], [("x2", s, 0), ("x2", s, 1)], (), "s_o%d" % s)
                else:
                    dma(X2s[tb * 128:(tb + 1) * 128, :], x2[s][:], [("x2", s, 0), ("x2", s, 1)], (), "s_o%d" % s)
        cm.close()
        if stage == "full":
            S.barrier()
            PEER_PHASE()
        if stage not in ("A", "full"):
            dma(out_d[0:128, 0:128], identf[:], ["identf"], (), "s_o0")
        S.emit()
    return nc


def host_inputs(inputs, b, shared=None):
    if shared is None:
        shared = host_shared(inputs)
    f32 = np.float32
    x = np.ascontiguousarray(inputs["x"][b], dtype=f32)
    pos = np.arange(S_TOK, dtype=f32)
    rot_dim = 16
    rope_inv = np.power(f32(500000.0), -np.arange(rot_dim // 2, dtype=f32) * f32(2.0) / f32(rot_dim)).astype(f32)
    ret_inv = (f32(1.0) / np.power(f32(10000.0), np.linspace(0.0, 1.0, 32, dtype=f32))).astype(f32)

    def tab(inv):
        ang = (pos[:, None] * inv[None, :]).astype(f32)
        c = np.cos(ang).astype(f32).reshape(NTB, 128, -1).transpose(1, 0, 2)
        s_ = np.sin(ang).astype(f32).reshape(NTB, 128, -1).transpose(1, 0, 2)
        return np.ascontiguousarray(np.stack([c, s_], axis=1))

    jl = np.arange(128, dtype=f32)[:, None]
    xx = np.arange(256, dtype=f32)[None, :]
    rt = np.zeros((128, 10, 256), f32)
    rt[:, 0] = xx - jl
    rt[:, 1] = jl - xx + 128.0
    for r in range(2):
        dd = xx - 128.0 * r - jl
        rt[:, 2 + r] = np.maximum(dd, 0)
        rt[:, 4 + r] = np.maximum(-dd, 0)
        rt[:, 6 + r] = (dd >= 0).astype(f32)
        rt[:, 8 + r] = (dd < 0).astype(f32)
    gm = np.concatenate([np.tile(inputs["diff_norm_g"][0][:, None], (1, 4)), inputs["ret_norm_g"][0].T], axis=1)
    return {
        "x": x,
        "w_in": np.ascontiguousarray(inputs["w_in"][0], dtype=f32),
        "gattn": np.ascontiguousarray(inputs["attn_norm_g"][0].reshape(8, 128).T, dtype=f32),
        "ropd": tab(rope_inv),
        "ropr": tab(ret_inv),
        "dlam": np.ascontiguousarray(np.tile(inputs["diff_lambda"][0].reshape(1, 256), (128, 1)), dtype=f32),
        "rld": np.ascontiguousarray(np.tile(inputs["ret_log_decay"][0].reshape(1, 8), (128, 1)), dtype=f32),
        "rtab": rt,
        "e128": np.ascontiguousarray(np.tile((128.0 * np.arange(32, dtype=f32))[None, :], (128, 1))),
        "ident": np.eye(128, dtype=f32),
        "w_out": np.ascontiguousarray(inputs["w_out"][0], dtype=f32),
        "gmix": np.ascontiguousarray(gm, dtype=f32),
        "wq": np.ascontiguousarray(inputs["peer_w_query"][0], dtype=f32),
        "gffn": np.ascontiguousarray(inputs["ffn_norm_g"][0].reshape(8, 128).T, dtype=f32),
        "skT": np.ascontiguousarray(inputs["peer_sub_keys"][0].reshape(16, 128, 128).transpose(2, 0, 1), dtype=f32),
        "uT": shared["uT"],
        "pv": shared["pv"],
        "gfin": np.ascontiguousarray(np.tile(inputs["final_norm_g"].reshape(1, D), (128, 1)), dtype=f32),
        "iota": np.ascontiguousarray(np.tile(np.arange(128, dtype=f32)[None, :], (128, 1))),
        "io4": np.ascontiguousarray(np.tile(np.arange(16, dtype=f32)[None, :], (128, 128))),
    }


def host_shared(inputs):
    return {"uT": np.ascontiguousarray(np.asarray(inputs["peer_u"][0], dtype=np.float32).T),
            "pv": np.ascontiguousarray(inputs["peer_v"][0], dtype=np.float32)}


def kernel(**inputs):
    inputs = {k: np.asarray(v) for k, v in inputs.items()}
    nc = build("full")
    shared = host_shared(inputs)
    in_maps = [host_inputs(inputs, b, shared) for b in range(8)]
    res = run_bass_kernel_spmd(nc, in_maps, core_ids=list(range(8)))
    return np.stack([np.asarray(r["out"], dtype=np.float32) for r in res.results], axis=0)
```
